# Optimizing a Trainium2 kernel written in Bass

```python
import math
import jax, jax.numpy as jnp
from jax import lax
import numpy as np

D_MODEL = 1024
BATCH = 1
SEQ = 16384
DEPTH = 4

GRID_W = 64
CTX_LEN = 256
HEAD_DIM = 64
BLK = 128
WINDOW = 128
ROPE_THETA = 10000.0
EPS = 1e-6
NEG = -1e30
N_HEADS_A = 8
N_KV_A = 2
GROUP_A = N_HEADS_A // N_KV_A
N_HEADS_B = 8
N_KV_B = 2
GROUP_B = N_HEADS_B // N_KV_B
QA_W = N_HEADS_A * HEAD_DIM
KVA_W = N_KV_A * HEAD_DIM
QB_W = N_HEADS_B * HEAD_DIM
KVB_W = N_KV_B * HEAD_DIM
IN_AB = QA_W + 2 * KVA_W + QB_W + 2 * KVB_W
AB_SPLITS = (QA_W, QA_W + KVA_W, QA_W + 2 * KVA_W, QA_W + 2 * KVA_W + QB_W, QA_W + 2 * KVA_W + QB_W + KVB_W)
MIX_AB = QA_W + QB_W
N_HEADS_C = 8
DV_C = 2 * HEAD_DIM
IN_C = 3 * N_HEADS_C * DV_C
MIX_C = N_HEADS_C * DV_C
N_GROUPS = 4
EXPERTS_PER_GROUP = 8
N_EXPERTS = N_GROUPS * EXPERTS_PER_GROUP
TOP_K = 2
D_EXPERT = 512
N_EVEN = (DEPTH + 1) // 2
N_ODD = DEPTH // 2

kernel_name = 'hybrid_dit_window_axial_diffattn_hmoe'


def rms_norm(x, g):
    xf = x.astype(jnp.float32)
    y = xf * lax.rsqrt(jnp.mean(xf * xf, axis=-1, keepdims=True) + EPS)
    return (y * g.astype(jnp.float32)).astype(x.dtype)


def modulate(x, g, shift, scale):
    return rms_norm(x, g) * (1 + scale) + shift


def axial_rope_tables(rows_n):
    rows = jnp.broadcast_to(jnp.arange(rows_n, dtype=jnp.float32)[:, None], (rows_n, GRID_W)).reshape(-1)
    cols = jnp.broadcast_to(jnp.arange(GRID_W, dtype=jnp.float32)[None, :], (rows_n, GRID_W)).reshape(-1)
    half = HEAD_DIM // 2
    inv = ROPE_THETA ** (-jnp.arange(0, half, 2, dtype=jnp.float32) / half)
    ang = jnp.concatenate([rows[:, None] * inv, cols[:, None] * inv], axis=-1)
    return jnp.cos(ang), jnp.sin(ang)


def apply_rope(x, cos, sin):
    shp = (1, cos.shape[0]) + (1,) * (x.ndim - 3) + (cos.shape[1],)
    cos = cos.reshape(shp)
    sin = sin.reshape(shp)
    x1, x2 = jnp.split(x.astype(jnp.float32), 2, axis=-1)
    return jnp.concatenate([x1 * cos - x2 * sin, x2 * cos + x1 * sin], axis=-1).astype(x.dtype)


def gqa_sweep(q, k, v):
    b, l, hkv, g, d = q.shape
    qb = q.reshape(b, l // BLK, BLK, hkv, g, d).transpose(1, 0, 2, 3, 4, 5)
    scale = d ** -0.5

    def one_block(qi):
        s = jnp.einsum('bqhgd,bkhd->bhgqk', qi, k).astype(jnp.float32) * scale
        p = jax.nn.softmax(s, axis=-1).astype(v.dtype)
        return jnp.einsum('bhgqk,bkhd->bqhgd', p, v)

    o = lax.map(one_block, qb)
    return o.transpose(1, 0, 2, 3, 4, 5).reshape(b, l, hkv, g, d)


def window_sink_attention(q, k, v, kc, vc, sink):
    b, seq, hkv, g, d = q.shape
    nb = seq // BLK
    qb = q.reshape(b, nb, BLK, hkv, g, d)

    def bands(t):
        tp = jnp.pad(t, ((0, 0), (BLK, BLK), (0, 0), (0, 0))).reshape(b, nb + 2, BLK, hkv, d)
        return jnp.concatenate([tp[:, :-2], tp[:, 1:-1], tp[:, 2:]], axis=2)

    kw, vw = bands(k), bands(v)
    qi = jnp.arange(BLK)[:, None]
    ki = jnp.arange(3 * BLK)[None, :]
    rel = ki - BLK - qi
    kpos = jnp.arange(nb)[:, None, None] * BLK + ki[None] - BLK
    mask = (jnp.abs(rel)[None] <= WINDOW) & (kpos >= 0) & (kpos < seq)
    scale = d ** -0.5
    s_loc = jnp.einsum('bnqhgd,bnkhd->bnhgqk', qb, kw).astype(jnp.float32) * scale
    s_loc = jnp.where(mask[None, :, None, None], s_loc, NEG)
    s_ctx = jnp.einsum('bnqhgd,bchd->bnhgqc', qb, kc).astype(jnp.float32) * scale
    sink_col = jnp.broadcast_to(sink.astype(jnp.float32)[None, None, :, :, None, None], s_loc.shape[:-1] + (1,))
    p = jax.nn.softmax(jnp.concatenate([s_loc, s_ctx, sink_col], axis=-1), axis=-1).astype(v.dtype)
    o = (jnp.einsum('bnhgqk,bnkhd->bnqhgd', p[..., :3 * BLK], vw)
         + jnp.einsum('bnhgqc,bchd->bnqhgd', p[..., 3 * BLK:-1], vc))
    return o.reshape(b, seq, hkv, g, d)


def ctx_sink_attention(q, k, v, sink):
    scale = q.shape[-1] ** -0.5
    s = jnp.einsum('bqhgd,bkhd->bhgqk', q, k).astype(jnp.float32) * scale
    sink_col = jnp.broadcast_to(sink.astype(jnp.float32)[None, :, :, None, None], s.shape[:-1] + (1,))
    p = jax.nn.softmax(jnp.concatenate([s, sink_col], axis=-1), axis=-1)[..., :-1].astype(v.dtype)
    return jnp.einsum('bhgqk,bkhd->bqhgd', p, v)


def diff_sweep(q, k, v, lam):
    b, l, h, _, d = q.shape
    qb = q.reshape(b, l // BLK, BLK, h, 2, d).transpose(1, 0, 2, 3, 4, 5)
    scale = d ** -0.5

    def one_block(qi):
        s = jnp.einsum('bqhcd,bkhcd->bhcqk', qi, k).astype(jnp.float32) * scale
        p = jax.nn.softmax(s, axis=-1)
        a = p[:, :, 0] - lam * p[:, :, 1]
        return jnp.einsum('bhqk,bkhe->bqhe', a.astype(v.dtype), v)

    o = lax.map(one_block, qb)
    return o.transpose(1, 0, 2, 3, 4).reshape(b, l, h, v.shape[-1])


def mixer_ab(h_lat, h_ctx, cos, sin, w_in, w_out, qn_a, kn_a, sink_a, qn_b, kn_b, with_ctx_out):
    def project(h, rope):
        bb, ll, _ = h.shape
        qa, ka, va, qb, kb, vb = jnp.split(h @ w_in, AB_SPLITS, axis=-1)
        qa = rms_norm(qa.reshape(bb, ll, N_KV_A, GROUP_A, HEAD_DIM), qn_a)
        ka = rms_norm(ka.reshape(bb, ll, N_KV_A, HEAD_DIM), kn_a)
        va = va.reshape(bb, ll, N_KV_A, HEAD_DIM)
        qb = rms_norm(qb.reshape(bb, ll, N_KV_B, GROUP_B, HEAD_DIM), qn_b)
        kb = rms_norm(kb.reshape(bb, ll, N_KV_B, HEAD_DIM), kn_b)
        vb = vb.reshape(bb, ll, N_KV_B, HEAD_DIM)
        if rope:
            qa, ka = apply_rope(qa, cos, sin), apply_rope(ka, cos, sin)
            qb, kb = apply_rope(qb, cos, sin), apply_rope(kb, cos, sin)
        return qa, ka, va, qb, kb, vb

    b, seq, _ = h_lat.shape
    qa, ka, va, qb, kb, vb = project(h_lat, True)
    cqa, cka, cva, cqb, ckb, cvb = project(h_ctx, False)
    sink = sink_a.reshape(N_KV_A, GROUP_A)
    oa = window_sink_attention(qa, ka, va, cka, cva, sink)
    ob = gqa_sweep(qb, jnp.concatenate([kb, ckb], axis=1), jnp.concatenate([vb, cvb], axis=1))
    o_lat = jnp.concatenate([oa.reshape(b, seq, QA_W), ob.reshape(b, seq, QB_W)], axis=-1) @ w_out
    o_ctx = None
    if with_ctx_out:
        cl = h_ctx.shape[1]
        oca = ctx_sink_attention(cqa, cka, cva, sink)
        ocb = gqa_sweep(cqb, ckb, cvb)
        o_ctx = jnp.concatenate([oca.reshape(b, cl, QA_W), ocb.reshape(b, cl, QB_W)], axis=-1) @ w_out
    return o_lat, o_ctx


def mixer_c(h_lat, h_ctx, cos, sin, w_in, w_out, qn, kn, lam_p, subln, lam_init, with_ctx_out):
    lp = lam_p.astype(jnp.float32)
    lam = jnp.exp(jnp.sum(lp[0] * lp[1])) - jnp.exp(jnp.sum(lp[2] * lp[3])) + lam_init

    def project(h, rope):
        bb, ll, _ = h.shape
        q, k, v = jnp.split(h @ w_in, 3, axis=-1)
        q = rms_norm(q.reshape(bb, ll, N_HEADS_C, 2, HEAD_DIM), qn)
        k = rms_norm(k.reshape(bb, ll, N_HEADS_C, 2, HEAD_DIM), kn)
        v = v.reshape(bb, ll, N_HEADS_C, DV_C)
        if rope:
            q, k = apply_rope(q, cos, sin), apply_rope(k, cos, sin)
        return q, k, v

    def finish(o):
        o = rms_norm(o, subln) * (1 - lam_init)
        return o.reshape(o.shape[0], o.shape[1], MIX_C) @ w_out

    q_l, k_l, v_l = project(h_lat, True)
    q_c, k_c, v_c = project(h_ctx, False)
    o_lat = finish(diff_sweep(q_l, jnp.concatenate([k_l, k_c], axis=1), jnp.concatenate([v_l, v_c], axis=1), lam))
    o_ctx = finish(diff_sweep(q_c, k_c, v_c, lam)) if with_ctx_out else None
    return o_lat, o_ctx


def hier_moe(h, w_group, b_group, w_expert, b_expert, w1, w3, w2):
    t, d = h.shape
    pg = jax.nn.softmax((h @ w_group).astype(jnp.float32) + b_group.astype(jnp.float32), axis=-1)
    g_prob, g_idx = lax.top_k(pg, 1)
    le = ((h @ w_expert).astype(jnp.float32) + b_expert.astype(jnp.float32)).reshape(t, N_GROUPS, EXPERTS_PER_GROUP)
    le = jnp.take_along_axis(le, g_idx[:, :, None], axis=1)[:, 0]
    e_prob, e_idx = lax.top_k(jax.nn.softmax(le, axis=-1), TOP_K)
    weights = g_prob * e_prob / jnp.sum(e_prob, axis=-1, keepdims=True)
    expert_id = g_idx * EXPERTS_PER_GROUP + e_idx
    flat_e = expert_id.reshape(-1)
    flat_tok = jnp.repeat(jnp.arange(t), TOP_K)
    flat_w = weights.reshape(-1)
    order = jnp.argsort(flat_e)
    se, stok, sw = flat_e[order], flat_tok[order], flat_w[order]
    counts = jnp.bincount(flat_e, length=N_EXPERTS)
    padded = (counts + BLK - 1) // BLK * BLK
    start = jnp.cumsum(counts) - counts
    pend = jnp.cumsum(padded)
    dest = (pend - padded)[se] + jnp.arange(flat_e.shape[0]) - start[se]
    n_rows = (t * TOP_K + BLK - 1) // BLK * BLK + N_EXPERTS * BLK
    n_blk = n_rows // BLK
    buf = jnp.zeros((n_rows, d), h.dtype).at[dest].set(h[stok])
    blk_e = jnp.minimum(jnp.searchsorted(pend, jnp.arange(n_blk) * BLK, side='right'), N_EXPERTS - 1)

    def expert_block(args):
        xb, e = args
        return (jax.nn.silu(xb @ w1[e]) * (xb @ w3[e])) @ w2[e]

    yb = lax.map(expert_block, (buf.reshape(n_blk, BLK, d), blk_e))
    y = yb.reshape(n_rows, d)[dest] * sw[:, None].astype(h.dtype)
    return jnp.zeros_like(h).at[stok].add(y)


def setup_inputs(seed: int = 0) -> dict:
    key = jax.random.key(seed)
    ks = iter(jax.random.split(key, 40))

    def nrm(shape, scale):
        return jax.random.normal(next(ks), shape, jnp.float32) * scale

    D = D_MODEL
    return {
        'x': nrm((BATCH, SEQ, D), 1.0),
        'c': nrm((BATCH, D), 1.0),
        'ctx': nrm((BATCH, CTX_LEN, D), 1.0),
        'c_ctx': nrm((D,), 1.0),
        'w_mod': nrm((DEPTH, D, 6 * D), 0.5 * D ** -0.5),
        'b_mod': nrm((DEPTH, 6 * D), 0.01),
        'norm_mix': 1.0 + nrm((DEPTH, D), 0.05),
        'norm_ffn': 1.0 + nrm((DEPTH, D), 0.05),
        'w_in_ab': nrm((N_EVEN, D, IN_AB), D ** -0.5),
        'w_out_ab': nrm((N_EVEN, MIX_AB, D), MIX_AB ** -0.5),
        'qn_a': 1.0 + nrm((N_EVEN, HEAD_DIM), 0.05),
        'kn_a': 1.0 + nrm((N_EVEN, HEAD_DIM), 0.05),
        'sink_a': nrm((N_EVEN, N_HEADS_A), 0.5),
        'qn_b': 1.0 + nrm((N_EVEN, HEAD_DIM), 0.05),
        'kn_b': 1.0 + nrm((N_EVEN, HEAD_DIM), 0.05),
        'w_in_c': nrm((N_ODD, D, IN_C), D ** -0.5),
        'w_out_c': nrm((N_ODD, MIX_C, D), MIX_C ** -0.5),
        'qn_c': 1.0 + nrm((N_ODD, HEAD_DIM), 0.05),
        'kn_c': 1.0 + nrm((N_ODD, HEAD_DIM), 0.05),
        'lam_c': nrm((N_ODD, 4, HEAD_DIM), 0.1),
        'subln_c': 1.0 + nrm((N_ODD, DV_C), 0.05),
        'w_group': nrm((DEPTH, D, N_GROUPS), D ** -0.5),
        'b_group': nrm((DEPTH, N_GROUPS), 0.01),
        'w_expert': nrm((DEPTH, D, N_EXPERTS), D ** -0.5),
        'b_expert': nrm((DEPTH, N_EXPERTS), 0.01),
        'w1': nrm((DEPTH, N_EXPERTS, D, D_EXPERT), D ** -0.5),
        'w3': nrm((DEPTH, N_EXPERTS, D, D_EXPERT), D ** -0.5),
        'w2': nrm((DEPTH, N_EXPERTS, D_EXPERT, D), D_EXPERT ** -0.5),
    }


def reference(x, c, ctx, c_ctx, w_mod, b_mod, norm_mix, norm_ffn, w_in_ab, w_out_ab, qn_a, kn_a, sink_a,
              qn_b, kn_b, w_in_c, w_out_c, qn_c, kn_c, lam_c, subln_c, w_group, b_group, w_expert, b_expert,
              w1, w3, w2):
    b, s_lat, d = x.shape
    c_len = ctx.shape[1]
    rows_n = s_lat // GRID_W
    cos, sin = axial_rope_tables(rows_n)
    for l in range(DEPTH):
        last = l == DEPTH - 1
        i = l // 2
        mod = jax.nn.silu(c) @ w_mod[l] + b_mod[l]
        mod_c = jax.nn.silu(c_ctx) @ w_mod[l] + b_mod[l]
        sh1, sc1, gt1, sh2, sc2, gt2 = jnp.split(mod[:, None, :], 6, axis=-1)
        csh1, csc1, cgt1, csh2, csc2, cgt2 = jnp.split(mod_c, 6, axis=-1)
        h_lat = modulate(x, norm_mix[l], sh1, sc1)
        h_ctx = modulate(ctx, norm_mix[l], csh1, csc1)
        if l % 2 == 0:
            o_lat, o_ctx = mixer_ab(h_lat, h_ctx, cos, sin, w_in_ab[i], w_out_ab[i], qn_a[i], kn_a[i], sink_a[i],
                                    qn_b[i], kn_b[i], not last)
        else:
            lam_init = 0.8 - 0.6 * math.exp(-0.3 * l)
            o_lat, o_ctx = mixer_c(h_lat, h_ctx, cos, sin, w_in_c[i], w_out_c[i], qn_c[i], kn_c[i], lam_c[i],
                                   subln_c[i], lam_init, not last)
        x = x + gt1 * o_lat
        if not last:
            ctx = ctx + cgt1 * o_ctx
            tokens = jnp.concatenate([modulate(ctx, norm_ffn[l], csh2, csc2),
                                      modulate(x, norm_ffn[l], sh2, sc2)], axis=1).reshape(-1, d)
            f = hier_moe(tokens, w_group[l], b_group[l], w_expert[l], b_expert[l], w1[l], w3[l], w2[l])
            f = f.reshape(b, c_len + s_lat, d)
            ctx = ctx + cgt2 * f[:, :c_len]
            x = x + gt2 * f[:, c_len:]
        else:
            tokens = modulate(x, norm_ffn[l], sh2, sc2).reshape(-1, d)
            f = hier_moe(tokens, w_group[l], b_group[l], w_expert[l], b_expert[l], w1[l], w3[l], w2[l])
            x = x + gt2 * f.reshape(b, s_lat, d)
    return x
```

```python
import contextlib
import math
import numpy as np
import ml_dtypes
import concourse.bass as bass
import concourse.mybir as mybir
from concourse.bass_utils import run_bass_kernel_spmd

F32 = mybir.dt.float32
BF16 = mybir.dt.bfloat16
I32 = mybir.dt.int32
AF = mybir.ActivationFunctionType
ALU = mybir.AluOpType
AX = mybir.AxisListType
NPBF = ml_dtypes.bfloat16

ENGS = ("tensor", "vector", "scalar", "gpsimd", "sync")
NCORES = 8
SEQ = 16384
D = 1024
CTX = 256
OWN = SEQ // NCORES
NT = (OWN + CTX) // 128
TOK = NT * 128
NKT = (SEQ + CTX) // 128
NBLK = 2 * NT + 32
EPS = 1e-6


class _Op:
    __slots__ = ("eng", "fn", "deps", "is_dma", "chan", "chan_val", "needs_inc", "inc_val")

    def __init__(self, eng, fn, is_dma=False, chan=None):
        self.eng = eng
        self.fn = fn
        self.deps = []
        self.is_dma = is_dma
        self.chan = chan
        self.chan_val = 0
        self.needs_inc = False
        self.inc_val = 0


class Prog:
    _uid = 0

    def __init__(self, nc):
        self.nc = nc
        self.ops = {e: [] for e in ENGS}
        self.last_write = {}
        self.reads_since = {}
        self.chan_count = {}

    def _add(self, op, reads, writes):
        deps = []
        for r in reads:
            w = self.last_write.get(r)
            if w is not None:
                deps.append(w)
        for r in writes:
            w = self.last_write.get(r)
            if w is not None:
                deps.append(w)
            deps.extend(self.reads_since.get(r, ()))
        seen = set()
        for d in deps:
            if d is op or id(d) in seen:
                continue
            seen.add(id(d))
            if (not d.is_dma) and (not op.is_dma) and d.eng == "tensor" and op.eng == "tensor":
                continue
            op.deps.append(d)
        for r in reads:
            self.reads_since.setdefault(r, []).append(op)
        for r in writes:
            self.last_write[r] = op
            self.reads_since[r] = []
        self.ops[op.eng].append(op)
        return op

    def op(self, eng, fn, reads=(), writes=()):
        return self._add(_Op(eng, fn), reads, writes)

    def dma(self, eng, fn, chan, reads=(), writes=()):
        o = _Op(eng, fn, is_dma=True, chan=chan)
        self.chan_count[chan] = self.chan_count.get(chan, 0) + 16
        o.chan_val = self.chan_count[chan]
        return self._add(o, reads, writes)

    def emit(self):
        nc = self.nc
        for e in ENGS:
            for o in self.ops[e]:
                for d in o.deps:
                    if not d.is_dma:
                        d.needs_inc = True
        for e in ENGS:
            c = 0
            for o in self.ops[e]:
                if (not o.is_dma) and o.needs_inc:
                    c += 1
                    o.inc_val = c
        chans = sorted(self.chan_count.keys(), key=str)
        prog = self
        with contextlib.ExitStack() as st:
            Prog._uid += 1
            u = Prog._uid
            esem = {e: st.enter_context(nc.semaphore("se%d_%s" % (u, e))) for e in ENGS if e != "sync"}
            csem = {c: st.enter_context(nc.semaphore("sc%d_%d" % (u, i))) for i, c in enumerate(chans)}
            block = st.enter_context(nc.Block())

            def make(ename):
                def body(eng):
                    waited = {}
                    for o in prog.ops[ename]:
                        for d in o.deps:
                            if d.is_dma:
                                key, val, sem = ("c", d.chan), d.chan_val, csem[d.chan]
                            else:
                                key, val, sem = ("e", d.eng), d.inc_val, esem[d.eng]
                            if waited.get(key, 0) >= val:
                                continue
                            waited[key] = val
                            eng.wait_ge(sem, val)
                        ins = o.fn(eng)
                        if o.is_dma:
                            ins.then_inc(csem[o.chan], 16)
                        elif o.needs_inc:
                            ins.then_inc(esem[ename], 1)
                    if ename == "sync":
                        for c in chans:
                            eng.wait_ge(csem[c], prog.chan_count[c])
                return body

            for e in ENGS:
                getattr(block, e)(make(e))


def MM(P, out, lhsT, rhs, start, stop, reads, writes):
    P.op("tensor", lambda e: e.matmul(out, lhsT=lhsT, rhs=rhs, start=start, stop=stop), reads, writes)


def TR(P, out, in_, ident, reads, writes):
    P.op("tensor", lambda e: e.transpose(out, in_, ident), reads, writes)


def ACT(P, out, in_, func, reads, writes, scale=None, bias=None, accum_out=None):
    kw = {}
    if scale is not None:
        kw["scale"] = scale
    if bias is not None:
        kw["bias"] = bias
    if accum_out is not None:
        kw["accum_out"] = accum_out
    P.op("scalar", lambda e: e.activation(out=out, in_=in_, func=func, **kw), reads, writes)


def TT(P, eng, out, in0, in1, op, reads, writes):
    P.op(eng, lambda e: e.tensor_tensor(out=out, in0=in0, in1=in1, op=op), reads, writes)


def TS(P, eng, out, in0, s1, op0, reads, writes, s2=None, op1=None):
    if op1 is None:
        P.op(eng, lambda e: e.tensor_scalar(out=out, in0=in0, scalar1=s1, scalar2=None, op0=op0), reads, writes)
    else:
        P.op(eng, lambda e: e.tensor_scalar(out=out, in0=in0, scalar1=s1, scalar2=s2, op0=op0, op1=op1), reads, writes)


def STT(P, out, in0, scalar, in1, op0, op1, reads, writes):
    P.op("vector", lambda e: e.scalar_tensor_tensor(out=out, in0=in0, scalar=scalar, in1=in1, op0=op0, op1=op1),
         reads, writes)


def CP(P, eng, out, in_, reads, writes):
    if eng == "scalar":
        P.op(eng, lambda e: e.copy(out=out, in_=in_), reads, writes)
    else:
        P.op(eng, lambda e: e.tensor_copy(out=out, in_=in_), reads, writes)


def RED(P, out, in_, op, reads, writes):
    P.op("vector", lambda e: e.tensor_reduce(out=out, in_=in_, axis=AX.X, op=op), reads, writes)


def RCP(P, out, in_, reads, writes):
    P.op("vector", lambda e: e.reciprocal(out=out, in_=in_), reads, writes)


def MSET(P, eng, ap, val, writes):
    P.op(eng, lambda e: e.memset(ap, val), (), writes)


def DMA(P, eng, out, in_, chan, reads, writes):
    P.dma(eng, lambda e: e.dma_start(out=out, in_=in_), chan, reads, writes)


def make_ident(P, nc, ident, idf):
    P.op("gpsimd", lambda e: e.iota(idf[:], pattern=[[1, 128]], base=0, channel_multiplier=-1,
                                     allow_small_or_imprecise_dtypes=True), (), ["idf"])
    TS(P, "vector", ident[:], idf[:], 0.0, ALU.is_equal, ["idf"], ["ident"])


def rms_rstd(P, x_ap, junk_ap, ss, rt, rstd, epst, n, rd, tag):
    ACT(P, junk_ap, x_ap, AF.Square, rd, ["junk" + tag, "ss" + tag], accum_out=ss[:, 0:1])
    ACT(P, rt[:, 0:1], ss[:, 0:1], AF.Sqrt, ["ss" + tag], ["rt" + tag], scale=1.0 / n, bias=epst[:, 0:1])
    RCP(P, rstd[:, 0:1], rt[:, 0:1], ["rt" + tag], ["rstd" + tag])


def mod_broadcast(P, dst_ap, psb, sel_sb, mod_sb, r, col0, mul_ap, reads_extra, wname, k, pname="psb"):
    for half in range(2):
        ps = psb[(k + half) % 2]
        pn = pname + "%d" % ((k + half) % 2)
        MM(P, ps[:], sel_sb[:, r, :], mod_sb[:, col0 + half * 512: col0 + (half + 1) * 512], True, True,
           ["sel", "mod"], [pn])
        d = dst_ap[:, half * 512:(half + 1) * 512]
        if mul_ap is None:
            CP(P, "scalar", d, ps[:], [pn], [wname])
        else:
            STT(P, d, ps[:], 1.0, mul_ap[:, half * 512:(half + 1) * 512], ALU.add, ALU.mult,
                [pn] + reads_extra, [wname])


def load_mod_bcast(nc, P, stack, modi, sel, col0, ncols, tag, psb=None):
    mod_sb = stack.enter_context(nc.sbuf_tensor("mod_sb" + tag, [2, ncols], F32))
    sel_sb = stack.enter_context(nc.sbuf_tensor("sel_sb" + tag, [2, 2, 128], F32))
    if psb is None:
        psb = [stack.enter_context(nc.psum_tensor("psb%s%d" % (tag, i), [128, 512], F32)) for i in range(2)]
    DMA(P, "sync", mod_sb[:], modi[:, col0:col0 + ncols], "c_mod", [], ["mod"])
    DMA(P, "sync", sel_sb[:], sel[:, :, :], "c_sel", [], ["sel"])
    return mod_sb, sel_sb, psb


def build_pre(kind):
    ncol = 1536 if kind == "ab" else 3072
    nnorm = 1280 if kind == "ab" else 2048
    G = nnorm // 64
    nc = bass.Bass("TRN2", target_bir_lowering=False)

    def din(name, shape, dt=F32):
        return nc.dram_tensor(name, shape, dt, kind="ExternalInput").ap()

    xin = din("xin", [TOK, D])
    scT = din("scT", [128, 8, 2])
    wmod = din("wmod", [D, 6 * D])
    bmod2 = din("bmod2", [2, 6 * D])
    nmix = din("nmix", [128, D])
    win = din("win", [D, ncol])
    gains = din("gains", [128, nnorm])
    cs = din("cs", [128, 16, 64])
    sel = din("sel", [2, 2, 128])
    qkv = nc.dram_tensor("qkv", [TOK, ncol], BF16, kind="ExternalOutput").ap()
    modo = nc.dram_tensor("modo", [2, 6 * D], F32, kind="ExternalOutput").ap()

    with contextlib.ExitStack() as st:
        def S(name, shape, dt):
            return st.enter_context(nc.sbuf_tensor(name, shape, dt))

        def PS(name, shape, dt):
            return st.enter_context(nc.psum_tensor(name, shape, dt))

        ident = S("ident", [128, 128], BF16)
        idf = S("idf", [128, 128], F32)
        win_sb = S("win_sb", [128, 8, ncol], BF16)
        Gb = S("Gb", [128, 2, D], F32)
        SHb = S("SHb", [128, 2, D], F32)
        gains_sb = S("gains_sb", [128, nnorm], F32)
        cs_sb = S("cs_sb", [128, 16, 64], F32)
        epst = S("epst", [128, 1], F32)
        st0 = contextlib.ExitStack()

        def S0(name, shape, dt):
            return st0.enter_context(nc.sbuf_tensor(name, shape, dt))

        def PS0(name, shape, dt):
            return st0.enter_context(nc.psum_tensor(name, shape, dt))

        mod_sb = S0("mod_sb", [2, 6 * D], F32)
        bm_sb = S0("bm_sb", [2, 6 * D], F32)
        sel_sb = S0("sel_sb", [2, 2, 128], F32)
        nmix_sb = S0("nmix_sb", [128, D], F32)
        sct = S0("sct", [128, 8, 2], F32)
        wmt = [S0("wm%d" % i, [128, 8, 512], F32) for i in range(2)]
        psm = [PS0("psm%d" % i, [2, 512], F32) for i in range(2)]
        psb = [PS0("psb%d" % i, [128, 512], F32) for i in range(2)]

        P = Prog(nc)
        make_ident(P, nc, ident, idf)
        MSET(P, "vector", epst[:], EPS, ["eps"])
        DMA(P, "sync", sct[:], scT[:, :, :], "c_sc", [], ["sct"])
        ACT(P, sct[:], sct[:], AF.Silu, ["sct"], ["sct"])
        DMA(P, "sync", bm_sb[:], bmod2[:, :], "c_bm", [], ["bm"])
        DMA(P, "sync", sel_sb[:], sel[:, :, :], "c_sel", [], ["sel"])
        DMA(P, "sync", nmix_sb[:], nmix[:, :], "c_nm", [], ["nmix"])
        DMA(P, "sync", gains_sb[:], gains[:, :], "c_gn", [], ["gains"])
        DMA(P, "sync", cs_sb[:], cs[:, :, :], "c_cs", [], ["cs"])
        for c in range(8):
            DMA(P, "gpsimd", win_sb[:, c, :], win[c * 128:(c + 1) * 128, :], "c_win", [], ["win%d" % c])
        wmod_v = wmod.rearrange("(c p) n -> p c n", p=128)
        for j in range(12):
            b = j % 2
            DMA(P, "sync", wmt[b][:], wmod_v[:, :, j * 512:(j + 1) * 512], "c_wm%d" % b, [], ["wm%d" % b])
            for c in range(8):
                MM(P, psm[b][:], sct[:, c, :], wmt[b][:, c, :], c == 0, c == 7, ["sct", "wm%d" % b], ["psm%d" % b])
            TT(P, "vector", mod_sb[:, j * 512:(j + 1) * 512], psm[b][:], bm_sb[:, j * 512:(j + 1) * 512], ALU.add,
               ["psm%d" % b, "bm"], ["mod"])
        DMA(P, "sync", modo[:, :], mod_sb[:], "c_mo", ["mod"], [])
        for r in range(2):
            mod_broadcast(P, SHb[:, r, :], psb, sel_sb, mod_sb, r, 0, None, [], "SHb", 0)
            mod_broadcast(P, Gb[:, r, :], psb, sel_sb, mod_sb, r, 1024, nmix_sb, ["nmix"], "Gb", 0)
        P.emit()
        st0.close()

        xt = [S("xt%d" % i, [128, D], F32) for i in range(2)]
        junk = S("junk", [128, D], BF16)
        ss = [S("ss%d" % i, [128, 1], F32) for i in range(2)]
        rt = [S("rt%d" % i, [128, 1], F32) for i in range(2)]
        rstd = [S("rstd%d" % i, [128, 1], F32) for i in range(2)]
        hb = [S("hb%d" % i, [128, D], BF16) for i in range(2)]
        hT = [S("hT%d" % i, [128, D], BF16) for i in range(2)]
        qf = [S("qf%d" % i, [128, ncol], F32) for i in range(2)]
        sqs = [S("sq%d" % i, [128, nnorm], F32) for i in range(2)]
        ssqs = [S("ssq%d" % i, [128, G], F32) for i in range(2)]
        rqs = [S("rq%d" % i, [128, G], F32) for i in range(2)]
        rq2s = [S("rq2%d" % i, [128, G], F32) for i in range(2)]
        t1 = S("t1", [128, G, 32], F32)
        t2 = S("t2", [128, G, 32], F32)
        t3 = S("t3", [128, G, 32], F32)
        t4 = S("t4", [128, G, 32], F32)
        ob = [S("ob%d" % i, [128, ncol], BF16) for i in range(2)]
        pT = PS("pT", [128, D], BF16)
        pq = [PS("pq%d" % i, [128, 512], F32) for i in range(3)]

        P = Prog(nc)
        nq_box = [0]

        def stage_a(t):
            b = t % 2
            r = 0 if t < 16 else 1
            X = "x%d" % b
            if t == 0:
                DMA(P, "sync", xt[0][:], xin[0:128, :], "c_x0", [], ["x0"])
            if t + 1 < NT:
                nb_ = (t + 1) % 2
                DMA(P, "sync", xt[nb_][:], xin[(t + 1) * 128:(t + 2) * 128, :], "c_x%d" % nb_, [], ["x%d" % nb_])
            rms_rstd(P, xt[b][:], junk[:], ss[b], rt[b], rstd[b], epst, D, [X], str(b))
            STT(P, xt[b][:], xt[b][:], rstd[b][:, 0:1], Gb[:, r, :], ALU.mult, ALU.mult, [X, "rstd%d" % b], [X])
            TT(P, "gpsimd", hb[b][:], xt[b][:], SHb[:, r, :], ALU.add, [X], ["hb%d" % b])
            for c in range(8):
                TR(P, pT[:, c * 128:(c + 1) * 128], hb[b][:, c * 128:(c + 1) * 128], ident[:], ["hb%d" % b], ["pT"])
            CP(P, "scalar", hT[b][:], pT[:], ["pT"], ["hT%d" % b])
            for jc in range(ncol // 512):
                pp = nq_box[0] % 3
                nq_box[0] += 1
                for c in range(8):
                    MM(P, pq[pp][:], hT[b][:, c * 128:(c + 1) * 128], win_sb[:, c, jc * 512:(jc + 1) * 512],
                       c == 0, c == 7, ["hT%d" % b], ["pq%d" % pp])
                CP(P, "scalar" if jc % 2 == 0 else "vector", qf[b][:, jc * 512:(jc + 1) * 512], pq[pp][:],
                   ["pq%d" % pp], ["qf%d" % b])
            QF = "qf%d" % b
            OB = "ob%d" % b

        def stage_b(t):
            b = t % 2
            QF = "qf%d" % b
            OB = "ob%d" % b
            sq, ssq, rq, rq2 = sqs[b], ssqs[b], rqs[b], rq2s[b]
            SQ, SSQ, RQ, RQ2 = "sq%d" % b, "ssq%d" % b, "rq%d" % b, "rq2%d" % b
            TT(P, "gpsimd", sq[:], qf[b][:, 0:nnorm], qf[b][:, 0:nnorm], ALU.mult, [QF], [SQ])
            RED(P, ssq[:], sq[:].rearrange("p (g d) -> p g d", d=64), ALU.add, [SQ], [SSQ])
            ACT(P, rq[:], ssq[:], AF.Sqrt, [SSQ], [RQ], scale=1.0 / 64, bias=epst[:, 0:1])
            RCP(P, rq2[:], rq[:], [RQ], [RQ2])
            qfv = qf[b][:, 0:nnorm].rearrange("p (g d) -> p g d", d=64)
            TT(P, "vector", qfv, qfv, rq2[:].unsqueeze(2).broadcast_to([128, G, 64]), ALU.mult, [QF, RQ2], [QF])
            if t < 16:
                TT(P, "gpsimd", qf[b][:, 0:nnorm], qf[b][:, 0:nnorm], gains_sb[:], ALU.mult, [QF], [QF])
                qv = qf[b][:, 0:nnorm].rearrange("p (g h d) -> p g h d", h=2, d=32)
                ov = ob[b][:, 0:nnorm].rearrange("p (g h d) -> p g h d", h=2, d=32)
                cosb = cs_sb[:, t, 0:32].unsqueeze(1).broadcast_to([128, G, 32])
                sinb = cs_sb[:, t, 32:64].unsqueeze(1).broadcast_to([128, G, 32])
                TT(P, "vector", t1[:], qv[:, :, 0, :], cosb, ALU.mult, [QF], ["t1"])
                TT(P, "vector", t2[:], qv[:, :, 1, :], sinb, ALU.mult, [QF], ["t2"])
                TT(P, "vector", ov[:, :, 0, :], t1[:], t2[:], ALU.subtract, ["t1", "t2"], [OB + "a"])
                TT(P, "gpsimd", t3[:], qv[:, :, 1, :], cosb, ALU.mult, [QF], ["t3"])
                TT(P, "gpsimd", t4[:], qv[:, :, 0, :], sinb, ALU.mult, [QF], ["t4"])
                TT(P, "gpsimd", ov[:, :, 1, :], t3[:], t4[:], ALU.add, ["t3", "t4"], [OB + "b"])
            else:
                TT(P, "gpsimd", ob[b][:, 0:nnorm], qf[b][:, 0:nnorm], gains_sb[:], ALU.mult, [QF], [OB + "a", OB + "b"])
            CP(P, "scalar", ob[b][:, nnorm:ncol], qf[b][:, nnorm:ncol], [QF], [OB + "c"])
            DMA(P, "sync", qkv[t * 128:(t + 1) * 128, :], ob[b][:], "c_o%d" % b, [OB + "a", OB + "b", OB + "c"], [])

        stage_a(0)
        for t in range(NT):
            if t + 1 < NT:
                stage_a(t + 1)
            stage_b(t)
        P.emit()
    return nc


_PROG_CACHE = {}


def get_prog(key, builder):
    if key not in _PROG_CACHE:
        _PROG_CACHE[key] = builder()
    return _PROG_CACHE[key]


def rope_tables():
    rows_n = SEQ // 64
    rows = np.repeat(np.arange(rows_n, dtype=np.float32), 64)
    cols = np.tile(np.arange(64, dtype=np.float32), rows_n)
    inv = (np.float32(10000.0) ** (-np.arange(0, 32, 2, dtype=np.float32) / np.float32(32))).astype(np.float32)
    ang = np.concatenate([rows[:, None] * inv, cols[:, None] * inv], axis=-1).astype(np.float32)
    return np.cos(ang).astype(np.float32), np.sin(ang).astype(np.float32)


def pre_inputs(inp, l, x, ctx):
    i = l // 2
    kind = "ab" if l % 2 == 0 else "c"
    cc = np.stack([inp["c"][0], inp["c_ctx"]]).astype(np.float32)
    scT = np.ascontiguousarray(cc.reshape(2, 8, 128).transpose(2, 1, 0))
    bmod2 = np.ascontiguousarray(np.broadcast_to(inp["b_mod"][l], (2, 6 * D)))
    nmix = np.ascontiguousarray(np.broadcast_to(inp["norm_mix"][l], (128, D)))
    if kind == "ab":
        w = inp["w_in_ab"][i]
        win = np.concatenate([w[:, 0:512], w[:, 768:1280], w[:, 512:640], w[:, 1280:1408], w[:, 640:768],
                              w[:, 1408:1536]], axis=1)
        g = np.concatenate([np.tile(inp["qn_a"][i], 8), np.tile(inp["qn_b"][i], 8), np.tile(inp["kn_a"][i], 2),
                            np.tile(inp["kn_b"][i], 2)])
    else:
        win = inp["w_in_c"][i]
        g = np.concatenate([np.tile(inp["qn_c"][i], 16), np.tile(inp["kn_c"][i], 16)])
    win = np.ascontiguousarray(win.astype(np.float32))
    gains = np.ascontiguousarray(np.broadcast_to(g.astype(np.float32), (128, g.shape[0])))
    cos, sin = rope_tables()
    sel = np.zeros((2, 2, 128), np.float32)
    sel[0, 0] = 1
    sel[1, 1] = 1
    maps = []
    for c in range(NCORES):
        sl = slice(c * OWN, (c + 1) * OWN)
        cs = np.concatenate([cos[sl], sin[sl]], axis=-1).reshape(16, 128, 64).transpose(1, 0, 2)
        maps.append({
            "xin": np.ascontiguousarray(np.concatenate([x[sl], ctx], axis=0)),
            "scT": scT, "wmod": inp["w_mod"][l], "bmod2": bmod2, "nmix": nmix, "win": win, "gains": gains,
            "cs": np.ascontiguousarray(cs), "sel": sel,
        })
    return maps


class _Cnt:
    def __init__(self):
        self.d = {}

    def nxt(self, k, n):
        v = self.d.get(k, 0)
        self.d[k] = v + 1
        return v % n


def build_post(kind):
    ab = kind == "ab"
    nc = bass.Bass("TRN2", target_bir_lowering=False)

    def din(name, shape, dt=F32):
        return nc.dram_tensor(name, shape, dt, kind="ExternalInput").ap()

    xin = din("xin", [TOK, D])
    modi = din("modi", [2, 6 * D])
    sel = din("sel", [2, 2, 128])
    wout = din("wout", [D, D])
    nffn = din("nffn", [128, D])
    wr = din("wr", [D, 36])
    br = din("br", [128, 36])
    w1 = din("w1", [32, D, 512])
    w3 = din("w3", [32, D, 512])
    w2 = din("w2", [32, 512, D])
    tri128 = din("tri128", [128, 128], BF16)
    tri32 = din("tri32", [32, 32], BF16)
    thr = din("thr", [128, 36])
    biota = din("biota", [128, NBLK])
    piota = din("piota", [128, 1])
    if ab:
        qta = din("qta", [128, 2, NT, 512], BF16)
        qtb = din("qtb", [128, 2, NT, 512], BF16)
        kta = din("kta", [128, 2560], BF16)
        va = din("va", [128, 20, 2, 65], BF16)
        ktb = din("ktb", [128, NKT * 128], BF16)
        vb = din("vb", [128, NKT, 2, 65], BF16)
        sink = din("sink", [1, 8])
        flags = din("flags", [128, 2])
        mlo = din("mlo", [128, 512], BF16)
        mhi = din("mhi", [128, 512], BF16)
    else:
        qtc = din("qtc", [2, 128, 8, TOK], BF16)
        ktc = din("ktc", [8, 128, NKT * 128], BF16)
        vc = din("vc", [8, 128, NKT, 128], BF16)
        lamp = din("lamp", [128, 4, 64])
        laminit = din("laminit", [128, 1])
        oml = din("oml", [128, 1])
        subln = din("subln", [128, 1])
    xo = nc.dram_tensor("xo", [TOK, D], F32, kind="ExternalOutput").ap()
    x1s = nc.dram_tensor("x1s", [TOK, D], F32, kind="Internal").ap()
    buf = nc.dram_tensor("buf", [NBLK * 128, D], BF16, kind="Internal").ap()
    ybuf = nc.dram_tensor("ybuf", [NBLK * 128, D], F32, kind="Internal").ap()
    h2s = nc.dram_tensor("h2s", [TOK, D], BF16, kind="Internal").ap()

    with contextlib.ExitStack() as st:
        def S(name, shape, dt):
            return st.enter_context(nc.sbuf_tensor(name, shape, dt))

        ident = S("ident", [128, 128], BF16)
        identf = S("identf", [128, 128], F32)
        onesf = S("onesf", [128, 128], F32)
        epst = S("epst", [128, 1], F32)

        if True:
            P = Prog(nc)
            P.op("gpsimd", lambda e: e.iota(identf[:], pattern=[[1, 128]], base=0, channel_multiplier=-1,
                                             allow_small_or_imprecise_dtypes=True), (), ["idf0"])
            TS(P, "vector", ident[:], identf[:], 0.0, ALU.is_equal, ["idf0"], ["ident"])
            TS(P, "vector", identf[:], identf[:], 0.0, ALU.is_equal, ["idf0", "ident"], ["idf0"])
            MSET(P, "vector", epst[:], EPS, ["eps"])
            MSET(P, "vector", onesf[:], 1.0, ["onesf"])
            P.emit()

        sM = contextlib.ExitStack()
        M0b = sM.enter_context(nc.sbuf_tensor("M0b", [128, NT, 32], BF16))
        M1b = sM.enter_context(nc.sbuf_tensor("M1b", [128, NT, 32], BF16))
        wts = sM.enter_context(nc.sbuf_tensor("wts", [128, NT, 2], F32))
        desti = sM.enter_context(nc.sbuf_tensor("desti", [128, NT, 2], I32))
        idxA = sM.enter_context(nc.sbuf_tensor("idxA", [128, NBLK], I32))
        idxB = sM.enter_context(nc.sbuf_tensor("idxB", [128, NBLK], I32))
        sA = contextlib.ExitStack()
        if ab:
            oT = sA.enter_context(nc.sbuf_tensor("oT", [64, 16, TOK], BF16))
        else:
            oT = sA.enter_context(nc.sbuf_tensor("oT", [128, 8, TOK], BF16))

        with contextlib.ExitStack() as s1:
            def S1(name, shape, dt):
                return s1.enter_context(nc.sbuf_tensor(name, shape, dt))

            def PS1(name, shape, dt):
                return s1.enter_context(nc.psum_tensor(name, shape, dt))

            P = Prog(nc)
            cnt = _Cnt()
            if ab:
                kta_sb = S1("kta_sb", [128, 2560], BF16)
                va_sb = S1("va_sb", [128, 20, 2, 65], BF16)
                ktb_sb = S1("ktb_sb", [128, NKT * 128], BF16)
                vb_sb = S1("vb_sb", [128, NKT, 2, 65], BF16)
                q_sb = [S1("q_sb%d" % i, [128, 512], BF16) for i in range(2)]
                pt = [S1("pt%d" % i, [128, 512], BF16) for i in range(3)]
                ptm = [S1("ptm%d" % i, [128, 512], BF16) for i in range(2)]
                rrow = S1("rrow", [65, 512], F32)
                bc_sb = S1("bc_sb", [64, 512], F32)
                sink_sb = S1("sink_sb", [1, 8], F32)
                es_sb = S1("es_sb", [1, 8], F32)
                esrow = S1("esrow", [1, 2, 512], F32)
                e64 = S1("e64", [1, 65], F32)
                flags_sb = S1("flags_sb", [128, 2], F32)
                mlo_sb = S1("mlo_sb", [128, 512], BF16)
                mhi_sb = S1("mhi_sb", [128, 512], BF16)
                ps_s = [PS1("ps_s%d" % i, [128, 512], F32) for i in range(3)]
                ps_o = [PS1("ps_o%d" % i, [65, 512], F32) for i in range(2)]
                ps_b = PS1("ps_b", [64, 512], F32)

                DMA(P, "sync", kta_sb[:], kta[:, :], "c_kta", [], ["kta"])
                DMA(P, "sync", ktb_sb[:], ktb[:, :], "c_ktb", [], ["ktb"])
                DMA(P, "sync", vb_sb[:], vb[:, :, :, :], "c_vb", [], ["vb"])
                DMA(P, "sync", va_sb[:], va[:, :, :, :], "c_va", [], ["va"])
                DMA(P, "sync", sink_sb[:], sink[:, :], "c_sink", [], ["sink"])
                DMA(P, "sync", flags_sb[:], flags[:, :], "c_fl", [], ["flags"])
                DMA(P, "sync", mlo_sb[:], mlo[:, :], "c_mlo", [], ["mlo"])
                DMA(P, "sync", mhi_sb[:], mhi[:, :], "c_mhi", [], ["mhi"])
                ACT(P, es_sb[:], sink_sb[:], AF.Exp, ["sink"], ["es"])
                for kvh in range(2):
                    CP(P, "vector", esrow[0:1, kvh, :].rearrange("o (g q) -> o g q", q=128),
                       es_sb[0:1, kvh * 4:(kvh + 1) * 4].unsqueeze(2).broadcast_to([1, 4, 128]), ["es"], ["esrow"])
                MSET(P, "vector", e64[:], 0.0, ["e64"])
                MSET(P, "vector", e64[0:1, 64:65], 1.0, ["e64"])

                units = []
                for kvh in range(2):
                    for t in range(NT):
                        def key(kt, m=None, f=None, kvh=kvh):
                            return (kta_sb[:, kt * 128:(kt + 1) * 128], "kta", va_sb[:, kt, kvh, :], "va", m, f)
                        if t < 16:
                            keys = [key(t, mlo_sb[:], flags_sb[:, 0:1] if t == 0 else None), key(t + 1),
                                    key(t + 2, mhi_sb[:], flags_sb[:, 1:2] if t == 15 else None), key(18), key(19)]
                        else:
                            keys = [key(18), key(19)]
                        units.append((qta[:, kvh, t, :], keys, esrow[0:1, kvh, :],
                                      oT[:, kvh * 4:(kvh + 1) * 4, t * 128:(t + 1) * 128]))
                for kvh in range(2):
                    for t in range(NT):
                        kts = range(NKT) if t < 16 else (NKT - 2, NKT - 1)
                        keys = [(ktb_sb[:, kt * 128:(kt + 1) * 128], "ktb", vb_sb[:, kt, kvh, :], "vb", None, None)
                                for kt in kts]
                        units.append((qtb[:, kvh, t, :], keys, None,
                                      oT[:, 8 + kvh * 4:8 + (kvh + 1) * 4, t * 128:(t + 1) * 128]))
                flat = [(ui, i) for ui, u in enumerate(units) for i in range(len(u[1]))]
                LA = 2

                def issue_S(j):
                    ui, i = flat[j]
                    q_src, keys, _, _ = units[ui]
                    qb = ui % 2
                    QN = "q%d" % qb
                    if i == 0:
                        DMA(P, "sync", q_sb[qb][:], q_src, "c_q%d" % qb, [], [QN])
                    si = j % 3
                    MM(P, ps_s[si][:], keys[i][0], q_sb[qb][:], True, True, [QN, keys[i][1]], ["ps_s%d" % si])

                def issue_rest(j):
                    ui, i = flat[j]
                    q_src, keys, sink_rhs, out_ap = units[ui]
                    kT_ap, kname, v_ap, vname, mask_ap, flag_ap = keys[i]
                    n = len(keys)
                    si = j % 3
                    oi = ui % 2
                    ON = "ps_o%d" % oi
                    ACT(P, pt[si][:], ps_s[si][:], AF.Exp, ["ps_s%d" % si], ["pt%d" % si], scale=0.125)
                    rhs, rn = pt[si][:], "pt%d" % si
                    if mask_ap is not None:
                        mi = cnt.nxt("m", 2)
                        if flag_ap is not None:
                            STT(P, ptm[mi][:], pt[si][:], flag_ap, mask_ap, ALU.mult, ALU.mult,
                                [rn, "flags", "mlo", "mhi"], ["ptm%d" % mi])
                        else:
                            TT(P, "vector", ptm[mi][:], pt[si][:], mask_ap, ALU.mult, [rn, "mlo", "mhi"],
                               ["ptm%d" % mi])
                        rhs, rn = ptm[mi][:], "ptm%d" % mi
                    MM(P, ps_o[oi][:], v_ap, rhs, i == 0, (i == n - 1) and sink_rhs is None, [rn, vname], [ON])
                    if i == n - 1:
                        if sink_rhs is not None:
                            MM(P, ps_o[oi][:], e64[0:1, :], sink_rhs, False, True, ["e64", "esrow"], [ON])
                        RCP(P, rrow[64:65, :], ps_o[oi][64:65, :], [ON], ["rrow"])
                        MM(P, ps_b[:], onesf[64:65, 0:64], rrow[64:65, :], True, True, ["rrow", "onesf"], ["ps_b"])
                        CP(P, "scalar", bc_sb[:], ps_b[:], ["ps_b"], ["bc"])
                        TT(P, "vector", out_ap, ps_o[oi][0:64, :].rearrange("d (g q) -> d g q", q=128),
                           bc_sb[:].rearrange("d (g q) -> d g q", q=128), ALU.mult, [ON, "bc"], ["oT"])

                for j in range(min(LA, len(flat))):
                    issue_S(j)
                for j in range(len(flat)):
                    if j + LA < len(flat):
                        issue_S(j + LA)
                    issue_rest(j)
            else:
                kt_sbs = [S1("kt_sb%d" % i, [128, NKT * 128], BF16) for i in range(2)]
                v_sbs = [S1("v_sb%d" % i, [128, NKT, 128], BF16) for i in range(2)]
                q1_sb = [S1("q1_sb%d" % i, [128, 512], BF16) for i in range(2)]
                q2_sb = [S1("q2_sb%d" % i, [128, 512], BF16) for i in range(2)]
                p12 = [S1("p12_%d" % i, [128, 1024], BF16) for i in range(3)]
                p1 = [t_[:, 0:512] for t_ in p12]
                p2 = [t_[:, 512:1024] for t_ in p12]
                E0 = S1("E0", [128, 128], F32)
                E1 = S1("E1", [128, 128], BF16)
                acc1 = S1("acc1", [128, 512], F32)
                acc2 = S1("acc2", [128, 512], F32)
                rr = S1("rr", [33, 512], F32)
                b1 = S1("b1", [128, 512], F32)
                d1 = S1("d1", [128, 512], F32)
                d2 = S1("d2", [128, 512], F32)
                Os = S1("Os", [128, 512], F32)
                sqo = S1("sqo", [128, 512], F32)
                rs = S1("rs", [128, 512], F32)
                rs2 = S1("rs2", [128, 512], F32)
                lamp_sb = S1("lamp_sb", [128, 4, 64], F32)
                lp = S1("lp", [128, 2, 64], F32)
                s2 = S1("s2", [128, 2], F32)
                e2 = S1("e2", [128, 2], F32)
                lamv = S1("lamv", [128, 1], F32)
                nlam = S1("nlam", [128, 1], F32)
                li_sb = S1("li_sb", [128, 1], F32)
                oml_sb = S1("oml_sb", [128, 1], F32)
                sub_sb = S1("sub_sb", [128, 1], F32)
                subs = S1("subs", [128, 1], F32)
                ps_s12 = [PS1("ps_s12_%d" % i, [128, 1024], F32) for i in range(2)]
                ps_s1 = [t_[:, 0:512] for t_ in ps_s12]
                ps_s2 = [t_[:, 512:1024] for t_ in ps_s12]
                ps_o1 = PS1("ps_o1", [128, 512], F32)
                ps_o2 = PS1("ps_o2", [128, 512], F32)
                ps_sum = PS1("ps_sum", [128, 512], F32)
                ps_x = PS1("ps_x", [128, 512], F32)

                DMA(P, "sync", lamp_sb[:], lamp[:, :, :], "c_lamp", [], ["lamp"])
                DMA(P, "sync", li_sb[:], laminit[:, :], "c_li", [], ["li"])
                DMA(P, "sync", oml_sb[:], oml[:, :], "c_oml", [], ["oml"])
                DMA(P, "sync", sub_sb[:], subln[:, :], "c_sub", [], ["sub"])
                TT(P, "vector", lp[:, 0, :], lamp_sb[:, 0, :], lamp_sb[:, 1, :], ALU.mult, ["lamp"], ["lp"])
                TT(P, "vector", lp[:, 1, :], lamp_sb[:, 2, :], lamp_sb[:, 3, :], ALU.mult, ["lamp"], ["lp"])
                RED(P, s2[:], lp[:], ALU.add, ["lp"], ["s2"])
                ACT(P, e2[:], s2[:], AF.Exp, ["s2"], ["e2"])
                TT(P, "vector", lamv[:], e2[:, 1:2], e2[:, 0:1], ALU.subtract, ["e2"], ["lamv"])
                TT(P, "vector", nlam[:], lamv[:], li_sb[:], ALU.subtract, ["lamv", "li"], ["nlam"])
                TT(P, "vector", subs[:], sub_sb[:], oml_sb[:], ALU.mult, ["sub", "oml"], ["subs"])
                MSET(P, "vector", E0[:], 0.0, ["E0"])
                MSET(P, "vector", E0[:, 0:1], 1.0, ["E0"])
                MSET(P, "vector", E1[:], 0.0, ["E1"])
                MSET(P, "vector", E1[:, 32:33], 1.0, ["E1"])
                items = []
                for h in range(8):
                    for u in range(5):
                        tok0, N = (u * 512, 512) if u < 4 else (OWN, CTX)
                        kts = list(range(NKT)) if u < 4 else [NKT - 2, NKT - 1]
                        for i, kt in enumerate(kts):
                            items.append((h, u, tok0, N, kt, i, len(kts)))

                def kv_load(h):
                    DMA(P, "sync", kt_sbs[h % 2][:], ktc[h, :, :], "c_kt%d" % (h % 2), [], ["kt%d" % (h % 2)])
                    DMA(P, "sync", v_sbs[h % 2][:], vc[h, :, :, :], "c_v%d" % (h % 2), [], ["v%d" % (h % 2)])

                def issue_S(j):
                    h, u, tok0, N, kt, i, n = items[j]
                    qb = (h * 5 + u) % 2
                    QN = "q%d" % qb
                    if i == 0:
                        if u == 0 and h == 0:
                            kv_load(0)
                            kv_load(1)
                        DMA(P, "sync", q1_sb[qb][:, 0:N], qtc[0, :, h, tok0:tok0 + N], "c_q%d" % qb, [], [QN])
                        DMA(P, "sync", q2_sb[qb][:, 0:N], qtc[1, :, h, tok0:tok0 + N], "c_q%d" % qb, [], [QN])
                    b = j % 2
                    ksl = slice(kt * 128, (kt + 1) * 128)
                    kt_sb = kt_sbs[h % 2]
                    KT = "kt%d" % (h % 2)
                    MM(P, ps_s1[b][:, 0:N], kt_sb[:, ksl], q1_sb[qb][:, 0:N], True, True, [QN, KT], ["ps_s1_%d" % b])
                    MM(P, ps_s2[b][:, 0:N], kt_sb[:, ksl], q2_sb[qb][:, 0:N], True, True, [QN, KT], ["ps_s2_%d" % b])

                def issue_rest(j):
                    h, u, tok0, N, kt, i, n = items[j]
                    b = j % 2
                    v_sb = v_sbs[h % 2]
                    VN = "v%d" % (h % 2)
                    pb = j % 3
                    if N == 512:
                        ACT(P, p12[pb][:], ps_s12[b][:], AF.Exp, ["ps_s1_%d" % b, "ps_s2_%d" % b],
                            ["p1_%d" % pb, "p2_%d" % pb], scale=0.125)
                    else:
                        ACT(P, p1[pb][:, 0:N], ps_s1[b][:, 0:N], AF.Exp, ["ps_s1_%d" % b], ["p1_%d" % pb], scale=0.125)
                        ACT(P, p2[pb][:, 0:N], ps_s2[b][:, 0:N], AF.Exp, ["ps_s2_%d" % b], ["p2_%d" % pb], scale=0.125)
                    MM(P, ps_o2[:, 0:N], v_sb[:, kt, :], p2[pb][:, 0:N], i == 0, i == n - 1, [VN, "p2_%d" % pb], ["ps_o2"])
                    MM(P, ps_o1[:, 0:N], v_sb[:, kt, :], p1[pb][:, 0:N], i == 0, i == n - 1, [VN, "p1_%d" % pb], ["ps_o1"])
                    MM(P, ps_sum[:, 0:N], E1[:], p2[pb][:, 0:N], i == 0, False, ["E1", "p2_%d" % pb], ["ps_sum"])
                    if i == 0:
                        CP(P, "vector", acc1[:, 0:N], p1[pb][:, 0:N], ["p1_%d" % pb], ["acc1"])
                    else:
                        TT(P, "vector", acc1[:, 0:N], acc1[:, 0:N], p1[pb][:, 0:N], ALU.add, ["p1_%d" % pb, "acc1"], ["acc1"])
                    if i != n - 1:
                        return
                    MM(P, ps_sum[:, 0:N], E0[:], acc1[:, 0:N], False, True, ["E0", "acc1"], ["ps_sum"])
                    if u == 4 and 1 <= h + 1 < 7:
                        kv_load(h + 2)
                    RCP(P, rr[0:1, 0:N], ps_sum[0:1, 0:N], ["ps_sum"], ["rr"])
                    RCP(P, rr[32:33, 0:N], ps_sum[32:33, 0:N], ["ps_sum"], ["rr"])
                    TS(P, "vector", rr[32:33, 0:N], rr[32:33, 0:N], nlam[32:33, 0:1], ALU.mult, ["rr", "nlam"], ["rr"])
                    MM(P, ps_x[:, 0:N], onesf[0:1, :], rr[0:1, 0:N], True, True, ["rr", "onesf"], ["ps_x"])
                    CP(P, "scalar", b1[:, 0:N], ps_x[:, 0:N], ["ps_x"], ["b1"])
                    TT(P, "vector", d1[:, 0:N], ps_o1[:, 0:N], b1[:, 0:N], ALU.mult, ["ps_o1", "b1"], ["d1"])
                    MM(P, ps_x[:, 0:N], onesf[32:33, :], rr[32:33, 0:N], True, True, ["rr", "onesf"], ["ps_x"])
                    CP(P, "scalar", b1[:, 0:N], ps_x[:, 0:N], ["ps_x"], ["b1"])
                    TT(P, "vector", d2[:, 0:N], ps_o2[:, 0:N], b1[:, 0:N], ALU.mult, ["ps_o2", "b1"], ["d2"])
                    TT(P, "gpsimd", Os[:, 0:N], d1[:, 0:N], d2[:, 0:N], ALU.add, ["d1", "d2"], ["Os"])
                    TT(P, "gpsimd", sqo[:, 0:N], Os[:, 0:N], Os[:, 0:N], ALU.mult, ["Os"], ["sqo"])
                    MM(P, ps_x[:, 0:N], onesf[:], sqo[:, 0:N], True, True, ["sqo", "onesf"], ["ps_x"])
                    ACT(P, rs[:, 0:N], ps_x[:, 0:N], AF.Sqrt, ["ps_x"], ["rs"], scale=1.0 / 128, bias=epst[:, 0:1])
                    RCP(P, rs2[:, 0:N], rs[:, 0:N], ["rs"], ["rs2"])
                    STT(P, oT[:, h, tok0:tok0 + N], Os[:, 0:N], subs[:, 0:1], rs2[:, 0:N], ALU.mult, ALU.mult,
                        ["Os", "subs", "rs2"], ["oT"])

                issue_S(0)
                for j in range(len(items)):
                    if j + 1 < len(items):
                        issue_S(j + 1)
                    issue_rest(j)
            P.emit()

        with contextlib.ExitStack() as s2_:
            def S2(name, shape, dt):
                return s2_.enter_context(nc.sbuf_tensor(name, shape, dt))

            def PS2(name, shape, dt):
                return s2_.enter_context(nc.psum_tensor(name, shape, dt))

            KC = 16 if ab else 8
            KP = 64 if ab else 128
            GT1b = S2("GT1b", [128, 2, D], F32)
            SH2b = S2("SH2b", [128, 2, D], F32)
            G2b = S2("G2b", [128, 2, D], F32)
            nffn_sb = S2("nffn_sb", [128, D], F32)
            wout_sb = S2("wout_sb", [KP, KC, D], BF16)
            wr_sb = S2("wr_sb", [128, 8, 36], F32)
            br_sb = S2("br_sb", [128, 36], F32)
            xt = [S2("xt%d" % i, [128, D], F32) for i in range(2)]
            x1 = [S2("x1_%d" % i, [128, D], F32) for i in range(2)]
            junk = S2("junk", [128, D], BF16)
            ss = [S2("ss%d" % i, [128, 1], F32) for i in range(2)]
            rt = [S2("rt%d" % i, [128, 1], F32) for i in range(2)]
            rstd = [S2("rstd%d" % i, [128, 1], F32) for i in range(2)]
            h2f = [S2("h2f%d" % i, [128, D], F32) for i in range(2)]
            h2Ts = [S2("h2T%d" % i, [128, D], F32) for i in range(2)]
            h2bt = [S2("h2bt%d" % i, [128, D], BF16) for i in range(2)]
            lgs = [S2("lg%d" % i, [128, 36], F32) for i in range(2)]
            gmax = S2("gmax", [128, 1], F32)
            ngmax = S2("ngmax", [128, 1], F32)
            gm = S2("gm", [128, 4], F32)
            ge = S2("ge", [128, 4], F32)
            gsum = S2("gsum", [128, 1], F32)
            gp = S2("gp", [128, 1], F32)
            pen = S2("pen", [128, 4], F32)
            lem = S2("lem", [128, 32], F32)
            m8 = S2("m8", [128, 8], F32)
            dm = S2("dm", [128, 1], F32)
            rr_ = S2("rr_", [128, 1], F32)
            den = S2("den", [128, 1], F32)
            rden = S2("rden", [128, 1], F32)
            pq = [PS2("pq%d" % i, [128, 512], F32) for i in range(2)]
            pTf = PS2("pTf", [128, D], F32)
            ps_lg = PS2("ps_lg", [128, 36], F32)

            P = Prog(nc)
            mod_sb, sel_sb, psb = load_mod_bcast(nc, P, s2_, modi, sel, 2048, 3072, "o")
            DMA(P, "sync", nffn_sb[:], nffn[:, :], "c_nf", [], ["nffn"])
            for r in range(2):
                mod_broadcast(P, GT1b[:, r, :], psb, sel_sb, mod_sb, r, 0, None, [], "GT1b", 0)
                mod_broadcast(P, SH2b[:, r, :], psb, sel_sb, mod_sb, r, 1024, None, [], "SH2b", 0)
                mod_broadcast(P, G2b[:, r, :], psb, sel_sb, mod_sb, r, 2048, nffn_sb, ["nffn"], "G2b", 0)
            wv = wout.rearrange("(c p) n -> p c n", p=KP)
            for c in range(KC):
                DMA(P, "gpsimd", wout_sb[:, c, :], wv[:, c, :], "c_wo", [], ["wout%d" % c])
            DMA(P, "sync", wr_sb[:], wr.rearrange("(c p) n -> p c n", p=128), "c_wr", [], ["wr"])
            DMA(P, "sync", br_sb[:], br[:, :], "c_br", [], ["br"])
            def stage1(t):
                b = t % 2
                r = 0 if t < 16 else 1
                X, X1, H2 = "x%d" % b, "x1_%d" % b, "h2f%d" % b
                if t == 0:
                    DMA(P, "sync", xt[0][:], xin[0:128, :], "c_x0", [], ["x0"])
                if t + 1 < NT:
                    nb_ = (t + 1) % 2
                    DMA(P, "sync", xt[nb_][:], xin[(t + 1) * 128:(t + 2) * 128, :], "c_x%d" % nb_, [], ["x%d" % nb_])
                for half in range(2):
                    hs = slice(half * 512, (half + 1) * 512)
                    for c in range(KC):
                        MM(P, pq[half][:], oT[:, c, t * 128:(t + 1) * 128], wout_sb[:, c, hs], c == 0, c == KC - 1,
                           ["wout%d" % c], ["pq%d" % half])
                    TT(P, "vector", x1[b][:, hs], pq[half][:], GT1b[:, r, hs], ALU.mult, ["pq%d" % half, "GT1b"], [X1])
                TT(P, "gpsimd", x1[b][:], x1[b][:], xt[b][:], ALU.add, [X1, X], [X1])
                DMA(P, "sync", x1s[t * 128:(t + 1) * 128, :], x1[b][:], "c_x1s%d" % b, [X1], ["x1s%d" % t])
                rms_rstd(P, x1[b][:], junk[:], ss[b], rt[b], rstd[b], epst, D, [X1], str(b))
                STT(P, h2f[b][:], x1[b][:], rstd[b][:, 0:1], G2b[:, r, :], ALU.mult, ALU.mult, [X1, "rstd%d" % b, "G2b"], [H2])
                TT(P, "gpsimd", h2f[b][:], h2f[b][:], SH2b[:, r, :], ALU.add, [H2, "SH2b"], [H2])
                CP(P, "scalar", h2bt[b][:], h2f[b][:], [H2], ["h2bt%d" % b])
                DMA(P, "sync", h2s[t * 128:(t + 1) * 128, :], h2bt[b][:], "c_h2s%d" % b, ["h2bt%d" % b], ["h2s%d" % t])
                for c in range(8):
                    TR(P, pTf[:, c * 128:(c + 1) * 128], h2f[b][:, c * 128:(c + 1) * 128], identf[:], [H2], ["pTf"])
                h2T = h2Ts[b]
                CP(P, "vector", h2T[:], pTf[:], ["pTf"], ["h2T%d" % b])
                for c in range(8):
                    MM(P, ps_lg[:], h2T[:, c * 128:(c + 1) * 128], wr_sb[:, c, :], c == 0, c == 7, ["h2T%d" % b, "wr"], ["ps_lg"])
                TT(P, "vector", lgs[b][:], ps_lg[:], br_sb[:], ALU.add, ["ps_lg", "br"], ["lg%d" % b])

            def stage2(t):
                b = t % 2
                lg = lgs[b]
                RED(P, gmax[:], lg[:, 0:4], ALU.max, ["lg%d" % b], ["gmax"])
                TS(P, "vector", gm[:], lg[:, 0:4], gmax[:, 0:1], ALU.is_equal, ["lg%d" % b, "gmax"], ["gm"])
                TS(P, "vector", ngmax[:], gmax[:], -1.0, ALU.mult, ["gmax"], ["ngmax"])
                ACT(P, ge[:], lg[:, 0:4], AF.Exp, ["lg%d" % b, "ngmax"], ["ge", "gsum"], bias=ngmax[:, 0:1], accum_out=gsum[:, 0:1])
                RCP(P, gp[:], gsum[:], ["gsum"], ["gp"])
                TS(P, "vector", pen[:], gm[:], 1e30, ALU.mult, ["gm"], ["pen"], s2=-1e30, op1=ALU.add)
                TT(P, "vector", lem[:].rearrange("p (g e) -> p g e", e=8), lg[:, 4:36].rearrange("p (g e) -> p g e", e=8),
                   pen[:].unsqueeze(2).broadcast_to([128, 4, 8]), ALU.add, ["lg%d" % b, "pen"], ["lem"])
                P.op("vector", lambda e: e.max(out=m8[:], in_=lem[:]), ["lem"], ["m8"])
                TS(P, "vector", M0b[:, t, :], lem[:], m8[:, 0:1], ALU.is_equal, ["lem", "m8"], ["M0b"])
                TS(P, "vector", M1b[:, t, :], lem[:], m8[:, 1:2], ALU.is_equal, ["lem", "m8"], ["M1b"])
                TT(P, "vector", dm[:], m8[:, 1:2], m8[:, 0:1], ALU.subtract, ["m8"], ["dm"])
                ACT(P, rr_[:], dm[:], AF.Exp, ["dm"], ["rr_"])
                TS(P, "vector", den[:], rr_[:], 1.0, ALU.add, ["rr_"], ["den"])
                RCP(P, rden[:], den[:], ["den"], ["rden"])
                TT(P, "vector", wts[:, t, 0:1], rden[:], gp[:], ALU.mult, ["rden", "gp"], ["wts"])
                TT(P, "vector", wts[:, t, 1:2], wts[:, t, 0:1], rr_[:], ALU.mult, ["wts", "rr_"], ["wts"])

            stage1(0)
            for t in range(NT):
                if t + 1 < NT:
                    stage1(t + 1)
                stage2(t)
            P.emit()
        with contextlib.ExitStack() as s3:
            def S3(name, shape, dt):
                return s3.enter_context(nc.sbuf_tensor(name, shape, dt))

            def PS3(name, shape, dt):
                return s3.enter_context(nc.psum_tensor(name, shape, dt))

            tri_sb = S3("tri_sb", [128, 128], BF16)
            tri32_sb = S3("tri32_sb", [32, 32], BF16)
            ones_b = S3("ones_b", [128, 128], BF16)
            thr_sb = S3("thr_sb", [128, 36], F32)
            bio_sb = S3("bio_sb", [128, NBLK], F32)
            pio_sb = S3("pio_sb", [128, 1], F32)
            Ms = S3("Ms", [128, NT, 32], BF16)
            Cs = S3("Cs", [128, NT, 32], F32)
            cntf = S3("cntf", [128, 32], F32)
            cmp1 = S3("cmp1", [128, 32, 36], F32)
            nblk = S3("nblk", [128, 32], F32)
            nblkb = S3("nblkb", [128, 32], BF16)
            nbT = S3("nbT", [32, 128], BF16)
            Sx = S3("Sx", [128, 32], F32)
            pend = S3("pend", [128, 32], F32)
            base = S3("base", [128, 32], F32)
            tall = S3("tall", [128, NT, 32], F32)
            prod = S3("prod", [128, NT, 32], F32)
            destf = S3("destf", [128, NT, 2], F32)
            cmp2 = S3("cmp2", [128, NBLK, 32], F32)
            be = S3("be", [128, NBLK], F32)
            idxf = S3("idxf", [128, NBLK], F32)
            ps_c = [PS3("ps_c%d" % i, [128, 32], F32) for i in range(2)]
            ps_t = PS3("ps_t", [32, 128], BF16)
            ps_S = PS3("ps_S", [128, 32], F32)

            P = Prog(nc)
            DMA(P, "sync", tri_sb[:], tri128[:, :], "c_tri", [], ["tri"])
            DMA(P, "sync", tri32_sb[:], tri32[:, :], "c_tri32", [], ["tri32"])
            DMA(P, "sync", thr_sb[:], thr[:, :], "c_thr", [], ["thr"])
            DMA(P, "sync", bio_sb[:], biota[:, :], "c_bio", [], ["bio"])
            DMA(P, "sync", pio_sb[:], piota[:, :], "c_pio", [], ["pio"])
            MSET(P, "vector", ones_b[:], 1.0, ["ones_b"])
            TT(P, "vector", Ms[:], M0b[:], M1b[:], ALU.add, [], ["Ms"])
            for t in range(NT):
                pc = ps_c[t % 2]
                pn = "ps_c%d" % (t % 2)
                MM(P, pc[:], tri_sb[:], Ms[:, t, :], True, t == 0, ["tri", "Ms"], [pn])
                for i in range(t):
                    MM(P, pc[:], ones_b[:], Ms[:, i, :], False, i == t - 1, ["ones_b", "Ms"], [pn])
                CP(P, "vector", Cs[:, t, :], pc[:], [pn], ["Cs"])
            pc = ps_c[NT % 2]
            pn = "ps_c%d" % (NT % 2)
            for i in range(NT):
                MM(P, pc[:], ones_b[:], Ms[:, i, :], i == 0, i == NT - 1, ["ones_b", "Ms"], [pn])
            CP(P, "vector", cntf[:], pc[:], [pn], ["cntf"])
            TT(P, "vector", cmp1[:], cntf[:].unsqueeze(2).broadcast_to([128, 32, 36]),
               thr_sb[:].unsqueeze(1).broadcast_to([128, 32, 36]), ALU.is_gt, ["cntf", "thr"], ["cmp1"])
            RED(P, nblk[:], cmp1[:], ALU.add, ["cmp1"], ["nblk"])
            CP(P, "vector", nblkb[:], nblk[:], ["nblk"], ["nblkb"])
            TR(P, ps_t[:], nblkb[:], ident[:], ["nblkb"], ["ps_t"])
            CP(P, "vector", nbT[:], ps_t[:], ["ps_t"], ["nbT"])
            MM(P, ps_S[:], nbT[:], tri32_sb[:], True, True, ["nbT", "tri32"], ["ps_S"])
            CP(P, "vector", Sx[:], ps_S[:], ["ps_S"], ["Sx"])
            TT(P, "vector", pend[:], Sx[:], nblk[:], ALU.add, ["Sx", "nblk"], ["pend"])
            TS(P, "vector", base[:], Sx[:], 128.0, ALU.mult, ["Sx"], ["base"])
            TT(P, "vector", tall[:], Cs[:], base[:].unsqueeze(1).broadcast_to([128, NT, 32]), ALU.add, ["Cs", "base"], ["tall"])
            TT(P, "vector", prod[:], tall[:], M0b[:], ALU.mult, ["tall"], ["prod"])
            RED(P, destf[:, :, 0], prod[:], ALU.add, ["prod"], ["destf"])
            TT(P, "vector", prod[:], tall[:], M1b[:], ALU.mult, ["tall"], ["prod"])
            RED(P, destf[:, :, 1], prod[:], ALU.add, ["prod"], ["destf"])
            CP(P, "vector", desti[:], destf[:], ["destf"], ["desti"])
            TT(P, "vector", cmp2[:], pend[:].unsqueeze(1).broadcast_to([128, NBLK, 32]),
               bio_sb[:].unsqueeze(2).broadcast_to([128, NBLK, 32]), ALU.is_le, ["pend", "bio"], ["cmp2"])
            RED(P, be[:], cmp2[:], ALU.add, ["cmp2"], ["be"])
            TS(P, "vector", be[:], be[:], 31.0, ALU.min, ["be"], ["be"])
            TS(P, "vector", idxf[:], be[:], 256.0, ALU.mult, ["be", "pio"], ["idxf"], s2=pio_sb[:, 0:1], op1=ALU.add)
            CP(P, "vector", idxA[:], idxf[:], ["idxf"], ["idxA"])
            TS(P, "vector", idxf[:], idxf[:], 1.0, ALU.add, ["idxf", "idxA"], ["idxf"])
            CP(P, "vector", idxB[:], idxf[:], ["idxf"], ["idxB"])
            P.emit()

        sA.close()
        with contextlib.ExitStack() as s4:
            def S4(name, shape, dt):
                return s4.enter_context(nc.sbuf_tensor(name, shape, dt))

            def PS4(name, shape, dt):
                return s4.enter_context(nc.psum_tensor(name, shape, dt))

            NW = 3
            GT2b = S4("GT2b", [128, 2, D], F32)
            W1b = [S4("W1b%d" % i, [128, 4096], BF16) for i in range(NW)]
            W3b = [S4("W3b%d" % i, [128, 4096], BF16) for i in range(NW)]
            W2b = [S4("W2b%d" % i, [128, 4096], BF16) for i in range(NW)]
            xb = [S4("xb%d" % i, [128, D], BF16) for i in range(2)]
            xbT = [S4("xbT%d" % i, [128, 8, 128], BF16) for i in range(2)]
            s1t = S4("s1t", [128, 512], F32)
            gT = [S4("gT%d" % i, [128, 4, 128], BF16) for i in range(2)]
            ysb = [S4("ysb%d" % i, [128, D], F32) for i in range(2)]
            y0 = [S4("y0_%d" % i, [128, D], F32) for i in range(2)]
            y1 = [S4("y1_%d" % i, [128, D], F32) for i in range(2)]
            xc = [S4("xc%d" % i, [128, D], F32) for i in range(2)]
            f0 = S4("f0", [128, D], F32)
            f1 = S4("f1", [128, D], F32)
            f2 = f0
            xo_sb = [S4("xo_sb%d" % i, [128, D], F32) for i in range(2)]
            pxT = PS4("pxT", [128, 8, 128], BF16)
            ps1 = [PS4("ps1_%d" % i, [128, 4, 128], F32) for i in range(2)]
            ps3 = [PS4("ps3_%d" % i, [128, 4, 128], F32) for i in range(2)]
            psy = [PS4("psy%d" % i, [128, 512], F32) for i in range(2)]

            P = Prog(nc)
            for t in range(NT):
                b = t % 2
                DMA(P, "sync", xb[b][:], h2s[t * 128:(t + 1) * 128, :], "c_xb%d" % b, [], ["xb%d" % b])
                for k in range(2):
                    P.dma("gpsimd", (lambda t, k, b: lambda e: e.indirect_dma_start(
                        out=buf[:, :], out_offset=bass.IndirectOffsetOnAxis(ap=desti[:, t, k:k + 1], axis=0),
                        in_=xb[b][:, :], in_offset=None))(t, k, b), "c_sc%d" % b, ["xb%d" % b], ["buf_%d_%d" % (t, k)])
            w1v = w1.rearrange("e (p h c) f -> (e p h) (c f)", h=2, c=4)
            w3v = w3.rearrange("e (p h c) f -> (e p h) (c f)", h=2, c=4)
            w2v = w2.rearrange("e (p h c) n -> (e p h) (c n)", h=2, c=2)
            mod_sb, sel_sb, psb = load_mod_bcast(nc, P, s4, modi, sel, 5120, 1024, "m", psb=psy)
            for r in range(2):
                mod_broadcast(P, GT2b[:, r, :], psb, sel_sb, mod_sb, r, 0, None, [], "GT2b", 0, pname="psy")

            def stage_w(blk):
                wb = blk % NW
                for (wv_, Wt, nm) in ((w1v, W1b, "W1"), (w3v, W3b, "W3"), (w2v, W2b, "W2")):
                    for hh, idx in ((0, idxA), (1, idxB)):
                        P.dma("gpsimd", (lambda wv_, Wt, hh, idx, blk, wb: lambda e: e.indirect_dma_start(
                            out=Wt[wb][:, hh * 2048:(hh + 1) * 2048], out_offset=None, in_=wv_[:, :],
                            in_offset=bass.IndirectOffsetOnAxis(ap=idx[:, blk:blk + 1], axis=0)))(wv_, Wt, hh, idx, blk, wb),
                            "c_%s%d_%d" % (nm, wb, hh), [], ["%s%d_%d" % (nm, wb, hh)])

            def stage_a(blk):
                b = blk % 2
                wb = blk % NW
                DMA(P, "sync", xb[b][:], buf[blk * 128:(blk + 1) * 128, :], "c_xb%d" % b,
                    ["buf_%d_%d" % (t_, k_) for t_ in range(NT) for k_ in range(2)], ["xb%d" % b])
                xv = xb[b][:].rearrange("s (p c) -> s c p", c=8)
                for c in range(8):
                    TR(P, pxT[:, c, :], xv[:, c, :], ident[:], ["xb%d" % b], ["pxT"])
                CP(P, "vector", xbT[b][:], pxT[:], ["pxT"], ["xbT%d" % b])
                W1v = W1b[wb][:].rearrange("p (cc j q) -> p cc j q", cc=8, q=4)
                W3v = W3b[wb][:].rearrange("p (cc j q) -> p cc j q", cc=8, q=4)
                for cq in range(4):
                    for c in range(8):
                        MM(P, ps1[b][:, cq, :], W1v[:, c, :, cq], xbT[b][:, c, :], c == 0, c == 7,
                           ["W1%d_0" % wb, "W1%d_1" % wb, "xbT%d" % b], ["ps1_%d" % b])
                for cq in range(4):
                    for c in range(8):
                        MM(P, ps3[b][:, cq, :], W3v[:, c, :, cq], xbT[b][:, c, :], c == 0, c == 7,
                           ["W3%d_0" % wb, "W3%d_1" % wb, "xbT%d" % b], ["ps3_%d" % b])

            def stage_b(blk):
                b = blk % 2
                wb = blk % NW
                ACT(P, s1t[:], ps1[b][:].rearrange("p a s -> p (a s)"), AF.Silu, ["ps1_%d" % b], ["s1t"])
                TT(P, "vector", gT[b][:].rearrange("p a s -> p (a s)"), s1t[:], ps3[b][:].rearrange("p a s -> p (a s)"),
                   ALU.mult, ["s1t", "ps3_%d" % b], ["gT%d" % b])
                for half in range(2):
                    for cq in range(4):
                        MM(P, psy[half][:], gT[b][:, cq, :], W2b[wb][:, cq * 1024 + half * 512: cq * 1024 + (half + 1) * 512],
                           cq == 0, cq == 3, ["gT%d" % b, "W2%d_0" % wb, "W2%d_1" % wb], ["psy%d" % half])
                    CP(P, "scalar" if half == 0 else "vector", ysb[b][:, half * 512:(half + 1) * 512], psy[half][:],
                       ["psy%d" % half], ["ysb%d" % b])
                DMA(P, "sync", ybuf[blk * 128:(blk + 1) * 128, :], ysb[b][:], "c_yb%d" % b, ["ysb%d" % b], ["ybuf%d" % blk])

            stage_w(0)
            stage_w(1)
            stage_a(0)
            for blk in range(NBLK):
                if blk + 2 < NBLK:
                    stage_w(blk + 2)
                if blk + 1 < NBLK:
                    stage_a(blk + 1)
                stage_b(blk)
            for t in range(NT):
                b = t % 2
                r = 0 if t < 16 else 1
                for k, yt in ((0, y0), (1, y1)):
                    P.dma("gpsimd", (lambda t, k, yt, b: lambda e: e.indirect_dma_start(
                        out=yt[b][:, :], out_offset=None, in_=ybuf[:, :],
                        in_offset=bass.IndirectOffsetOnAxis(ap=desti[:, t, k:k + 1], axis=0)))(t, k, yt, b),
                        "c_y%d_%d" % (k, b), ["ybuf%d" % q_ for q_ in range(NBLK)], ["y%d_%d" % (k, b)])
                DMA(P, "sync", xc[b][:], x1s[t * 128:(t + 1) * 128, :], "c_xc%d" % b, [], ["xc%d" % b])
                TS(P, "vector", f0[:], y0[b][:], wts[:, t, 0:1], ALU.mult, ["y0_%d" % b], ["f0"])
                STT(P, f1[:], y1[b][:], wts[:, t, 1:2], f0[:], ALU.mult, ALU.add, ["y1_%d" % b, "f0"], ["f1"])
                TT(P, "gpsimd", f2[:], f1[:], GT2b[:, r, :], ALU.mult, ["f1", "GT2b"], ["f0"])
                TT(P, "vector", xo_sb[b][:], f2[:], xc[b][:], ALU.add, ["f0", "xc%d" % b], ["xo%d" % b])
                DMA(P, "sync", xo[t * 128:(t + 1) * 128, :], xo_sb[b][:], "c_xo%d" % b, ["xo%d" % b], [])
            P.emit()
        sM.close()
    return nc


def post_consts():
    k = np.arange(128)
    tri128 = (k[:, None] < k[None, :]).astype(NPBF)
    e = np.arange(32)
    tri32 = (e[:, None] < e[None, :]).astype(NPBF)
    thr = np.ascontiguousarray(np.broadcast_to((128.0 * np.arange(36)).astype(np.float32), (128, 36)))
    biota = np.ascontiguousarray(np.broadcast_to(np.arange(NBLK, dtype=np.float32), (128, NBLK)))
    piota = (2.0 * np.arange(128, dtype=np.float32)).reshape(128, 1)
    sel = np.zeros((2, 2, 128), np.float32)
    sel[0, 0] = 1
    sel[1, 1] = 1
    q = np.arange(128)
    mlo = np.tile((k[:, None] >= q[None, :]).astype(NPBF), (1, 4))
    mhi = np.tile((k[:, None] <= q[None, :]).astype(NPBF), (1, 4))
    return dict(tri128=tri128, tri32=tri32, thr=thr, biota=biota, piota=piota, sel=sel), dict(mlo=mlo, mhi=mhi)


def post_inputs(inp, l, x, ctx, qkvs, mod):
    i = l // 2
    kind = "ab" if l % 2 == 0 else "c"
    common, masks = post_consts()
    common.update({
        "modi": np.ascontiguousarray(mod),
        "wout": (inp["w_out_ab"][i] if kind == "ab" else inp["w_out_c"][i]),
        "nffn": np.ascontiguousarray(np.broadcast_to(inp["norm_ffn"][l], (128, D))),
        "wr": np.ascontiguousarray(np.concatenate([inp["w_group"][l], inp["w_expert"][l]], axis=1)),
        "br": np.ascontiguousarray(np.broadcast_to(np.concatenate([inp["b_group"][l], inp["b_expert"][l]]), (128, 36))),
        "w1": inp["w1"][l], "w3": inp["w3"][l], "w2": inp["w2"][l],
    })
    maps = []
    if kind == "ab":
        common.update(masks)
        common["sink"] = np.ascontiguousarray(inp["sink_a"][i].reshape(1, 8))
        lat = [q[:OWN] for q in qkvs]
        cx = qkvs[0][OWN:]
        kb_all = np.concatenate([q_[:, 1152:1280] for q_ in lat] + [cx[:, 1152:1280]], axis=0)
        vb_all = np.concatenate([q_[:, 1408:1536] for q_ in lat] + [cx[:, 1408:1536]], axis=0)
        ktb = np.ascontiguousarray(kb_all.T)
        vb1 = np.concatenate([vb_all.reshape(NKT * 128, 2, 64), np.ones((NKT * 128, 2, 1), NPBF)], axis=-1)
        vb = np.ascontiguousarray(vb1.reshape(NKT, 128, 2, 65).transpose(1, 0, 2, 3))

        def qpad(qcols):
            qt = qcols.reshape(NT, 128, 2, 4, 64).transpose(4, 2, 0, 3, 1).reshape(64, 2, NT, 512)
            out = np.zeros((128, 2, NT, 512), NPBF)
            out[0:64, 0] = qt[:, 0]
            out[64:128, 1] = qt[:, 1]
            return out
        zero = np.zeros((128, 128), NPBF)
        for c in range(NCORES):
            q = qkvs[c]
            qta = qpad(q[:, 0:512])
            qtb = qpad(q[:, 512:1024])

            def halo(cols):
                left = qkvs[c - 1][OWN - 128:OWN, cols] if c > 0 else zero
                right = qkvs[c + 1][0:128, cols] if c < NCORES - 1 else zero
                return np.concatenate([left, q[:OWN, cols], right, cx[:, cols]], axis=0)
            ka = halo(slice(1024, 1152))
            va_ = halo(slice(1280, 1408))
            kta = np.ascontiguousarray(ka.T)
            va1 = np.concatenate([va_.reshape(2560, 2, 64), np.ones((2560, 2, 1), NPBF)], axis=-1)
            va = np.ascontiguousarray(va1.reshape(20, 128, 2, 65).transpose(1, 0, 2, 3))
            flags = np.ones((128, 2), np.float32)
            if c == 0:
                flags[:, 0] = 0
            if c == NCORES - 1:
                flags[:, 1] = 0
            m = dict(common)
            m.update(xin=np.ascontiguousarray(np.concatenate([x[c * OWN:(c + 1) * OWN], ctx], axis=0)),
                     qta=qta, qtb=qtb, kta=kta, va=va, ktb=ktb, vb=vb, flags=flags)
            maps.append(m)
    else:
        lam_init = 0.8 - 0.6 * math.exp(-0.3 * l)
        common["lamp"] = np.ascontiguousarray(np.broadcast_to(inp["lam_c"][i], (128, 4, 64)))
        common["laminit"] = np.full((128, 1), lam_init, np.float32)
        common["oml"] = np.full((128, 1), 1.0 - lam_init, np.float32)
        common["subln"] = np.ascontiguousarray(inp["subln_c"][i].reshape(128, 1))
        lat = [q[:OWN] for q in qkvs]
        cx = qkvs[0][OWN:]
        k_all = np.concatenate([q_[:, 1024:2048] for q_ in lat] + [cx[:, 1024:2048]], axis=0)
        v_all = np.concatenate([q_[:, 2048:3072] for q_ in lat] + [cx[:, 2048:3072]], axis=0)
        ktc = np.ascontiguousarray(k_all.reshape(NKT * 128, 8, 128).transpose(1, 2, 0))
        vc = np.ascontiguousarray(v_all.reshape(NKT, 128, 8, 128).transpose(2, 1, 0, 3))
        for c in range(NCORES):
            q = qkvs[c]
            qt = q[:, 0:1024].reshape(TOK, 8, 128).transpose(2, 1, 0)
            qtc = np.zeros((2, 128, 8, TOK), NPBF)
            qtc[0, 0:64] = qt[0:64]
            qtc[1, 64:128] = qt[64:128]
            m = dict(common)
            m.update(xin=np.ascontiguousarray(np.concatenate([x[c * OWN:(c + 1) * OWN], ctx], axis=0)),
                     qtc=qtc, ktc=ktc, vc=vc)
            maps.append(m)
    return maps


def run_layer(inp, l, x, ctx):
    kind = "ab" if l % 2 == 0 else "c"
    pre = get_prog(("pre", kind), lambda: build_pre(kind))
    res = run_bass_kernel_spmd(pre, pre_inputs(inp, l, x, ctx), core_ids=list(range(NCORES)))
    qkvs = [np.asarray(r["qkv"]) for r in res.results]
    mod = np.asarray(res.results[0]["modo"])
    post = get_prog(("post", kind), lambda: build_post(kind))
    res = run_bass_kernel_spmd(post, post_inputs(inp, l, x, ctx, qkvs, mod), core_ids=list(range(NCORES)))
    xo = [np.asarray(r["xo"]) for r in res.results]
    x_new = np.concatenate([o[:OWN] for o in xo], axis=0)
    ctx_new = xo[0][OWN:]
    return x_new, ctx_new


def kernel(**inputs):
    inp = {k: np.asarray(v) for k, v in inputs.items()}
    x = np.ascontiguousarray(inp["x"][0].astype(np.float32))
    ctx = np.ascontiguousarray(inp["ctx"][0].astype(np.float32))
    for l in range(4):
        x, ctx = run_layer(inp, l, x, ctx)
    return x[None].astype(np.float32)
```

```python
import contextlib
import math
import numpy as np
import ml_dtypes
import concourse.bass as bass
import concourse.mybir as mybir
from concourse.bass_utils import run_bass_kernel_spmd

F32 = mybir.dt.float32
BF16 = mybir.dt.bfloat16
I32 = mybir.dt.int32
AF = mybir.ActivationFunctionType
ALU = mybir.AluOpType
AX = mybir.AxisListType
NPBF = ml_dtypes.bfloat16

ENGS = ("tensor", "vector", "scalar", "gpsimd", "sync")
NCORES = 8
SEQ = 16384
D = 1024
CTX = 256
OWN = SEQ // NCORES
NT = (OWN + CTX) // 128
TOK = NT * 128
NKT = (SEQ + CTX) // 128
NBLK = 2 * NT + 32
EPS = 1e-6


class _Op:
    __slots__ = ("eng", "fn", "deps", "is_dma", "chan", "chan_val", "needs_inc", "inc_val")

    def __init__(self, eng, fn, is_dma=False, chan=None):
        self.eng = eng
        self.fn = fn
        self.deps = []
        self.is_dma = is_dma
        self.chan = chan
        self.chan_val = 0
        self.needs_inc = False
        self.inc_val = 0


class Prog:
    _uid = 0

    def __init__(self, nc):
        self.nc = nc
        self.ops = {e: [] for e in ENGS}
        self.last_write = {}
        self.reads_since = {}
        self.chan_count = {}

    def _add(self, op, reads, writes):
        deps = []
        for r in reads:
            w = self.last_write.get(r)
            if w is not None:
                deps.append(w)
        for r in writes:
            w = self.last_write.get(r)
            if w is not None:
                deps.append(w)
            deps.extend(self.reads_since.get(r, ()))
        seen = set()
        for d in deps:
            if d is op or id(d) in seen:
                continue
            seen.add(id(d))
            if (not d.is_dma) and (not op.is_dma) and d.eng == "tensor" and op.eng == "tensor":
                continue
            op.deps.append(d)
        for r in reads:
            self.reads_since.setdefault(r, []).append(op)
        for r in writes:
            self.last_write[r] = op
            self.reads_since[r] = []
        self.ops[op.eng].append(op)
        return op

    def op(self, eng, fn, reads=(), writes=()):
        return self._add(_Op(eng, fn), reads, writes)

    def dma(self, eng, fn, chan, reads=(), writes=()):
        o = _Op(eng, fn, is_dma=True, chan=chan)
        self.chan_count[chan] = self.chan_count.get(chan, 0) + 16
        o.chan_val = self.chan_count[chan]
        return self._add(o, reads, writes)

    def emit(self):
        nc = self.nc
        for e in ENGS:
            for o in self.ops[e]:
                for d in o.deps:
                    if not d.is_dma:
                        d.needs_inc = True
        for e in ENGS:
            c = 0
            for o in self.ops[e]:
                if (not o.is_dma) and o.needs_inc:
                    c += 1
                    o.inc_val = c
        chans = sorted(self.chan_count.keys(), key=str)
        prog = self
        with contextlib.ExitStack() as st:
            Prog._uid += 1
            u = Prog._uid
            esem = {e: st.enter_context(nc.semaphore("se%d_%s" % (u, e))) for e in ENGS if e != "sync"}
            csem = {c: st.enter_context(nc.semaphore("sc%d_%d" % (u, i))) for i, c in enumerate(chans)}
            block = st.enter_context(nc.Block())

            def make(ename):
                def body(eng):
                    waited = {}
                    for o in prog.ops[ename]:
                        for d in o.deps:
                            if d.is_dma:
                                key, val, sem = ("c", d.chan), d.chan_val, csem[d.chan]
                            else:
                                key, val, sem = ("e", d.eng), d.inc_val, esem[d.eng]
                            if waited.get(key, 0) >= val:
                                continue
                            waited[key] = val
                            eng.wait_ge(sem, val)
                        ins = o.fn(eng)
                        if o.is_dma:
                            ins.then_inc(csem[o.chan], 16)
                        elif o.needs_inc:
                            ins.then_inc(esem[ename], 1)
                    if ename == "sync":
                        for c in chans:
                            eng.wait_ge(csem[c], prog.chan_count[c])
                return body

            for e in ENGS:
                getattr(block, e)(make(e))


def MM(P, out, lhsT, rhs, start, stop, reads, writes):
    P.op("tensor", lambda e: e.matmul(out, lhsT=lhsT, rhs=rhs, start=start, stop=stop), reads, writes)


def TR(P, out, in_, ident, reads, writes):
    P.op("tensor", lambda e: e.transpose(out, in_, ident), reads, writes)


def ACT(P, out, in_, func, reads, writes, scale=None, bias=None, accum_out=None):
    kw = {}
    if scale is not None:
        kw["scale"] = scale
    if bias is not None:
        kw["bias"] = bias
    if accum_out is not None:
        kw["accum_out"] = accum_out
    P.op("scalar", lambda e: e.activation(out=out, in_=in_, func=func, **kw), reads, writes)


def TT(P, eng, out, in0, in1, op, reads, writes):
    P.op(eng, lambda e: e.tensor_tensor(out=out, in0=in0, in1=in1, op=op), reads, writes)


def TS(P, eng, out, in0, s1, op0, reads, writes, s2=None, op1=None):
    if op1 is None:
        P.op(eng, lambda e: e.tensor_scalar(out=out, in0=in0, scalar1=s1, scalar2=None, op0=op0), reads, writes)
    else:
        P.op(eng, lambda e: e.tensor_scalar(out=out, in0=in0, scalar1=s1, scalar2=s2, op0=op0, op1=op1), reads, writes)


def STT(P, out, in0, scalar, in1, op0, op1, reads, writes):
    P.op("vector", lambda e: e.scalar_tensor_tensor(out=out, in0=in0, scalar=scalar, in1=in1, op0=op0, op1=op1),
         reads, writes)


def CP(P, eng, out, in_, reads, writes):
    if eng == "scalar":
        P.op(eng, lambda e: e.copy(out=out, in_=in_), reads, writes)
    else:
        P.op(eng, lambda e: e.tensor_copy(out=out, in_=in_), reads, writes)


def RED(P, out, in_, op, reads, writes):
    P.op("vector", lambda e: e.tensor_reduce(out=out, in_=in_, axis=AX.X, op=op), reads, writes)


def RCP(P, out, in_, reads, writes):
    P.op("vector", lambda e: e.reciprocal(out=out, in_=in_), reads, writes)


def MSET(P, eng, ap, val, writes):
    P.op(eng, lambda e: e.memset(ap, val), (), writes)


def DMA(P, eng, out, in_, chan, reads, writes):
    P.dma(eng, lambda e: e.dma_start(out=out, in_=in_), chan, reads, writes)


def make_ident(P, nc, ident, idf):
    P.op("gpsimd", lambda e: e.iota(idf[:], pattern=[[1, 128]], base=0, channel_multiplier=-1,
                                     allow_small_or_imprecise_dtypes=True), (), ["idf"])
    TS(P, "vector", ident[:], idf[:], 0.0, ALU.is_equal, ["idf"], ["ident"])


def rms_rstd(P, x_ap, junk_ap, ss, rt, rstd, epst, n, rd, tag):
    ACT(P, junk_ap, x_ap, AF.Square, rd, ["junk" + tag, "ss" + tag], accum_out=ss[:, 0:1])
    ACT(P, rt[:, 0:1], ss[:, 0:1], AF.Sqrt, ["ss" + tag], ["rt" + tag], scale=1.0 / n, bias=epst[:, 0:1])
    RCP(P, rstd[:, 0:1], rt[:, 0:1], ["rt" + tag], ["rstd" + tag])


def mod_broadcast(P, dst_ap, psb, sel_sb, mod_sb, r, col0, mul_ap, reads_extra, wname, k, pname="psb"):
    for half in range(2):
        ps = psb[(k + half) % 2]
        pn = pname + "%d" % ((k + half) % 2)
        MM(P, ps[:], sel_sb[:, r, :], mod_sb[:, col0 + half * 512: col0 + (half + 1) * 512], True, True,
           ["sel", "mod"], [pn])
        d = dst_ap[:, half * 512:(half + 1) * 512]
        if mul_ap is None:
            CP(P, "scalar", d, ps[:], [pn], [wname])
        else:
            STT(P, d, ps[:], 1.0, mul_ap[:, half * 512:(half + 1) * 512], ALU.add, ALU.mult,
                [pn] + reads_extra, [wname])


def load_mod_bcast(nc, P, stack, modi, sel, col0, ncols, tag, psb=None):
    mod_sb = stack.enter_context(nc.sbuf_tensor("mod_sb" + tag, [2, ncols], F32))
    sel_sb = stack.enter_context(nc.sbuf_tensor("sel_sb" + tag, [2, 2, 128], F32))
    if psb is None:
        psb = [stack.enter_context(nc.psum_tensor("psb%s%d" % (tag, i), [128, 512], F32)) for i in range(2)]
    DMA(P, "sync", mod_sb[:], modi[:, col0:col0 + ncols], "c_mod", [], ["mod"])
    DMA(P, "sync", sel_sb[:], sel[:, :, :], "c_sel", [], ["sel"])
    return mod_sb, sel_sb, psb


def build_pre(kind):
    ncol = 1536 if kind == "ab" else 3072
    nnorm = 1280 if kind == "ab" else 2048
    G = nnorm // 64
    nc = bass.Bass("TRN2", target_bir_lowering=False)

    def din(name, shape, dt=F32):
        return nc.dram_tensor(name, shape, dt, kind="ExternalInput").ap()

    xin = din("xin", [TOK, D])
    scT = din("scT", [128, 8, 2])
    wmod = din("wmod", [D, 6 * D])
    bmod2 = din("bmod2", [2, 6 * D])
    nmix = din("nmix", [128, D])
    win = din("win", [D, ncol])
    gains = din("gains", [128, nnorm])
    cs = din("cs", [128, 16, 64])
    sel = din("sel", [2, 2, 128])
    qkv = nc.dram_tensor("qkv", [TOK, ncol], BF16, kind="ExternalOutput").ap()
    modo = nc.dram_tensor("modo", [2, 6 * D], F32, kind="ExternalOutput").ap()

    with contextlib.ExitStack() as st:
        def S(name, shape, dt):
            return st.enter_context(nc.sbuf_tensor(name, shape, dt))

        def PS(name, shape, dt):
            return st.enter_context(nc.psum_tensor(name, shape, dt))

        ident = S("ident", [128, 128], BF16)
        idf = S("idf", [128, 128], F32)
        win_sb = S("win_sb", [128, 8, ncol], BF16)
        Gb = S("Gb", [128, 2, D], F32)
        SHb = S("SHb", [128, 2, D], F32)
        gains_sb = S("gains_sb", [128, nnorm], F32)
        cs_sb = S("cs_sb", [128, 16, 64], F32)
        epst = S("epst", [128, 1], F32)
        st0 = contextlib.ExitStack()

        def S0(name, shape, dt):
            return st0.enter_context(nc.sbuf_tensor(name, shape, dt))

        def PS0(name, shape, dt):
            return st0.enter_context(nc.psum_tensor(name, shape, dt))

        mod_sb = S0("mod_sb", [2, 6 * D], F32)
        bm_sb = S0("bm_sb", [2, 6 * D], F32)
        sel_sb = S0("sel_sb", [2, 2, 128], F32)
        nmix_sb = S0("nmix_sb", [128, D], F32)
        sct = S0("sct", [128, 8, 2], F32)
        wmt = [S0("wm%d" % i, [128, 8, 512], F32) for i in range(2)]
        psm = [PS0("psm%d" % i, [2, 512], F32) for i in range(2)]
        psb = [PS0("psb%d" % i, [128, 512], F32) for i in range(2)]

        P = Prog(nc)
        make_ident(P, nc, ident, idf)
        MSET(P, "vector", epst[:], EPS, ["eps"])
        DMA(P, "sync", sct[:], scT[:, :, :], "c_sc", [], ["sct"])
        ACT(P, sct[:], sct[:], AF.Silu, ["sct"], ["sct"])
        DMA(P, "sync", bm_sb[:], bmod2[:, :], "c_bm", [], ["bm"])
        DMA(P, "sync", sel_sb[:], sel[:, :, :], "c_sel", [], ["sel"])
        DMA(P, "sync", nmix_sb[:], nmix[:, :], "c_nm", [], ["nmix"])
        DMA(P, "sync", gains_sb[:], gains[:, :], "c_gn", [], ["gains"])
        DMA(P, "sync", cs_sb[:], cs[:, :, :], "c_cs", [], ["cs"])
        for c in range(8):
            DMA(P, "gpsimd", win_sb[:, c, :], win[c * 128:(c + 1) * 128, :], "c_win", [], ["win%d" % c])
        wmod_v = wmod.rearrange("(c p) n -> p c n", p=128)
        for j in range(12):
            b = j % 2
            DMA(P, "sync", wmt[b][:], wmod_v[:, :, j * 512:(j + 1) * 512], "c_wm%d" % b, [], ["wm%d" % b])
            for c in range(8):
                MM(P, psm[b][:], sct[:, c, :], wmt[b][:, c, :], c == 0, c == 7, ["sct", "wm%d" % b], ["psm%d" % b])
            TT(P, "vector", mod_sb[:, j * 512:(j + 1) * 512], psm[b][:], bm_sb[:, j * 512:(j + 1) * 512], ALU.add,
               ["psm%d" % b, "bm"], ["mod"])
        DMA(P, "sync", modo[:, :], mod_sb[:], "c_mo", ["mod"], [])
        for r in range(2):
            mod_broadcast(P, SHb[:, r, :], psb, sel_sb, mod_sb, r, 0, None, [], "SHb", 0)
            mod_broadcast(P, Gb[:, r, :], psb, sel_sb, mod_sb, r, 1024, nmix_sb, ["nmix"], "Gb", 0)
        P.emit()
        st0.close()

        xt = [S("xt%d" % i, [128, D], F32) for i in range(2)]
        junk = S("junk", [128, D], BF16)
        ss = [S("ss%d" % i, [128, 1], F32) for i in range(2)]
        rt = [S("rt%d" % i, [128, 1], F32) for i in range(2)]
        rstd = [S("rstd%d" % i, [128, 1], F32) for i in range(2)]
        hb = [S("hb%d" % i, [128, D], BF16) for i in range(2)]
        hT = [S("hT%d" % i, [128, D], BF16) for i in range(2)]
        qf = [S("qf%d" % i, [128, ncol], F32) for i in range(2)]
        sqs = [S("sq%d" % i, [128, nnorm], F32) for i in range(2)]
        ssqs = [S("ssq%d" % i, [128, G], F32) for i in range(2)]
        rqs = [S("rq%d" % i, [128, G], F32) for i in range(2)]
        rq2s = [S("rq2%d" % i, [128, G], F32) for i in range(2)]
        t1 = S("t1", [128, G, 32], F32)
        t2 = S("t2", [128, G, 32], F32)
        t3 = S("t3", [128, G, 32], F32)
        t4 = S("t4", [128, G, 32], F32)
        ob = [S("ob%d" % i, [128, ncol], BF16) for i in range(2)]
        pT = PS("pT", [128, D], BF16)
        pq = [PS("pq%d" % i, [128, 512], F32) for i in range(3)]

        P = Prog(nc)
        nq_box = [0]

        def stage_a(t):
            b = t % 2
            r = 0 if t < 16 else 1
            X = "x%d" % b
            if t == 0:
                DMA(P, "sync", xt[0][:], xin[0:128, :], "c_x0", [], ["x0"])
            if t + 1 < NT:
                nb_ = (t + 1) % 2
                DMA(P, "sync", xt[nb_][:], xin[(t + 1) * 128:(t + 2) * 128, :], "c_x%d" % nb_, [], ["x%d" % nb_])
            rms_rstd(P, xt[b][:], junk[:], ss[b], rt[b], rstd[b], epst, D, [X], str(b))
            STT(P, xt[b][:], xt[b][:], rstd[b][:, 0:1], Gb[:, r, :], ALU.mult, ALU.mult, [X, "rstd%d" % b], [X])
            TT(P, "gpsimd", hb[b][:], xt[b][:], SHb[:, r, :], ALU.add, [X], ["hb%d" % b])
            for c in range(8):
                TR(P, pT[:, c * 128:(c + 1) * 128], hb[b][:, c * 128:(c + 1) * 128], ident[:], ["hb%d" % b], ["pT"])
            CP(P, "scalar", hT[b][:], pT[:], ["pT"], ["hT%d" % b])
            for jc in range(ncol // 512):
                pp = nq_box[0] % 3
                nq_box[0] += 1
                for c in range(8):
                    MM(P, pq[pp][:], hT[b][:, c * 128:(c + 1) * 128], win_sb[:, c, jc * 512:(jc + 1) * 512],
                       c == 0, c == 7, ["hT%d" % b], ["pq%d" % pp])
                CP(P, "scalar" if jc % 2 == 0 else "vector", qf[b][:, jc * 512:(jc + 1) * 512], pq[pp][:],
                   ["pq%d" % pp], ["qf%d" % b])
            QF = "qf%d" % b
            OB = "ob%d" % b

        def stage_b(t):
            b = t % 2
            QF = "qf%d" % b
            OB = "ob%d" % b
            sq, ssq, rq, rq2 = sqs[b], ssqs[b], rqs[b], rq2s[b]
            SQ, SSQ, RQ, RQ2 = "sq%d" % b, "ssq%d" % b, "rq%d" % b, "rq2%d" % b
            ACT(P, sq[:], qf[b][:, 0:nnorm], AF.Square, [QF], [SQ])
            RED(P, ssq[:], sq[:].rearrange("p (g d) -> p g d", d=64), ALU.add, [SQ], [SSQ])
            ACT(P, rq[:], ssq[:], AF.Sqrt, [SSQ], [RQ], scale=1.0 / 64, bias=epst[:, 0:1])
            RCP(P, rq2[:], rq[:], [RQ], [RQ2])
            qfv = qf[b][:, 0:nnorm].rearrange("p (g d) -> p g d", d=64)
            TT(P, "vector", qfv, qfv, rq2[:].unsqueeze(2).broadcast_to([128, G, 64]), ALU.mult, [QF, RQ2], [QF])
            if t < 16:
                TT(P, "vector", qf[b][:, 0:nnorm], qf[b][:, 0:nnorm], gains_sb[:], ALU.mult, [QF], [QF])
                qv = qf[b][:, 0:nnorm].rearrange("p (g h d) -> p g h d", h=2, d=32)
                ov = ob[b][:, 0:nnorm].rearrange("p (g h d) -> p g h d", h=2, d=32)
                cosb = cs_sb[:, t, 0:32].unsqueeze(1).broadcast_to([128, G, 32])
                sinb = cs_sb[:, t, 32:64].unsqueeze(1).broadcast_to([128, G, 32])
                TT(P, "vector", t1[:], qv[:, :, 0, :], cosb, ALU.mult, [QF], ["t1"])
                TT(P, "vector", t2[:], qv[:, :, 1, :], sinb, ALU.mult, [QF], ["t2"])
                TT(P, "vector", ov[:, :, 0, :], t1[:], t2[:], ALU.subtract, ["t1", "t2"], [OB + "a"])
                TT(P, "gpsimd", t3[:], qv[:, :, 1, :], cosb, ALU.mult, [QF], ["t3"])
                TT(P, "gpsimd", t4[:], qv[:, :, 0, :], sinb, ALU.mult, [QF], ["t4"])
                TT(P, "gpsimd", ov[:, :, 1, :], t3[:], t4[:], ALU.add, ["t3", "t4"], [OB + "b"])
            else:
                TT(P, "gpsimd", ob[b][:, 0:nnorm], qf[b][:, 0:nnorm], gains_sb[:], ALU.mult, [QF], [OB + "a", OB + "b"])
            CP(P, "scalar", ob[b][:, nnorm:ncol], qf[b][:, nnorm:ncol], [QF], [OB + "c"])
            DMA(P, "sync", qkv[t * 128:(t + 1) * 128, :], ob[b][:], "c_o%d" % b, [OB + "a", OB + "b", OB + "c"], [])

        stage_a(0)
        for t in range(NT):
            if t + 1 < NT:
                stage_a(t + 1)
            stage_b(t)
        P.emit()
    return nc


_PROG_CACHE = {}


def get_prog(key, builder):
    if key not in _PROG_CACHE:
        _PROG_CACHE[key] = builder()
    return _PROG_CACHE[key]


def rope_tables():
    rows_n = SEQ // 64
    rows = np.repeat(np.arange(rows_n, dtype=np.float32), 64)
    cols = np.tile(np.arange(64, dtype=np.float32), rows_n)
    inv = (np.float32(10000.0) ** (-np.arange(0, 32, 2, dtype=np.float32) / np.float32(32))).astype(np.float32)
    ang = np.concatenate([rows[:, None] * inv, cols[:, None] * inv], axis=-1).astype(np.float32)
    return np.cos(ang).astype(np.float32), np.sin(ang).astype(np.float32)


def pre_inputs(inp, l, x, ctx):
    i = l // 2
    kind = "ab" if l % 2 == 0 else "c"
    cc = np.stack([inp["c"][0], inp["c_ctx"]]).astype(np.float32)
    scT = np.ascontiguousarray(cc.reshape(2, 8, 128).transpose(2, 1, 0))
    bmod2 = np.ascontiguousarray(np.broadcast_to(inp["b_mod"][l], (2, 6 * D)))
    nmix = np.ascontiguousarray(np.broadcast_to(inp["norm_mix"][l], (128, D)))
    if kind == "ab":
        w = inp["w_in_ab"][i]
        win = np.concatenate([w[:, 0:512], w[:, 768:1280], w[:, 512:640], w[:, 1280:1408], w[:, 640:768],
                              w[:, 1408:1536]], axis=1)
        g = np.concatenate([np.tile(inp["qn_a"][i], 8), np.tile(inp["qn_b"][i], 8), np.tile(inp["kn_a"][i], 2),
                            np.tile(inp["kn_b"][i], 2)])
    else:
        win = inp["w_in_c"][i]
        g = np.concatenate([np.tile(inp["qn_c"][i], 16), np.tile(inp["kn_c"][i], 16)])
    win = np.ascontiguousarray(win.astype(np.float32))
    gains = np.ascontiguousarray(np.broadcast_to(g.astype(np.float32), (128, g.shape[0])))
    cos, sin = rope_tables()
    sel = np.zeros((2, 2, 128), np.float32)
    sel[0, 0] = 1
    sel[1, 1] = 1
    maps = []
    for c in range(NCORES):
        sl = slice(c * OWN, (c + 1) * OWN)
        cs = np.concatenate([cos[sl], sin[sl]], axis=-1).reshape(16, 128, 64).transpose(1, 0, 2)
        maps.append({
            "xin": np.ascontiguousarray(np.concatenate([x[sl], ctx], axis=0)),
            "scT": scT, "wmod": inp["w_mod"][l], "bmod2": bmod2, "nmix": nmix, "win": win, "gains": gains,
            "cs": np.ascontiguousarray(cs), "sel": sel,
        })
    return maps


class _Cnt:
    def __init__(self):
        self.d = {}

    def nxt(self, k, n):
        v = self.d.get(k, 0)
        self.d[k] = v + 1
        return v % n


def build_post(kind):
    ab = kind == "ab"
    nc = bass.Bass("TRN2", target_bir_lowering=False)

    def din(name, shape, dt=F32):
        return nc.dram_tensor(name, shape, dt, kind="ExternalInput").ap()

    xin = din("xin", [TOK, D])
    modi = din("modi", [2, 6 * D])
    sel = din("sel", [2, 2, 128])
    wout = din("wout", [D, D])
    nffn = din("nffn", [128, D])
    wr = din("wr", [D, 36])
    br = din("br", [128, 36])
    w1 = din("w1", [32, D, 512])
    w3 = din("w3", [32, D, 512])
    w2 = din("w2", [32, 512, D])
    tri128 = din("tri128", [128, 128], BF16)
    tri32 = din("tri32", [32, 32], BF16)
    thr = din("thr", [128, 36])
    biota = din("biota", [128, NBLK])
    piota = din("piota", [128, 1])
    if ab:
        qta = din("qta", [128, 2, NT, 512], BF16)
        qtb = din("qtb", [128, 2, NT, 512], BF16)
        kta = din("kta", [128, 2560], BF16)
        va = din("va", [128, 20, 2, 65], BF16)
        ktb = din("ktb", [128, NKT * 128], BF16)
        vb = din("vb", [128, NKT, 2, 65], BF16)
        sink = din("sink", [1, 8])
        flags = din("flags", [128, 2])
        mlo = din("mlo", [128, 512], BF16)
        mhi = din("mhi", [128, 512], BF16)
    else:
        qtc = din("qtc", [2, 128, 8, TOK], BF16)
        ktc = din("ktc", [8, 128, NKT * 128], BF16)
        vc = din("vc", [8, 128, NKT, 128], BF16)
        lamp = din("lamp", [128, 4, 64])
        laminit = din("laminit", [128, 1])
        oml = din("oml", [128, 1])
        subln = din("subln", [128, 1])
    xo = nc.dram_tensor("xo", [TOK, D], F32, kind="ExternalOutput").ap()
    x1s = nc.dram_tensor("x1s", [TOK, D], F32, kind="Internal").ap()
    buf = nc.dram_tensor("buf", [NBLK * 128, D], BF16, kind="Internal").ap()
    ybuf = nc.dram_tensor("ybuf", [NBLK * 128, D], F32, kind="Internal").ap()
    h2s = nc.dram_tensor("h2s", [TOK, D], BF16, kind="Internal").ap()

    with contextlib.ExitStack() as st:
        def S(name, shape, dt):
            return st.enter_context(nc.sbuf_tensor(name, shape, dt))

        ident = S("ident", [128, 128], BF16)
        identf = S("identf", [128, 128], F32)
        onesf = S("onesf", [128, 128], F32)
        epst = S("epst", [128, 1], F32)

        if True:
            P = Prog(nc)
            P.op("gpsimd", lambda e: e.iota(identf[:], pattern=[[1, 128]], base=0, channel_multiplier=-1,
                                             allow_small_or_imprecise_dtypes=True), (), ["idf0"])
            TS(P, "vector", ident[:], identf[:], 0.0, ALU.is_equal, ["idf0"], ["ident"])
            TS(P, "vector", identf[:], identf[:], 0.0, ALU.is_equal, ["idf0", "ident"], ["idf0"])
            MSET(P, "vector", epst[:], EPS, ["eps"])
            MSET(P, "vector", onesf[:], 1.0, ["onesf"])
            P.emit()

        sM = contextlib.ExitStack()
        M0b = sM.enter_context(nc.sbuf_tensor("M0b", [128, NT, 32], BF16))
        M1b = sM.enter_context(nc.sbuf_tensor("M1b", [128, NT, 32], BF16))
        wts = sM.enter_context(nc.sbuf_tensor("wts", [128, NT, 2], F32))
        desti = sM.enter_context(nc.sbuf_tensor("desti", [128, NT, 2], I32))
        idxA = sM.enter_context(nc.sbuf_tensor("idxA", [128, NBLK], I32))
        idxB = sM.enter_context(nc.sbuf_tensor("idxB", [128, NBLK], I32))
        sA = contextlib.ExitStack()
        if ab:
            oT = sA.enter_context(nc.sbuf_tensor("oT", [64, 16, TOK], BF16))
        else:
            oT = sA.enter_context(nc.sbuf_tensor("oT", [128, 8, TOK], BF16))

        with contextlib.ExitStack() as s1:
            def S1(name, shape, dt):
                return s1.enter_context(nc.sbuf_tensor(name, shape, dt))

            def PS1(name, shape, dt):
                return s1.enter_context(nc.psum_tensor(name, shape, dt))

            P = Prog(nc)
            cnt = _Cnt()
            if ab:
                kta_sb = S1("kta_sb", [128, 2560], BF16)
                va_sb = S1("va_sb", [128, 20, 2, 65], BF16)
                ktb_sb = S1("ktb_sb", [128, NKT * 128], BF16)
                vb_sb = S1("vb_sb", [128, NKT, 2, 65], BF16)
                q_sb = [S1("q_sb%d" % i, [128, 512], BF16) for i in range(2)]
                pt = [S1("pt%d" % i, [128, 512], BF16) for i in range(3)]
                ptm = [S1("ptm%d" % i, [128, 512], BF16) for i in range(2)]
                rrow = S1("rrow", [65, 512], F32)
                bc_sb = S1("bc_sb", [64, 512], F32)
                sink_sb = S1("sink_sb", [1, 8], F32)
                es_sb = S1("es_sb", [1, 8], F32)
                esrow = S1("esrow", [1, 2, 512], F32)
                e64 = S1("e64", [1, 65], F32)
                flags_sb = S1("flags_sb", [128, 2], F32)
                mlo_sb = S1("mlo_sb", [128, 512], BF16)
                mhi_sb = S1("mhi_sb", [128, 512], BF16)
                ps_s = [PS1("ps_s%d" % i, [128, 512], F32) for i in range(3)]
                ps_o = [PS1("ps_o%d" % i, [65, 512], F32) for i in range(2)]
                ps_b = PS1("ps_b", [64, 512], F32)

                DMA(P, "sync", kta_sb[:], kta[:, :], "c_kta", [], ["kta"])
                DMA(P, "sync", ktb_sb[:], ktb[:, :], "c_ktb", [], ["ktb"])
                DMA(P, "sync", vb_sb[:], vb[:, :, :, :], "c_vb", [], ["vb"])
                DMA(P, "sync", va_sb[:], va[:, :, :, :], "c_va", [], ["va"])
                DMA(P, "sync", sink_sb[:], sink[:, :], "c_sink", [], ["sink"])
                DMA(P, "sync", flags_sb[:], flags[:, :], "c_fl", [], ["flags"])
                DMA(P, "sync", mlo_sb[:], mlo[:, :], "c_mlo", [], ["mlo"])
                DMA(P, "sync", mhi_sb[:], mhi[:, :], "c_mhi", [], ["mhi"])
                ACT(P, es_sb[:], sink_sb[:], AF.Exp, ["sink"], ["es"])
                for kvh in range(2):
                    CP(P, "vector", esrow[0:1, kvh, :].rearrange("o (g q) -> o g q", q=128),
                       es_sb[0:1, kvh * 4:(kvh + 1) * 4].unsqueeze(2).broadcast_to([1, 4, 128]), ["es"], ["esrow"])
                MSET(P, "vector", e64[:], 0.0, ["e64"])
                MSET(P, "vector", e64[0:1, 64:65], 1.0, ["e64"])

                units = []
                for kvh in range(2):
                    for t in range(NT):
                        def key(kt, m=None, f=None, kvh=kvh):
                            return (kta_sb[:, kt * 128:(kt + 1) * 128], "kta", va_sb[:, kt, kvh, :], "va", m, f)
                        if t < 16:
                            keys = [key(t, mlo_sb[:], flags_sb[:, 0:1] if t == 0 else None), key(t + 1),
                                    key(t + 2, mhi_sb[:], flags_sb[:, 1:2] if t == 15 else None), key(18), key(19)]
                        else:
                            keys = [key(18), key(19)]
                        units.append((qta[:, kvh, t, :], keys, esrow[0:1, kvh, :],
                                      oT[:, kvh * 4:(kvh + 1) * 4, t * 128:(t + 1) * 128]))
                for kvh in range(2):
                    for t in range(NT):
                        kts = range(NKT) if t < 16 else (NKT - 2, NKT - 1)
                        keys = [(ktb_sb[:, kt * 128:(kt + 1) * 128], "ktb", vb_sb[:, kt, kvh, :], "vb", None, None)
                                for kt in kts]
                        units.append((qtb[:, kvh, t, :], keys, None,
                                      oT[:, 8 + kvh * 4:8 + (kvh + 1) * 4, t * 128:(t + 1) * 128]))
                flat = [(ui, i) for ui, u in enumerate(units) for i in range(len(u[1]))]
                LA = 2

                def issue_S(j):
                    ui, i = flat[j]
                    q_src, keys, _, _ = units[ui]
                    qb = ui % 2
                    QN = "q%d" % qb
                    if i == 0:
                        DMA(P, "sync", q_sb[qb][:], q_src, "c_q%d" % qb, [], [QN])
                    si = j % 3
                    MM(P, ps_s[si][:], keys[i][0], q_sb[qb][:], True, True, [QN, keys[i][1]], ["ps_s%d" % si])

                def issue_rest(j):
                    ui, i = flat[j]
                    q_src, keys, sink_rhs, out_ap = units[ui]
                    kT_ap, kname, v_ap, vname, mask_ap, flag_ap = keys[i]
                    n = len(keys)
                    si = j % 3
                    oi = ui % 2
                    ON = "ps_o%d" % oi
                    ACT(P, pt[si][:], ps_s[si][:], AF.Exp, ["ps_s%d" % si], ["pt%d" % si], scale=0.125)
                    rhs, rn = pt[si][:], "pt%d" % si
                    if mask_ap is not None:
                        mi = cnt.nxt("m", 2)
                        if flag_ap is not None:
                            STT(P, ptm[mi][:], pt[si][:], flag_ap, mask_ap, ALU.mult, ALU.mult,
                                [rn, "flags", "mlo", "mhi"], ["ptm%d" % mi])
                        else:
                            TT(P, "vector", ptm[mi][:], pt[si][:], mask_ap, ALU.mult, [rn, "mlo", "mhi"],
                               ["ptm%d" % mi])
                        rhs, rn = ptm[mi][:], "ptm%d" % mi
                    MM(P, ps_o[oi][:], v_ap, rhs, i == 0, (i == n - 1) and sink_rhs is None, [rn, vname], [ON])
                    if i == n - 1:
                        if sink_rhs is not None:
                            MM(P, ps_o[oi][:], e64[0:1, :], sink_rhs, False, True, ["e64", "esrow"], [ON])
                        RCP(P, rrow[64:65, :], ps_o[oi][64:65, :], [ON], ["rrow"])
                        MM(P, ps_b[:], onesf[64:65, 0:64], rrow[64:65, :], True, True, ["rrow", "onesf"], ["ps_b"])
                        CP(P, "scalar", bc_sb[:], ps_b[:], ["ps_b"], ["bc"])
                        TT(P, "vector", out_ap, ps_o[oi][0:64, :].rearrange("d (g q) -> d g q", q=128),
                           bc_sb[:].rearrange("d (g q) -> d g q", q=128), ALU.mult, [ON, "bc"], ["oT"])

                for j in range(min(LA, len(flat))):
                    issue_S(j)
                for j in range(len(flat)):
                    if j + LA < len(flat):
                        issue_S(j + LA)
                    issue_rest(j)
            else:
                kt_sbs = [S1("kt_sb%d" % i, [128, NKT * 128], BF16) for i in range(2)]
                v_sbs = [S1("v_sb%d" % i, [128, NKT, 128], BF16) for i in range(2)]
                q1_sb = [S1("q1_sb%d" % i, [128, 512], BF16) for i in range(2)]
                q2_sb = [S1("q2_sb%d" % i, [128, 512], BF16) for i in range(2)]
                p1 = [S1("p1_%d" % i, [128, 512], BF16) for i in range(3)]
                p2 = [S1("p2_%d" % i, [128, 512], BF16) for i in range(3)]
                E0 = S1("E0", [128, 128], F32)
                E1 = S1("E1", [128, 128], BF16)
                acc1 = S1("acc1", [128, 512], F32)
                acc2 = S1("acc2", [128, 512], F32)
                rr = S1("rr", [33, 512], F32)
                b1 = S1("b1", [128, 512], F32)
                d1 = S1("d1", [128, 512], F32)
                d2 = S1("d2", [128, 512], F32)
                Os = S1("Os", [128, 512], F32)
                sqo = S1("sqo", [128, 512], F32)
                rs = S1("rs", [128, 512], F32)
                rs2 = S1("rs2", [128, 512], F32)
                lamp_sb = S1("lamp_sb", [128, 4, 64], F32)
                lp = S1("lp", [128, 2, 64], F32)
                s2 = S1("s2", [128, 2], F32)
                e2 = S1("e2", [128, 2], F32)
                lamv = S1("lamv", [128, 1], F32)
                nlam = S1("nlam", [128, 1], F32)
                li_sb = S1("li_sb", [128, 1], F32)
                oml_sb = S1("oml_sb", [128, 1], F32)
                sub_sb = S1("sub_sb", [128, 1], F32)
                subs = S1("subs", [128, 1], F32)
                ps_s1 = [PS1("ps_s1_%d" % i, [128, 512], F32) for i in range(2)]
                ps_s2 = [PS1("ps_s2_%d" % i, [128, 512], F32) for i in range(2)]
                ps_o1 = PS1("ps_o1", [128, 512], F32)
                ps_o2 = PS1("ps_o2", [128, 512], F32)
                ps_sum = PS1("ps_sum", [128, 512], F32)
                ps_x = PS1("ps_x", [128, 512], F32)

                DMA(P, "sync", lamp_sb[:], lamp[:, :, :], "c_lamp", [], ["lamp"])
                DMA(P, "sync", li_sb[:], laminit[:, :], "c_li", [], ["li"])
                DMA(P, "sync", oml_sb[:], oml[:, :], "c_oml", [], ["oml"])
                DMA(P, "sync", sub_sb[:], subln[:, :], "c_sub", [], ["sub"])
                TT(P, "vector", lp[:, 0, :], lamp_sb[:, 0, :], lamp_sb[:, 1, :], ALU.mult, ["lamp"], ["lp"])
                TT(P, "vector", lp[:, 1, :], lamp_sb[:, 2, :], lamp_sb[:, 3, :], ALU.mult, ["lamp"], ["lp"])
                RED(P, s2[:], lp[:], ALU.add, ["lp"], ["s2"])
                ACT(P, e2[:], s2[:], AF.Exp, ["s2"], ["e2"])
                TT(P, "vector", lamv[:], e2[:, 1:2], e2[:, 0:1], ALU.subtract, ["e2"], ["lamv"])
                TT(P, "vector", nlam[:], lamv[:], li_sb[:], ALU.subtract, ["lamv", "li"], ["nlam"])
                TT(P, "vector", subs[:], sub_sb[:], oml_sb[:], ALU.mult, ["sub", "oml"], ["subs"])
                MSET(P, "vector", E0[:], 0.0, ["E0"])
                MSET(P, "vector", E0[:, 0:1], 1.0, ["E0"])
                MSET(P, "vector", E1[:], 0.0, ["E1"])
                MSET(P, "vector", E1[:, 32:33], 1.0, ["E1"])
                items = []
                for h in range(8):
                    for u in range(5):
                        tok0, N = (u * 512, 512) if u < 4 else (OWN, CTX)
                        kts = list(range(NKT)) if u < 4 else [NKT - 2, NKT - 1]
                        for i, kt in enumerate(kts):
                            items.append((h, u, tok0, N, kt, i, len(kts)))

                def kv_load(h):
                    DMA(P, "sync", kt_sbs[h % 2][:], ktc[h, :, :], "c_kt%d" % (h % 2), [], ["kt%d" % (h % 2)])
                    DMA(P, "sync", v_sbs[h % 2][:], vc[h, :, :, :], "c_v%d" % (h % 2), [], ["v%d" % (h % 2)])

                def issue_S(j):
                    h, u, tok0, N, kt, i, n = items[j]
                    qb = (h * 5 + u) % 2
                    QN = "q%d" % qb
                    if i == 0:
                        if u == 0 and h == 0:
                            kv_load(0)
                            kv_load(1)
                        DMA(P, "sync", q1_sb[qb][:, 0:N], qtc[0, :, h, tok0:tok0 + N], "c_q%d" % qb, [], [QN])
                        DMA(P, "sync", q2_sb[qb][:, 0:N], qtc[1, :, h, tok0:tok0 + N], "c_q%d" % qb, [], [QN])
                    b = j % 2
                    ksl = slice(kt * 128, (kt + 1) * 128)
                    kt_sb = kt_sbs[h % 2]
                    KT = "kt%d" % (h % 2)
                    MM(P, ps_s1[b][:, 0:N], kt_sb[:, ksl], q1_sb[qb][:, 0:N], True, True, [QN, KT], ["ps_s1_%d" % b])
                    MM(P, ps_s2[b][:, 0:N], kt_sb[:, ksl], q2_sb[qb][:, 0:N], True, True, [QN, KT], ["ps_s2_%d" % b])

                def issue_rest(j):
                    h, u, tok0, N, kt, i, n = items[j]
                    b = j % 2
                    v_sb = v_sbs[h % 2]
                    VN = "v%d" % (h % 2)
                    pb = j % 3
                    ACT(P, p1[pb][:, 0:N], ps_s1[b][:, 0:N], AF.Exp, ["ps_s1_%d" % b, "ps_s2_%d" % b], ["p1_%d" % pb], scale=0.125)
                    ACT(P, p2[pb][:, 0:N], ps_s2[b][:, 0:N], AF.Exp, ["ps_s2_%d" % b], ["p2_%d" % pb], scale=0.125)
                    MM(P, ps_o2[:, 0:N], v_sb[:, kt, :], p2[pb][:, 0:N], i == 0, i == n - 1, [VN, "p2_%d" % pb], ["ps_o2"])
                    MM(P, ps_o1[:, 0:N], v_sb[:, kt, :], p1[pb][:, 0:N], i == 0, i == n - 1, [VN, "p1_%d" % pb], ["ps_o1"])
                    MM(P, ps_sum[:, 0:N], E1[:], p2[pb][:, 0:N], i == 0, False, ["E1", "p2_%d" % pb], ["ps_sum"])
                    if i == 0:
                        CP(P, "vector", acc1[:, 0:N], p1[pb][:, 0:N], ["p1_%d" % pb], ["acc1"])
                    else:
                        TT(P, "vector", acc1[:, 0:N], acc1[:, 0:N], p1[pb][:, 0:N], ALU.add, ["p1_%d" % pb, "acc1"], ["acc1"])
                    if i != n - 1:
                        return
                    MM(P, ps_sum[:, 0:N], E0[:], acc1[:, 0:N], False, True, ["E0", "acc1"], ["ps_sum"])
                    if u == 4 and 1 <= h + 1 < 7:
                        kv_load(h + 2)
                    RCP(P, rr[0:1, 0:N], ps_sum[0:1, 0:N], ["ps_sum"], ["rr"])
                    RCP(P, rr[32:33, 0:N], ps_sum[32:33, 0:N], ["ps_sum"], ["rr"])
                    TS(P, "vector", rr[32:33, 0:N], rr[32:33, 0:N], nlam[32:33, 0:1], ALU.mult, ["rr", "nlam"], ["rr"])
                    MM(P, ps_x[:, 0:N], onesf[0:1, :], rr[0:1, 0:N], True, True, ["rr", "onesf"], ["ps_x"])
                    CP(P, "scalar", b1[:, 0:N], ps_x[:, 0:N], ["ps_x"], ["b1"])
                    TT(P, "vector", d1[:, 0:N], ps_o1[:, 0:N], b1[:, 0:N], ALU.mult, ["ps_o1", "b1"], ["d1"])
                    MM(P, ps_x[:, 0:N], onesf[32:33, :], rr[32:33, 0:N], True, True, ["rr", "onesf"], ["ps_x"])
                    CP(P, "scalar", b1[:, 0:N], ps_x[:, 0:N], ["ps_x"], ["b1"])
                    TT(P, "vector", d2[:, 0:N], ps_o2[:, 0:N], b1[:, 0:N], ALU.mult, ["ps_o2", "b1"], ["d2"])
                    TT(P, "gpsimd", Os[:, 0:N], d1[:, 0:N], d2[:, 0:N], ALU.add, ["d1", "d2"], ["Os"])
                    TT(P, "gpsimd", sqo[:, 0:N], Os[:, 0:N], Os[:, 0:N], ALU.mult, ["Os"], ["sqo"])
                    MM(P, ps_x[:, 0:N], onesf[:], sqo[:, 0:N], True, True, ["sqo", "onesf"], ["ps_x"])
                    ACT(P, rs[:, 0:N], ps_x[:, 0:N], AF.Sqrt, ["ps_x"], ["rs"], scale=1.0 / 128, bias=epst[:, 0:1])
                    RCP(P, rs2[:, 0:N], rs[:, 0:N], ["rs"], ["rs2"])
                    STT(P, oT[:, h, tok0:tok0 + N], Os[:, 0:N], subs[:, 0:1], rs2[:, 0:N], ALU.mult, ALU.mult,
                        ["Os", "subs", "rs2"], ["oT"])

                issue_S(0)
                for j in range(len(items)):
                    if j + 1 < len(items):
                        issue_S(j + 1)
                    issue_rest(j)
            P.emit()

        with contextlib.ExitStack() as s2_:
            def S2(name, shape, dt):
                return s2_.enter_context(nc.sbuf_tensor(name, shape, dt))

            def PS2(name, shape, dt):
                return s2_.enter_context(nc.psum_tensor(name, shape, dt))

            KC = 16 if ab else 8
            KP = 64 if ab else 128
            GT1b = S2("GT1b", [128, 2, D], F32)
            SH2b = S2("SH2b", [128, 2, D], F32)
            G2b = S2("G2b", [128, 2, D], F32)
            nffn_sb = S2("nffn_sb", [128, D], F32)
            wout_sb = S2("wout_sb", [KP, KC, D], BF16)
            wr_sb = S2("wr_sb", [128, 8, 36], F32)
            br_sb = S2("br_sb", [128, 36], F32)
            xt = [S2("xt%d" % i, [128, D], F32) for i in range(2)]
            x1 = [S2("x1_%d" % i, [128, D], F32) for i in range(2)]
            junk = S2("junk", [128, D], BF16)
            ss = [S2("ss%d" % i, [128, 1], F32) for i in range(2)]
            rt = [S2("rt%d" % i, [128, 1], F32) for i in range(2)]
            rstd = [S2("rstd%d" % i, [128, 1], F32) for i in range(2)]
            h2f = [S2("h2f%d" % i, [128, D], F32) for i in range(2)]
            h2Ts = [S2("h2T%d" % i, [128, D], F32) for i in range(2)]
            h2bt = [S2("h2bt%d" % i, [128, D], BF16) for i in range(2)]
            lgs = [S2("lg%d" % i, [128, 36], F32) for i in range(2)]
            gmax = S2("gmax", [128, 1], F32)
            ngmax = S2("ngmax", [128, 1], F32)
            gm = S2("gm", [128, 4], F32)
            ge = S2("ge", [128, 4], F32)
            gsum = S2("gsum", [128, 1], F32)
            gp = S2("gp", [128, 1], F32)
            pen = S2("pen", [128, 4], F32)
            lem = S2("lem", [128, 32], F32)
            m8 = S2("m8", [128, 8], F32)
            dm = S2("dm", [128, 1], F32)
            rr_ = S2("rr_", [128, 1], F32)
            den = S2("den", [128, 1], F32)
            rden = S2("rden", [128, 1], F32)
            pq = [PS2("pq%d" % i, [128, 512], F32) for i in range(2)]
            pTf = PS2("pTf", [128, D], F32)
            ps_lg = PS2("ps_lg", [128, 36], F32)

            P = Prog(nc)
            mod_sb, sel_sb, psb = load_mod_bcast(nc, P, s2_, modi, sel, 2048, 3072, "o")
            DMA(P, "sync", nffn_sb[:], nffn[:, :], "c_nf", [], ["nffn"])
            for r in range(2):
                mod_broadcast(P, GT1b[:, r, :], psb, sel_sb, mod_sb, r, 0, None, [], "GT1b", 0)
                mod_broadcast(P, SH2b[:, r, :], psb, sel_sb, mod_sb, r, 1024, None, [], "SH2b", 0)
                mod_broadcast(P, G2b[:, r, :], psb, sel_sb, mod_sb, r, 2048, nffn_sb, ["nffn"], "G2b", 0)
            wv = wout.rearrange("(c p) n -> p c n", p=KP)
            for c in range(KC):
                DMA(P, "gpsimd", wout_sb[:, c, :], wv[:, c, :], "c_wo", [], ["wout%d" % c])
            DMA(P, "sync", wr_sb[:], wr.rearrange("(c p) n -> p c n", p=128), "c_wr", [], ["wr"])
            DMA(P, "sync", br_sb[:], br[:, :], "c_br", [], ["br"])
            def stage1(t):
                b = t % 2
                r = 0 if t < 16 else 1
                X, X1, H2 = "x%d" % b, "x1_%d" % b, "h2f%d" % b
                if t == 0:
                    DMA(P, "sync", xt[0][:], xin[0:128, :], "c_x0", [], ["x0"])
                if t + 1 < NT:
                    nb_ = (t + 1) % 2
                    DMA(P, "sync", xt[nb_][:], xin[(t + 1) * 128:(t + 2) * 128, :], "c_x%d" % nb_, [], ["x%d" % nb_])
                for half in range(2):
                    hs = slice(half * 512, (half + 1) * 512)
                    for c in range(KC):
                        MM(P, pq[half][:], oT[:, c, t * 128:(t + 1) * 128], wout_sb[:, c, hs], c == 0, c == KC - 1,
                           ["wout%d" % c], ["pq%d" % half])
                    TT(P, "vector", x1[b][:, hs], pq[half][:], GT1b[:, r, hs], ALU.mult, ["pq%d" % half, "GT1b"], [X1])
                TT(P, "gpsimd", x1[b][:], x1[b][:], xt[b][:], ALU.add, [X1, X], [X1])
                DMA(P, "sync", x1s[t * 128:(t + 1) * 128, :], x1[b][:], "c_x1s%d" % b, [X1], ["x1s%d" % t])
                rms_rstd(P, x1[b][:], junk[:], ss[b], rt[b], rstd[b], epst, D, [X1], str(b))
                STT(P, h2f[b][:], x1[b][:], rstd[b][:, 0:1], G2b[:, r, :], ALU.mult, ALU.mult, [X1, "rstd%d" % b, "G2b"], [H2])
                TT(P, "gpsimd", h2f[b][:], h2f[b][:], SH2b[:, r, :], ALU.add, [H2, "SH2b"], [H2])
                CP(P, "scalar", h2bt[b][:], h2f[b][:], [H2], ["h2bt%d" % b])
                DMA(P, "sync", h2s[t * 128:(t + 1) * 128, :], h2bt[b][:], "c_h2s%d" % b, ["h2bt%d" % b], ["h2s%d" % t])
                for c in range(8):
                    TR(P, pTf[:, c * 128:(c + 1) * 128], h2f[b][:, c * 128:(c + 1) * 128], identf[:], [H2], ["pTf"])
                h2T = h2Ts[b]
                CP(P, "vector", h2T[:], pTf[:], ["pTf"], ["h2T%d" % b])
                for c in range(8):
                    MM(P, ps_lg[:], h2T[:, c * 128:(c + 1) * 128], wr_sb[:, c, :], c == 0, c == 7, ["h2T%d" % b, "wr"], ["ps_lg"])
                TT(P, "vector", lgs[b][:], ps_lg[:], br_sb[:], ALU.add, ["ps_lg", "br"], ["lg%d" % b])

            def stage2(t):
                b = t % 2
                lg = lgs[b]
                RED(P, gmax[:], lg[:, 0:4], ALU.max, ["lg%d" % b], ["gmax"])
                TS(P, "vector", gm[:], lg[:, 0:4], gmax[:, 0:1], ALU.is_equal, ["lg%d" % b, "gmax"], ["gm"])
                TS(P, "vector", ngmax[:], gmax[:], -1.0, ALU.mult, ["gmax"], ["ngmax"])
                ACT(P, ge[:], lg[:, 0:4], AF.Exp, ["lg%d" % b, "ngmax"], ["ge", "gsum"], bias=ngmax[:, 0:1], accum_out=gsum[:, 0:1])
                RCP(P, gp[:], gsum[:], ["gsum"], ["gp"])
                TS(P, "vector", pen[:], gm[:], 1e30, ALU.mult, ["gm"], ["pen"], s2=-1e30, op1=ALU.add)
                TT(P, "vector", lem[:].rearrange("p (g e) -> p g e", e=8), lg[:, 4:36].rearrange("p (g e) -> p g e", e=8),
                   pen[:].unsqueeze(2).broadcast_to([128, 4, 8]), ALU.add, ["lg%d" % b, "pen"], ["lem"])
                P.op("vector", lambda e: e.max(out=m8[:], in_=lem[:]), ["lem"], ["m8"])
                TS(P, "vector", M0b[:, t, :], lem[:], m8[:, 0:1], ALU.is_equal, ["lem", "m8"], ["M0b"])
                TS(P, "vector", M1b[:, t, :], lem[:], m8[:, 1:2], ALU.is_equal, ["lem", "m8"], ["M1b"])
                TT(P, "vector", dm[:], m8[:, 1:2], m8[:, 0:1], ALU.subtract, ["m8"], ["dm"])
                ACT(P, rr_[:], dm[:], AF.Exp, ["dm"], ["rr_"])
                TS(P, "vector", den[:], rr_[:], 1.0, ALU.add, ["rr_"], ["den"])
                RCP(P, rden[:], den[:], ["den"], ["rden"])
                TT(P, "vector", wts[:, t, 0:1], rden[:], gp[:], ALU.mult, ["rden", "gp"], ["wts"])
                TT(P, "vector", wts[:, t, 1:2], wts[:, t, 0:1], rr_[:], ALU.mult, ["wts", "rr_"], ["wts"])

            stage1(0)
            for t in range(NT):
                if t + 1 < NT:
                    stage1(t + 1)
                stage2(t)
            P.emit()
        with contextlib.ExitStack() as s3:
            def S3(name, shape, dt):
                return s3.enter_context(nc.sbuf_tensor(name, shape, dt))

            def PS3(name, shape, dt):
                return s3.enter_context(nc.psum_tensor(name, shape, dt))

            tri_sb = S3("tri_sb", [128, 128], BF16)
            tri32_sb = S3("tri32_sb", [32, 32], BF16)
            ones_b = S3("ones_b", [128, 128], BF16)
            thr_sb = S3("thr_sb", [128, 36], F32)
            bio_sb = S3("bio_sb", [128, NBLK], F32)
            pio_sb = S3("pio_sb", [128, 1], F32)
            Ms = S3("Ms", [128, NT, 32], BF16)
            Cs = S3("Cs", [128, NT, 32], F32)
            cntf = S3("cntf", [128, 32], F32)
            cmp1 = S3("cmp1", [128, 32, 36], F32)
            nblk = S3("nblk", [128, 32], F32)
            nblkb = S3("nblkb", [128, 32], BF16)
            nbT = S3("nbT", [32, 128], BF16)
            Sx = S3("Sx", [128, 32], F32)
            pend = S3("pend", [128, 32], F32)
            base = S3("base", [128, 32], F32)
            tall = S3("tall", [128, NT, 32], F32)
            prod = S3("prod", [128, NT, 32], F32)
            destf = S3("destf", [128, NT, 2], F32)
            cmp2 = S3("cmp2", [128, NBLK, 32], F32)
            be = S3("be", [128, NBLK], F32)
            idxf = S3("idxf", [128, NBLK], F32)
            ps_c = [PS3("ps_c%d" % i, [128, 32], F32) for i in range(2)]
            ps_t = PS3("ps_t", [32, 128], BF16)
            ps_S = PS3("ps_S", [128, 32], F32)

            P = Prog(nc)
            DMA(P, "sync", tri_sb[:], tri128[:, :], "c_tri", [], ["tri"])
            DMA(P, "sync", tri32_sb[:], tri32[:, :], "c_tri32", [], ["tri32"])
            DMA(P, "sync", thr_sb[:], thr[:, :], "c_thr", [], ["thr"])
            DMA(P, "sync", bio_sb[:], biota[:, :], "c_bio", [], ["bio"])
            DMA(P, "sync", pio_sb[:], piota[:, :], "c_pio", [], ["pio"])
            MSET(P, "vector", ones_b[:], 1.0, ["ones_b"])
            TT(P, "vector", Ms[:], M0b[:], M1b[:], ALU.add, [], ["Ms"])
            for t in range(NT):
                pc = ps_c[t % 2]
                pn = "ps_c%d" % (t % 2)
                MM(P, pc[:], tri_sb[:], Ms[:, t, :], True, t == 0, ["tri", "Ms"], [pn])
                for i in range(t):
                    MM(P, pc[:], ones_b[:], Ms[:, i, :], False, i == t - 1, ["ones_b", "Ms"], [pn])
                CP(P, "vector", Cs[:, t, :], pc[:], [pn], ["Cs"])
            pc = ps_c[NT % 2]
            pn = "ps_c%d" % (NT % 2)
            for i in range(NT):
                MM(P, pc[:], ones_b[:], Ms[:, i, :], i == 0, i == NT - 1, ["ones_b", "Ms"], [pn])
            CP(P, "vector", cntf[:], pc[:], [pn], ["cntf"])
            TT(P, "vector", cmp1[:], cntf[:].unsqueeze(2).broadcast_to([128, 32, 36]),
               thr_sb[:].unsqueeze(1).broadcast_to([128, 32, 36]), ALU.is_gt, ["cntf", "thr"], ["cmp1"])
            RED(P, nblk[:], cmp1[:], ALU.add, ["cmp1"], ["nblk"])
            CP(P, "vector", nblkb[:], nblk[:], ["nblk"], ["nblkb"])
            TR(P, ps_t[:], nblkb[:], ident[:], ["nblkb"], ["ps_t"])
            CP(P, "vector", nbT[:], ps_t[:], ["ps_t"], ["nbT"])
            MM(P, ps_S[:], nbT[:], tri32_sb[:], True, True, ["nbT", "tri32"], ["ps_S"])
            CP(P, "vector", Sx[:], ps_S[:], ["ps_S"], ["Sx"])
            TT(P, "vector", pend[:], Sx[:], nblk[:], ALU.add, ["Sx", "nblk"], ["pend"])
            TS(P, "vector", base[:], Sx[:], 128.0, ALU.mult, ["Sx"], ["base"])
            TT(P, "vector", tall[:], Cs[:], base[:].unsqueeze(1).broadcast_to([128, NT, 32]), ALU.add, ["Cs", "base"], ["tall"])
            TT(P, "vector", prod[:], tall[:], M0b[:], ALU.mult, ["tall"], ["prod"])
            RED(P, destf[:, :, 0], prod[:], ALU.add, ["prod"], ["destf"])
            TT(P, "vector", prod[:], tall[:], M1b[:], ALU.mult, ["tall"], ["prod"])
            RED(P, destf[:, :, 1], prod[:], ALU.add, ["prod"], ["destf"])
            CP(P, "vector", desti[:], destf[:], ["destf"], ["desti"])
            TT(P, "vector", cmp2[:], pend[:].unsqueeze(1).broadcast_to([128, NBLK, 32]),
               bio_sb[:].unsqueeze(2).broadcast_to([128, NBLK, 32]), ALU.is_le, ["pend", "bio"], ["cmp2"])
            RED(P, be[:], cmp2[:], ALU.add, ["cmp2"], ["be"])
            TS(P, "vector", be[:], be[:], 31.0, ALU.min, ["be"], ["be"])
            TS(P, "vector", idxf[:], be[:], 256.0, ALU.mult, ["be", "pio"], ["idxf"], s2=pio_sb[:, 0:1], op1=ALU.add)
            CP(P, "vector", idxA[:], idxf[:], ["idxf"], ["idxA"])
            TS(P, "vector", idxf[:], idxf[:], 1.0, ALU.add, ["idxf", "idxA"], ["idxf"])
            CP(P, "vector", idxB[:], idxf[:], ["idxf"], ["idxB"])
            P.emit()

        sA.close()
        with contextlib.ExitStack() as s4:
            def S4(name, shape, dt):
                return s4.enter_context(nc.sbuf_tensor(name, shape, dt))

            def PS4(name, shape, dt):
                return s4.enter_context(nc.psum_tensor(name, shape, dt))

            NW = 3
            GT2b = S4("GT2b", [128, 2, D], F32)
            W1b = [S4("W1b%d" % i, [128, 4096], BF16) for i in range(NW)]
            W3b = [S4("W3b%d" % i, [128, 4096], BF16) for i in range(NW)]
            W2b = [S4("W2b%d" % i, [128, 4096], BF16) for i in range(NW)]
            xb = [S4("xb%d" % i, [128, D], BF16) for i in range(2)]
            xbT = [S4("xbT%d" % i, [128, 8, 128], BF16) for i in range(2)]
            s1t = S4("s1t", [128, 512], F32)
            gT = [S4("gT%d" % i, [128, 4, 128], BF16) for i in range(2)]
            ysb = [S4("ysb%d" % i, [128, D], F32) for i in range(2)]
            y0 = [S4("y0_%d" % i, [128, D], F32) for i in range(2)]
            y1 = [S4("y1_%d" % i, [128, D], F32) for i in range(2)]
            xc = [S4("xc%d" % i, [128, D], F32) for i in range(2)]
            f0 = S4("f0", [128, D], F32)
            f1 = S4("f1", [128, D], F32)
            f2 = f0
            xo_sb = [S4("xo_sb%d" % i, [128, D], F32) for i in range(2)]
            pxT = PS4("pxT", [128, 8, 128], BF16)
            ps1 = [PS4("ps1_%d" % i, [128, 4, 128], F32) for i in range(2)]
            ps3 = [PS4("ps3_%d" % i, [128, 4, 128], F32) for i in range(2)]
            psy = [PS4("psy%d" % i, [128, 512], F32) for i in range(2)]

            P = Prog(nc)
            for t in range(NT):
                b = t % 2
                DMA(P, "sync", xb[b][:], h2s[t * 128:(t + 1) * 128, :], "c_xb%d" % b, [], ["xb%d" % b])
                for k in range(2):
                    P.dma("gpsimd", (lambda t, k, b: lambda e: e.indirect_dma_start(
                        out=buf[:, :], out_offset=bass.IndirectOffsetOnAxis(ap=desti[:, t, k:k + 1], axis=0),
                        in_=xb[b][:, :], in_offset=None))(t, k, b), "c_sc%d" % b, ["xb%d" % b], ["buf_%d_%d" % (t, k)])
            w1v = w1.rearrange("e (p h c) f -> (e p h) (c f)", h=2, c=4)
            w3v = w3.rearrange("e (p h c) f -> (e p h) (c f)", h=2, c=4)
            w2v = w2.rearrange("e (p h c) n -> (e p h) (c n)", h=2, c=2)
            mod_sb, sel_sb, psb = load_mod_bcast(nc, P, s4, modi, sel, 5120, 1024, "m", psb=psy)
            for r in range(2):
                mod_broadcast(P, GT2b[:, r, :], psb, sel_sb, mod_sb, r, 0, None, [], "GT2b", 0, pname="psy")

            def stage_w(blk):
                wb = blk % NW
                for (wv_, Wt, nm) in ((w1v, W1b, "W1"), (w3v, W3b, "W3"), (w2v, W2b, "W2")):
                    for hh, idx in ((0, idxA), (1, idxB)):
                        P.dma("gpsimd", (lambda wv_, Wt, hh, idx, blk, wb: lambda e: e.indirect_dma_start(
                            out=Wt[wb][:, hh * 2048:(hh + 1) * 2048], out_offset=None, in_=wv_[:, :],
                            in_offset=bass.IndirectOffsetOnAxis(ap=idx[:, blk:blk + 1], axis=0)))(wv_, Wt, hh, idx, blk, wb),
                            "c_%s%d_%d" % (nm, wb, hh), [], ["%s%d_%d" % (nm, wb, hh)])

            def stage_a(blk):
                b = blk % 2
                wb = blk % NW
                DMA(P, "sync", xb[b][:], buf[blk * 128:(blk + 1) * 128, :], "c_xb%d" % b,
                    ["buf_%d_%d" % (t_, k_) for t_ in range(NT) for k_ in range(2)], ["xb%d" % b])
                xv = xb[b][:].rearrange("s (p c) -> s c p", c=8)
                for c in range(8):
                    TR(P, pxT[:, c, :], xv[:, c, :], ident[:], ["xb%d" % b], ["pxT"])
                CP(P, "vector", xbT[b][:], pxT[:], ["pxT"], ["xbT%d" % b])
                W1v = W1b[wb][:].rearrange("p (cc j q) -> p cc j q", cc=8, q=4)
                W3v = W3b[wb][:].rearrange("p (cc j q) -> p cc j q", cc=8, q=4)
                for cq in range(4):
                    for c in range(8):
                        MM(P, ps1[b][:, cq, :], W1v[:, c, :, cq], xbT[b][:, c, :], c == 0, c == 7,
                           ["W1%d_0" % wb, "W1%d_1" % wb, "xbT%d" % b], ["ps1_%d" % b])
                for cq in range(4):
                    for c in range(8):
                        MM(P, ps3[b][:, cq, :], W3v[:, c, :, cq], xbT[b][:, c, :], c == 0, c == 7,
                           ["W3%d_0" % wb, "W3%d_1" % wb, "xbT%d" % b], ["ps3_%d" % b])

            def stage_b(blk):
                b = blk % 2
                wb = blk % NW
                ACT(P, s1t[:], ps1[b][:].rearrange("p a s -> p (a s)"), AF.Silu, ["ps1_%d" % b], ["s1t"])
                TT(P, "vector", gT[b][:].rearrange("p a s -> p (a s)"), s1t[:], ps3[b][:].rearrange("p a s -> p (a s)"),
                   ALU.mult, ["s1t", "ps3_%d" % b], ["gT%d" % b])
                for half in range(2):
                    for cq in range(4):
                        MM(P, psy[half][:], gT[b][:, cq, :], W2b[wb][:, cq * 1024 + half * 512: cq * 1024 + (half + 1) * 512],
                           cq == 0, cq == 3, ["gT%d" % b, "W2%d_0" % wb, "W2%d_1" % wb], ["psy%d" % half])
                    CP(P, "scalar" if half == 0 else "vector", ysb[b][:, half * 512:(half + 1) * 512], psy[half][:],
                       ["psy%d" % half], ["ysb%d" % b])
                DMA(P, "sync", ybuf[blk * 128:(blk + 1) * 128, :], ysb[b][:], "c_yb%d" % b, ["ysb%d" % b], ["ybuf%d" % blk])

            stage_w(0)
            stage_w(1)
            stage_a(0)
            for blk in range(NBLK):
                if blk + 2 < NBLK:
                    stage_w(blk + 2)
                if blk + 1 < NBLK:
                    stage_a(blk + 1)
                stage_b(blk)
            for t in range(NT):
                b = t % 2
                r = 0 if t < 16 else 1
                for k, yt in ((0, y0), (1, y1)):
                    P.dma("gpsimd", (lambda t, k, yt, b: lambda e: e.indirect_dma_start(
                        out=yt[b][:, :], out_offset=None, in_=ybuf[:, :],
                        in_offset=bass.IndirectOffsetOnAxis(ap=desti[:, t, k:k + 1], axis=0)))(t, k, yt, b),
                        "c_y%d_%d" % (k, b), ["ybuf%d" % q_ for q_ in range(NBLK)], ["y%d_%d" % (k, b)])
                DMA(P, "sync", xc[b][:], x1s[t * 128:(t + 1) * 128, :], "c_xc%d" % b, [], ["xc%d" % b])
                TS(P, "vector", f0[:], y0[b][:], wts[:, t, 0:1], ALU.mult, ["y0_%d" % b], ["f0"])
                STT(P, f1[:], y1[b][:], wts[:, t, 1:2], f0[:], ALU.mult, ALU.add, ["y1_%d" % b, "f0"], ["f1"])
                TT(P, "gpsimd", f2[:], f1[:], GT2b[:, r, :], ALU.mult, ["f1", "GT2b"], ["f0"])
                TT(P, "vector", xo_sb[b][:], f2[:], xc[b][:], ALU.add, ["f0", "xc%d" % b], ["xo%d" % b])
                DMA(P, "sync", xo[t * 128:(t + 1) * 128, :], xo_sb[b][:], "c_xo%d" % b, ["xo%d" % b], [])
            P.emit()
        sM.close()
    return nc


def post_consts():
    k = np.arange(128)
    tri128 = (k[:, None] < k[None, :]).astype(NPBF)
    e = np.arange(32)
    tri32 = (e[:, None] < e[None, :]).astype(NPBF)
    thr = np.ascontiguousarray(np.broadcast_to((128.0 * np.arange(36)).astype(np.float32), (128, 36)))
    biota = np.ascontiguousarray(np.broadcast_to(np.arange(NBLK, dtype=np.float32), (128, NBLK)))
    piota = (2.0 * np.arange(128, dtype=np.float32)).reshape(128, 1)
    sel = np.zeros((2, 2, 128), np.float32)
    sel[0, 0] = 1
    sel[1, 1] = 1
    q = np.arange(128)
    mlo = np.tile((k[:, None] >= q[None, :]).astype(NPBF), (1, 4))
    mhi = np.tile((k[:, None] <= q[None, :]).astype(NPBF), (1, 4))
    return dict(tri128=tri128, tri32=tri32, thr=thr, biota=biota, piota=piota, sel=sel), dict(mlo=mlo, mhi=mhi)


def post_inputs(inp, l, x, ctx, qkvs, mod):
    i = l // 2
    kind = "ab" if l % 2 == 0 else "c"
    common, masks = post_consts()
    common.update({
        "modi": np.ascontiguousarray(mod),
        "wout": (inp["w_out_ab"][i] if kind == "ab" else inp["w_out_c"][i]),
        "nffn": np.ascontiguousarray(np.broadcast_to(inp["norm_ffn"][l], (128, D))),
        "wr": np.ascontiguousarray(np.concatenate([inp["w_group"][l], inp["w_expert"][l]], axis=1)),
        "br": np.ascontiguousarray(np.broadcast_to(np.concatenate([inp["b_group"][l], inp["b_expert"][l]]), (128, 36))),
        "w1": inp["w1"][l], "w3": inp["w3"][l], "w2": inp["w2"][l],
    })
    maps = []
    if kind == "ab":
        common.update(masks)
        common["sink"] = np.ascontiguousarray(inp["sink_a"][i].reshape(1, 8))
        lat = [q[:OWN] for q in qkvs]
        cx = qkvs[0][OWN:]
        kb_all = np.concatenate([q_[:, 1152:1280] for q_ in lat] + [cx[:, 1152:1280]], axis=0)
        vb_all = np.concatenate([q_[:, 1408:1536] for q_ in lat] + [cx[:, 1408:1536]], axis=0)
        ktb = np.ascontiguousarray(kb_all.T)
        vb1 = np.concatenate([vb_all.reshape(NKT * 128, 2, 64), np.ones((NKT * 128, 2, 1), NPBF)], axis=-1)
        vb = np.ascontiguousarray(vb1.reshape(NKT, 128, 2, 65).transpose(1, 0, 2, 3))

        def qpad(qcols):
            qt = qcols.reshape(NT, 128, 2, 4, 64).transpose(4, 2, 0, 3, 1).reshape(64, 2, NT, 512)
            out = np.zeros((128, 2, NT, 512), NPBF)
            out[0:64, 0] = qt[:, 0]
            out[64:128, 1] = qt[:, 1]
            return out
        zero = np.zeros((128, 128), NPBF)
        for c in range(NCORES):
            q = qkvs[c]
            qta = qpad(q[:, 0:512])
            qtb = qpad(q[:, 512:1024])

            def halo(cols):
                left = qkvs[c - 1][OWN - 128:OWN, cols] if c > 0 else zero
                right = qkvs[c + 1][0:128, cols] if c < NCORES - 1 else zero
                return np.concatenate([left, q[:OWN, cols], right, cx[:, cols]], axis=0)
            ka = halo(slice(1024, 1152))
            va_ = halo(slice(1280, 1408))
            kta = np.ascontiguousarray(ka.T)
            va1 = np.concatenate([va_.reshape(2560, 2, 64), np.ones((2560, 2, 1), NPBF)], axis=-1)
            va = np.ascontiguousarray(va1.reshape(20, 128, 2, 65).transpose(1, 0, 2, 3))
            flags = np.ones((128, 2), np.float32)
            if c == 0:
                flags[:, 0] = 0
            if c == NCORES - 1:
                flags[:, 1] = 0
            m = dict(common)
            m.update(xin=np.ascontiguousarray(np.concatenate([x[c * OWN:(c + 1) * OWN], ctx], axis=0)),
                     qta=qta, qtb=qtb, kta=kta, va=va, ktb=ktb, vb=vb, flags=flags)
            maps.append(m)
    else:
        lam_init = 0.8 - 0.6 * math.exp(-0.3 * l)
        common["lamp"] = np.ascontiguousarray(np.broadcast_to(inp["lam_c"][i], (128, 4, 64)))
        common["laminit"] = np.full((128, 1), lam_init, np.float32)
        common["oml"] = np.full((128, 1), 1.0 - lam_init, np.float32)
        common["subln"] = np.ascontiguousarray(inp["subln_c"][i].reshape(128, 1))
        lat = [q[:OWN] for q in qkvs]
        cx = qkvs[0][OWN:]
        k_all = np.concatenate([q_[:, 1024:2048] for q_ in lat] + [cx[:, 1024:2048]], axis=0)
        v_all = np.concatenate([q_[:, 2048:3072] for q_ in lat] + [cx[:, 2048:3072]], axis=0)
        ktc = np.ascontiguousarray(k_all.reshape(NKT * 128, 8, 128).transpose(1, 2, 0))
        vc = np.ascontiguousarray(v_all.reshape(NKT, 128, 8, 128).transpose(2, 1, 0, 3))
        for c in range(NCORES):
            q = qkvs[c]
            qt = q[:, 0:1024].reshape(TOK, 8, 128).transpose(2, 1, 0)
            qtc = np.zeros((2, 128, 8, TOK), NPBF)
            qtc[0, 0:64] = qt[0:64]
            qtc[1, 64:128] = qt[64:128]
            m = dict(common)
            m.update(xin=np.ascontiguousarray(np.concatenate([x[c * OWN:(c + 1) * OWN], ctx], axis=0)),
                     qtc=qtc, ktc=ktc, vc=vc)
            maps.append(m)
    return maps


def run_layer(inp, l, x, ctx):
    kind = "ab" if l % 2 == 0 else "c"
    pre = get_prog(("pre", kind), lambda: build_pre(kind))
    res = run_bass_kernel_spmd(pre, pre_inputs(inp, l, x, ctx), core_ids=list(range(NCORES)))
    qkvs = [np.asarray(r["qkv"]) for r in res.results]
    mod = np.asarray(res.results[0]["modo"])
    post = get_prog(("post", kind), lambda: build_post(kind))
    res = run_bass_kernel_spmd(post, post_inputs(inp, l, x, ctx, qkvs, mod), core_ids=list(range(NCORES)))
    xo = [np.asarray(r["xo"]) for r in res.results]
    x_new = np.concatenate([o[:OWN] for o in xo], axis=0)
    ctx_new = xo[0][OWN:]
    return x_new, ctx_new


def kernel(**inputs):
    inp = {k: np.asarray(v) for k, v in inputs.items()}
    x = np.ascontiguousarray(inp["x"][0].astype(np.float32))
    ctx = np.ascontiguousarray(inp["ctx"][0].astype(np.float32))
    for l in range(4):
        x, ctx = run_layer(inp, l, x, ctx)
    return x[None].astype(np.float32)
```

```python
import contextlib
import math
import numpy as np
import ml_dtypes
import concourse.bass as bass
import concourse.mybir as mybir
from concourse.bass_utils import run_bass_kernel_spmd

F32 = mybir.dt.float32
BF16 = mybir.dt.bfloat16
I32 = mybir.dt.int32
AF = mybir.ActivationFunctionType
ALU = mybir.AluOpType
AX = mybir.AxisListType
NPBF = ml_dtypes.bfloat16

ENGS = ("tensor", "vector", "scalar", "gpsimd", "sync")
NCORES = 8
SEQ = 16384
D = 1024
CTX = 256
OWN = SEQ // NCORES
NT = (OWN + CTX) // 128
TOK = NT * 128
NKT = (SEQ + CTX) // 128
NBLK = 2 * NT + 32
EPS = 1e-6


class _Op:
    __slots__ = ("eng", "fn", "deps", "is_dma", "chan", "chan_val", "needs_inc", "inc_val")

    def __init__(self, eng, fn, is_dma=False, chan=None):
        self.eng = eng
        self.fn = fn
        self.deps = []
        self.is_dma = is_dma
        self.chan = chan
        self.chan_val = 0
        self.needs_inc = False
        self.inc_val = 0


class Prog:
    _uid = 0

    def __init__(self, nc):
        self.nc = nc
        self.ops = {e: [] for e in ENGS}
        self.last_write = {}
        self.reads_since = {}
        self.chan_count = {}

    def _add(self, op, reads, writes):
        deps = []
        for r in reads:
            w = self.last_write.get(r)
            if w is not None:
                deps.append(w)
        for r in writes:
            w = self.last_write.get(r)
            if w is not None:
                deps.append(w)
            deps.extend(self.reads_since.get(r, ()))
        seen = set()
        for d in deps:
            if d is op or id(d) in seen:
                continue
            seen.add(id(d))
            if (not d.is_dma) and (not op.is_dma) and d.eng == "tensor" and op.eng == "tensor":
                continue
            op.deps.append(d)
        for r in reads:
            self.reads_since.setdefault(r, []).append(op)
        for r in writes:
            self.last_write[r] = op
            self.reads_since[r] = []
        self.ops[op.eng].append(op)
        return op

    def op(self, eng, fn, reads=(), writes=()):
        return self._add(_Op(eng, fn), reads, writes)

    def dma(self, eng, fn, chan, reads=(), writes=()):
        o = _Op(eng, fn, is_dma=True, chan=chan)
        self.chan_count[chan] = self.chan_count.get(chan, 0) + 16
        o.chan_val = self.chan_count[chan]
        return self._add(o, reads, writes)

    def emit(self):
        nc = self.nc
        for e in ENGS:
            for o in self.ops[e]:
                for d in o.deps:
                    if not d.is_dma:
                        d.needs_inc = True
        for e in ENGS:
            c = 0
            for o in self.ops[e]:
                if (not o.is_dma) and o.needs_inc:
                    c += 1
                    o.inc_val = c
        chans = sorted(self.chan_count.keys(), key=str)
        prog = self
        with contextlib.ExitStack() as st:
            Prog._uid += 1
            u = Prog._uid
            esem = {e: st.enter_context(nc.semaphore("se%d_%s" % (u, e))) for e in ENGS if e != "sync"}
            csem = {c: st.enter_context(nc.semaphore("sc%d_%d" % (u, i))) for i, c in enumerate(chans)}
            block = st.enter_context(nc.Block())

            def make(ename):
                def body(eng):
                    waited = {}
                    for o in prog.ops[ename]:
                        for d in o.deps:
                            if d.is_dma:
                                key, val, sem = ("c", d.chan), d.chan_val, csem[d.chan]
                            else:
                                key, val, sem = ("e", d.eng), d.inc_val, esem[d.eng]
                            if waited.get(key, 0) >= val:
                                continue
                            waited[key] = val
                            eng.wait_ge(sem, val)
                        ins = o.fn(eng)
                        if o.is_dma:
                            ins.then_inc(csem[o.chan], 16)
                        elif o.needs_inc:
                            ins.then_inc(esem[ename], 1)
                    if ename == "sync":
                        for c in chans:
                            eng.wait_ge(csem[c], prog.chan_count[c])
                return body

            for e in ENGS:
                getattr(block, e)(make(e))


def MM(P, out, lhsT, rhs, start, stop, reads, writes):
    P.op("tensor", lambda e: e.matmul(out, lhsT=lhsT, rhs=rhs, start=start, stop=stop), reads, writes)


def TR(P, out, in_, ident, reads, writes):
    P.op("tensor", lambda e: e.transpose(out, in_, ident), reads, writes)


def ACT(P, out, in_, func, reads, writes, scale=None, bias=None, accum_out=None):
    kw = {}
    if scale is not None:
        kw["scale"] = scale
    if bias is not None:
        kw["bias"] = bias
    if accum_out is not None:
        kw["accum_out"] = accum_out
    P.op("scalar", lambda e: e.activation(out=out, in_=in_, func=func, **kw), reads, writes)


def TT(P, eng, out, in0, in1, op, reads, writes):
    P.op(eng, lambda e: e.tensor_tensor(out=out, in0=in0, in1=in1, op=op), reads, writes)


def TS(P, eng, out, in0, s1, op0, reads, writes, s2=None, op1=None):
    if op1 is None:
        P.op(eng, lambda e: e.tensor_scalar(out=out, in0=in0, scalar1=s1, scalar2=None, op0=op0), reads, writes)
    else:
        P.op(eng, lambda e: e.tensor_scalar(out=out, in0=in0, scalar1=s1, scalar2=s2, op0=op0, op1=op1), reads, writes)


def STT(P, out, in0, scalar, in1, op0, op1, reads, writes):
    P.op("vector", lambda e: e.scalar_tensor_tensor(out=out, in0=in0, scalar=scalar, in1=in1, op0=op0, op1=op1),
         reads, writes)


def CP(P, eng, out, in_, reads, writes):
    if eng == "scalar":
        P.op(eng, lambda e: e.copy(out=out, in_=in_), reads, writes)
    else:
        P.op(eng, lambda e: e.tensor_copy(out=out, in_=in_), reads, writes)


def RED(P, out, in_, op, reads, writes):
    P.op("vector", lambda e: e.tensor_reduce(out=out, in_=in_, axis=AX.X, op=op), reads, writes)


def RCP(P, out, in_, reads, writes):
    P.op("vector", lambda e: e.reciprocal(out=out, in_=in_), reads, writes)


def MSET(P, eng, ap, val, writes):
    P.op(eng, lambda e: e.memset(ap, val), (), writes)


def DMA(P, eng, out, in_, chan, reads, writes):
    P.dma(eng, lambda e: e.dma_start(out=out, in_=in_), chan, reads, writes)


def make_ident(P, nc, ident, idf):
    P.op("gpsimd", lambda e: e.iota(idf[:], pattern=[[1, 128]], base=0, channel_multiplier=-1,
                                     allow_small_or_imprecise_dtypes=True), (), ["idf"])
    TS(P, "vector", ident[:], idf[:], 0.0, ALU.is_equal, ["idf"], ["ident"])


def rms_rstd(P, x_ap, junk_ap, ss, rt, rstd, epst, n, rd, tag):
    ACT(P, junk_ap, x_ap, AF.Square, rd, ["junk" + tag, "ss" + tag], accum_out=ss[:, 0:1])
    ACT(P, rt[:, 0:1], ss[:, 0:1], AF.Sqrt, ["ss" + tag], ["rt" + tag], scale=1.0 / n, bias=epst[:, 0:1])
    RCP(P, rstd[:, 0:1], rt[:, 0:1], ["rt" + tag], ["rstd" + tag])


def mod_broadcast(P, dst_ap, psb, sel_sb, mod_sb, r, col0, mul_ap, reads_extra, wname, k, pname="psb"):
    for half in range(2):
        ps = psb[(k + half) % 2]
        pn = pname + "%d" % ((k + half) % 2)
        MM(P, ps[:], sel_sb[:, r, :], mod_sb[:, col0 + half * 512: col0 + (half + 1) * 512], True, True,
           ["sel", "mod"], [pn])
        d = dst_ap[:, half * 512:(half + 1) * 512]
        if mul_ap is None:
            CP(P, "scalar", d, ps[:], [pn], [wname])
        else:
            STT(P, d, ps[:], 1.0, mul_ap[:, half * 512:(half + 1) * 512], ALU.add, ALU.mult,
                [pn] + reads_extra, [wname])


def load_mod_bcast(nc, P, stack, modi, sel, col0, ncols, tag, psb=None):
    mod_sb = stack.enter_context(nc.sbuf_tensor("mod_sb" + tag, [2, ncols], F32))
    sel_sb = stack.enter_context(nc.sbuf_tensor("sel_sb" + tag, [2, 2, 128], F32))
    if psb is None:
        psb = [stack.enter_context(nc.psum_tensor("psb%s%d" % (tag, i), [128, 512], F32)) for i in range(2)]
    DMA(P, "sync", mod_sb[:], modi[:, col0:col0 + ncols], "c_mod", [], ["mod"])
    DMA(P, "sync", sel_sb[:], sel[:, :, :], "c_sel", [], ["sel"])
    return mod_sb, sel_sb, psb


def build_pre(kind):
    ncol = 1536 if kind == "ab" else 3072
    nnorm = 1280 if kind == "ab" else 2048
    G = nnorm // 64
    nc = bass.Bass("TRN2", target_bir_lowering=False)

    def din(name, shape, dt=F32):
        return nc.dram_tensor(name, shape, dt, kind="ExternalInput").ap()

    xin = din("xin", [TOK, D])
    scT = din("scT", [128, 8, 2])
    wmod = din("wmod", [D, 6 * D])
    bmod2 = din("bmod2", [2, 6 * D])
    nmix = din("nmix", [128, D])
    win = din("win", [D, ncol])
    gains = din("gains", [128, nnorm])
    cs = din("cs", [128, 16, 64])
    sel = din("sel", [2, 2, 128])
    qkv = nc.dram_tensor("qkv", [TOK, ncol], BF16, kind="ExternalOutput").ap()
    modo = nc.dram_tensor("modo", [2, 6 * D], F32, kind="ExternalOutput").ap()

    with contextlib.ExitStack() as st:
        def S(name, shape, dt):
            return st.enter_context(nc.sbuf_tensor(name, shape, dt))

        def PS(name, shape, dt):
            return st.enter_context(nc.psum_tensor(name, shape, dt))

        ident = S("ident", [128, 128], BF16)
        idf = S("idf", [128, 128], F32)
        win_sb = S("win_sb", [128, 8, ncol], BF16)
        Gb = S("Gb", [128, 2, D], F32)
        SHb = S("SHb", [128, 2, D], F32)
        gains_sb = S("gains_sb", [128, nnorm], F32)
        cs_sb = S("cs_sb", [128, 16, 64], F32)
        epst = S("epst", [128, 1], F32)
        st0 = contextlib.ExitStack()

        def S0(name, shape, dt):
            return st0.enter_context(nc.sbuf_tensor(name, shape, dt))

        def PS0(name, shape, dt):
            return st0.enter_context(nc.psum_tensor(name, shape, dt))

        mod_sb = S0("mod_sb", [2, 6 * D], F32)
        bm_sb = S0("bm_sb", [2, 6 * D], F32)
        sel_sb = S0("sel_sb", [2, 2, 128], F32)
        nmix_sb = S0("nmix_sb", [128, D], F32)
        sct = S0("sct", [128, 8, 2], F32)
        wmt = [S0("wm%d" % i, [128, 8, 512], F32) for i in range(2)]
        psm = [PS0("psm%d" % i, [2, 512], F32) for i in range(2)]
        psb = [PS0("psb%d" % i, [128, 512], F32) for i in range(2)]

        P = Prog(nc)
        make_ident(P, nc, ident, idf)
        MSET(P, "vector", epst[:], EPS, ["eps"])
        DMA(P, "sync", sct[:], scT[:, :, :], "c_sc", [], ["sct"])
        ACT(P, sct[:], sct[:], AF.Silu, ["sct"], ["sct"])
        DMA(P, "sync", bm_sb[:], bmod2[:, :], "c_bm", [], ["bm"])
        DMA(P, "sync", sel_sb[:], sel[:, :, :], "c_sel", [], ["sel"])
        DMA(P, "sync", nmix_sb[:], nmix[:, :], "c_nm", [], ["nmix"])
        DMA(P, "sync", gains_sb[:], gains[:, :], "c_gn", [], ["gains"])
        DMA(P, "sync", cs_sb[:], cs[:, :, :], "c_cs", [], ["cs"])
        for c in range(8):
            DMA(P, "gpsimd", win_sb[:, c, :], win[c * 128:(c + 1) * 128, :], "c_win", [], ["win%d" % c])
        wmod_v = wmod.rearrange("(c p) n -> p c n", p=128)
        for j in range(12):
            b = j % 2
            DMA(P, "sync", wmt[b][:], wmod_v[:, :, j * 512:(j + 1) * 512], "c_wm%d" % b, [], ["wm%d" % b])
            for c in range(8):
                MM(P, psm[b][:], sct[:, c, :], wmt[b][:, c, :], c == 0, c == 7, ["sct", "wm%d" % b], ["psm%d" % b])
            TT(P, "vector", mod_sb[:, j * 512:(j + 1) * 512], psm[b][:], bm_sb[:, j * 512:(j + 1) * 512], ALU.add,
               ["psm%d" % b, "bm"], ["mod"])
        DMA(P, "sync", modo[:, :], mod_sb[:], "c_mo", ["mod"], [])
        for r in range(2):
            mod_broadcast(P, SHb[:, r, :], psb, sel_sb, mod_sb, r, 0, None, [], "SHb", 0)
            mod_broadcast(P, Gb[:, r, :], psb, sel_sb, mod_sb, r, 1024, nmix_sb, ["nmix"], "Gb", 0)
        P.emit()
        st0.close()

        xt = [S("xt%d" % i, [128, D], F32) for i in range(2)]
        junk = S("junk", [128, D], BF16)
        ss = [S("ss%d" % i, [128, 1], F32) for i in range(2)]
        rt = [S("rt%d" % i, [128, 1], F32) for i in range(2)]
        rstd = [S("rstd%d" % i, [128, 1], F32) for i in range(2)]
        hb = [S("hb%d" % i, [128, D], BF16) for i in range(2)]
        hT = [S("hT%d" % i, [128, D], BF16) for i in range(2)]
        qf = [S("qf%d" % i, [128, ncol], F32) for i in range(2)]
        sqs = [S("sq%d" % i, [128, nnorm], F32) for i in range(2)]
        ssqs = [S("ssq%d" % i, [128, G], F32) for i in range(2)]
        rqs = [S("rq%d" % i, [128, G], F32) for i in range(2)]
        rq2s = [S("rq2%d" % i, [128, G], F32) for i in range(2)]
        t1 = S("t1", [128, G, 32], F32)
        t2 = S("t2", [128, G, 32], F32)
        t3 = S("t3", [128, G, 32], F32)
        t4 = S("t4", [128, G, 32], F32)
        ob = [S("ob%d" % i, [128, ncol], BF16) for i in range(2)]
        pT = PS("pT", [128, D], BF16)
        pq = [PS("pq%d" % i, [128, 512], F32) for i in range(3)]

        P = Prog(nc)
        nq_box = [0]

        def stage_a(t):
            b = t % 2
            r = 0 if t < 16 else 1
            X = "x%d" % b
            if t == 0:
                DMA(P, "sync", xt[0][:], xin[0:128, :], "c_x0", [], ["x0"])
            if t + 1 < NT:
                nb_ = (t + 1) % 2
                DMA(P, "sync", xt[nb_][:], xin[(t + 1) * 128:(t + 2) * 128, :], "c_x%d" % nb_, [], ["x%d" % nb_])
            rms_rstd(P, xt[b][:], junk[:], ss[b], rt[b], rstd[b], epst, D, [X], str(b))
            STT(P, xt[b][:], xt[b][:], rstd[b][:, 0:1], Gb[:, r, :], ALU.mult, ALU.mult, [X, "rstd%d" % b], [X])
            TT(P, "gpsimd", hb[b][:], xt[b][:], SHb[:, r, :], ALU.add, [X], ["hb%d" % b])
            for c in range(8):
                TR(P, pT[:, c * 128:(c + 1) * 128], hb[b][:, c * 128:(c + 1) * 128], ident[:], ["hb%d" % b], ["pT"])
            CP(P, "scalar", hT[b][:], pT[:], ["pT"], ["hT%d" % b])
            for jc in range(ncol // 512):
                pp = nq_box[0] % 3
                nq_box[0] += 1
                for c in range(8):
                    MM(P, pq[pp][:], hT[b][:, c * 128:(c + 1) * 128], win_sb[:, c, jc * 512:(jc + 1) * 512],
                       c == 0, c == 7, ["hT%d" % b], ["pq%d" % pp])
                CP(P, "scalar" if jc % 2 == 0 else "vector", qf[b][:, jc * 512:(jc + 1) * 512], pq[pp][:],
                   ["pq%d" % pp], ["qf%d" % b])
            QF = "qf%d" % b
            OB = "ob%d" % b

        def stage_b(t):
            b = t % 2
            QF = "qf%d" % b
            OB = "ob%d" % b
            sq, ssq, rq, rq2 = sqs[b], ssqs[b], rqs[b], rq2s[b]
            SQ, SSQ, RQ, RQ2 = "sq%d" % b, "ssq%d" % b, "rq%d" % b, "rq2%d" % b
            TT(P, "gpsimd", sq[:], qf[b][:, 0:nnorm], qf[b][:, 0:nnorm], ALU.mult, [QF], [SQ])
            RED(P, ssq[:], sq[:].rearrange("p (g d) -> p g d", d=64), ALU.add, [SQ], [SSQ])
            ACT(P, rq[:], ssq[:], AF.Sqrt, [SSQ], [RQ], scale=1.0 / 64, bias=epst[:, 0:1])
            RCP(P, rq2[:], rq[:], [RQ], [RQ2])
            qfv = qf[b][:, 0:nnorm].rearrange("p (g d) -> p g d", d=64)
            TT(P, "vector", qfv, qfv, rq2[:].unsqueeze(2).broadcast_to([128, G, 64]), ALU.mult, [QF, RQ2], [QF])
            if t < 16:
                TT(P, "gpsimd", qf[b][:, 0:nnorm], qf[b][:, 0:nnorm], gains_sb[:], ALU.mult, [QF], [QF])
                qv = qf[b][:, 0:nnorm].rearrange("p (g h d) -> p g h d", h=2, d=32)
                ov = ob[b][:, 0:nnorm].rearrange("p (g h d) -> p g h d", h=2, d=32)
                cosb = cs_sb[:, t, 0:32].unsqueeze(1).broadcast_to([128, G, 32])
                sinb = cs_sb[:, t, 32:64].unsqueeze(1).broadcast_to([128, G, 32])
                TT(P, "vector", t1[:], qv[:, :, 0, :], cosb, ALU.mult, [QF], ["t1"])
                TT(P, "vector", t2[:], qv[:, :, 1, :], sinb, ALU.mult, [QF], ["t2"])
                TT(P, "vector", ov[:, :, 0, :], t1[:], t2[:], ALU.subtract, ["t1", "t2"], [OB + "a"])
                TT(P, "gpsimd", t3[:], qv[:, :, 1, :], cosb, ALU.mult, [QF], ["t3"])
                TT(P, "gpsimd", t4[:], qv[:, :, 0, :], sinb, ALU.mult, [QF], ["t4"])
                TT(P, "gpsimd", ov[:, :, 1, :], t3[:], t4[:], ALU.add, ["t3", "t4"], [OB + "b"])
            else:
                TT(P, "gpsimd", ob[b][:, 0:nnorm], qf[b][:, 0:nnorm], gains_sb[:], ALU.mult, [QF], [OB + "a", OB + "b"])
            CP(P, "scalar", ob[b][:, nnorm:ncol], qf[b][:, nnorm:ncol], [QF], [OB + "c"])
            DMA(P, "sync", qkv[t * 128:(t + 1) * 128, :], ob[b][:], "c_o%d" % b, [OB + "a", OB + "b", OB + "c"], [])

        stage_a(0)
        for t in range(NT):
            if t + 1 < NT:
                stage_a(t + 1)
            stage_b(t)
        P.emit()
    return nc


_PROG_CACHE = {}


def get_prog(key, builder):
    if key not in _PROG_CACHE:
        _PROG_CACHE[key] = builder()
    return _PROG_CACHE[key]


def rope_tables():
    rows_n = SEQ // 64
    rows = np.repeat(np.arange(rows_n, dtype=np.float32), 64)
    cols = np.tile(np.arange(64, dtype=np.float32), rows_n)
    inv = (np.float32(10000.0) ** (-np.arange(0, 32, 2, dtype=np.float32) / np.float32(32))).astype(np.float32)
    ang = np.concatenate([rows[:, None] * inv, cols[:, None] * inv], axis=-1).astype(np.float32)
    return np.cos(ang).astype(np.float32), np.sin(ang).astype(np.float32)


def pre_inputs(inp, l, x, ctx):
    i = l // 2
    kind = "ab" if l % 2 == 0 else "c"
    cc = np.stack([inp["c"][0], inp["c_ctx"]]).astype(np.float32)
    scT = np.ascontiguousarray(cc.reshape(2, 8, 128).transpose(2, 1, 0))
    bmod2 = np.ascontiguousarray(np.broadcast_to(inp["b_mod"][l], (2, 6 * D)))
    nmix = np.ascontiguousarray(np.broadcast_to(inp["norm_mix"][l], (128, D)))
    if kind == "ab":
        w = inp["w_in_ab"][i]
        win = np.concatenate([w[:, 0:512], w[:, 768:1280], w[:, 512:640], w[:, 1280:1408], w[:, 640:768],
                              w[:, 1408:1536]], axis=1)
        g = np.concatenate([np.tile(inp["qn_a"][i], 8), np.tile(inp["qn_b"][i], 8), np.tile(inp["kn_a"][i], 2),
                            np.tile(inp["kn_b"][i], 2)])
    else:
        win = inp["w_in_c"][i]
        g = np.concatenate([np.tile(inp["qn_c"][i], 16), np.tile(inp["kn_c"][i], 16)])
    win = np.ascontiguousarray(win.astype(np.float32))
    gains = np.ascontiguousarray(np.broadcast_to(g.astype(np.float32), (128, g.shape[0])))
    cos, sin = rope_tables()
    sel = np.zeros((2, 2, 128), np.float32)
    sel[0, 0] = 1
    sel[1, 1] = 1
    maps = []
    for c in range(NCORES):
        sl = slice(c * OWN, (c + 1) * OWN)
        cs = np.concatenate([cos[sl], sin[sl]], axis=-1).reshape(16, 128, 64).transpose(1, 0, 2)
        maps.append({
            "xin": np.ascontiguousarray(np.concatenate([x[sl], ctx], axis=0)),
            "scT": scT, "wmod": inp["w_mod"][l], "bmod2": bmod2, "nmix": nmix, "win": win, "gains": gains,
            "cs": np.ascontiguousarray(cs), "sel": sel,
        })
    return maps


class _Cnt:
    def __init__(self):
        self.d = {}

    def nxt(self, k, n):
        v = self.d.get(k, 0)
        self.d[k] = v + 1
        return v % n


def build_post(kind):
    ab = kind == "ab"
    nc = bass.Bass("TRN2", target_bir_lowering=False)

    def din(name, shape, dt=F32):
        return nc.dram_tensor(name, shape, dt, kind="ExternalInput").ap()

    xin = din("xin", [TOK, D])
    modi = din("modi", [2, 6 * D])
    sel = din("sel", [2, 2, 128])
    wout = din("wout", [D, D])
    nffn = din("nffn", [128, D])
    wr = din("wr", [D, 36])
    br = din("br", [128, 36])
    w1 = din("w1", [32, D, 512])
    w3 = din("w3", [32, D, 512])
    w2 = din("w2", [32, 512, D])
    tri128 = din("tri128", [128, 128], BF16)
    tri32 = din("tri32", [32, 32], BF16)
    thr = din("thr", [128, 36])
    biota = din("biota", [128, NBLK])
    piota = din("piota", [128, 1])
    if ab:
        qta = din("qta", [128, 2, NT, 512], BF16)
        qtb = din("qtb", [128, 2, NT, 512], BF16)
        kta = din("kta", [128, 2560], BF16)
        va = din("va", [128, 20, 2, 65], BF16)
        ktb = din("ktb", [128, NKT * 128], BF16)
        vb = din("vb", [128, NKT, 2, 65], BF16)
        sink = din("sink", [1, 8])
        flags = din("flags", [128, 2])
        mlo = din("mlo", [128, 512], BF16)
        mhi = din("mhi", [128, 512], BF16)
    else:
        qtc = din("qtc", [2, 128, 8, TOK], BF16)
        ktc = din("ktc", [8, 128, NKT * 128], BF16)
        vc = din("vc", [8, 128, NKT, 128], BF16)
        lamp = din("lamp", [128, 4, 64])
        laminit = din("laminit", [128, 1])
        oml = din("oml", [128, 1])
        subln = din("subln", [128, 1])
    xo = nc.dram_tensor("xo", [TOK, D], F32, kind="ExternalOutput").ap()
    x1s = nc.dram_tensor("x1s", [TOK, D], F32, kind="Internal").ap()
    buf = nc.dram_tensor("buf", [NBLK * 128, D], BF16, kind="Internal").ap()
    ybuf = nc.dram_tensor("ybuf", [NBLK * 128, D], F32, kind="Internal").ap()
    h2s = nc.dram_tensor("h2s", [TOK, D], BF16, kind="Internal").ap()

    with contextlib.ExitStack() as st:
        def S(name, shape, dt):
            return st.enter_context(nc.sbuf_tensor(name, shape, dt))

        ident = S("ident", [128, 128], BF16)
        identf = S("identf", [128, 128], F32)
        onesf = S("onesf", [128, 128], F32)
        epst = S("epst", [128, 1], F32)

        if True:
            P = Prog(nc)
            P.op("gpsimd", lambda e: e.iota(identf[:], pattern=[[1, 128]], base=0, channel_multiplier=-1,
                                             allow_small_or_imprecise_dtypes=True), (), ["idf0"])
            TS(P, "vector", ident[:], identf[:], 0.0, ALU.is_equal, ["idf0"], ["ident"])
            TS(P, "vector", identf[:], identf[:], 0.0, ALU.is_equal, ["idf0", "ident"], ["idf0"])
            MSET(P, "vector", epst[:], EPS, ["eps"])
            MSET(P, "vector", onesf[:], 1.0, ["onesf"])
            P.emit()

        sM = contextlib.ExitStack()
        M0b = sM.enter_context(nc.sbuf_tensor("M0b", [128, NT, 32], BF16))
        M1b = sM.enter_context(nc.sbuf_tensor("M1b", [128, NT, 32], BF16))
        wts = sM.enter_context(nc.sbuf_tensor("wts", [128, NT, 2], F32))
        desti = sM.enter_context(nc.sbuf_tensor("desti", [128, NT, 2], I32))
        idxA = sM.enter_context(nc.sbuf_tensor("idxA", [128, NBLK], I32))
        idxB = sM.enter_context(nc.sbuf_tensor("idxB", [128, NBLK], I32))
        sA = contextlib.ExitStack()
        if ab:
            oT = sA.enter_context(nc.sbuf_tensor("oT", [64, 16, TOK], BF16))
        else:
            oT = sA.enter_context(nc.sbuf_tensor("oT", [128, 8, TOK], BF16))

        with contextlib.ExitStack() as s1:
            def S1(name, shape, dt):
                return s1.enter_context(nc.sbuf_tensor(name, shape, dt))

            def PS1(name, shape, dt):
                return s1.enter_context(nc.psum_tensor(name, shape, dt))

            P = Prog(nc)
            cnt = _Cnt()
            if ab:
                kta_sb = S1("kta_sb", [128, 2560], BF16)
                va_sb = S1("va_sb", [128, 20, 2, 65], BF16)
                ktb_sb = S1("ktb_sb", [128, NKT * 128], BF16)
                vb_sb = S1("vb_sb", [128, NKT, 2, 65], BF16)
                q_sb = [S1("q_sb%d" % i, [128, 512], BF16) for i in range(2)]
                pt = [S1("pt%d" % i, [128, 512], BF16) for i in range(3)]
                ptm = [S1("ptm%d" % i, [128, 512], BF16) for i in range(2)]
                rrow = S1("rrow", [65, 512], F32)
                bc_sb = S1("bc_sb", [64, 512], F32)
                sink_sb = S1("sink_sb", [1, 8], F32)
                es_sb = S1("es_sb", [1, 8], F32)
                esrow = S1("esrow", [1, 2, 512], F32)
                e64 = S1("e64", [1, 65], F32)
                flags_sb = S1("flags_sb", [128, 2], F32)
                mlo_sb = S1("mlo_sb", [128, 512], BF16)
                mhi_sb = S1("mhi_sb", [128, 512], BF16)
                ps_s = [PS1("ps_s%d" % i, [128, 512], F32) for i in range(3)]
                ps_o = [PS1("ps_o%d" % i, [65, 512], F32) for i in range(2)]
                ps_b = PS1("ps_b", [64, 512], F32)

                DMA(P, "sync", kta_sb[:], kta[:, :], "c_kta", [], ["kta"])
                DMA(P, "sync", ktb_sb[:], ktb[:, :], "c_ktb", [], ["ktb"])
                DMA(P, "sync", vb_sb[:], vb[:, :, :, :], "c_vb", [], ["vb"])
                DMA(P, "sync", va_sb[:], va[:, :, :, :], "c_va", [], ["va"])
                DMA(P, "sync", sink_sb[:], sink[:, :], "c_sink", [], ["sink"])
                DMA(P, "sync", flags_sb[:], flags[:, :], "c_fl", [], ["flags"])
                DMA(P, "sync", mlo_sb[:], mlo[:, :], "c_mlo", [], ["mlo"])
                DMA(P, "sync", mhi_sb[:], mhi[:, :], "c_mhi", [], ["mhi"])
                ACT(P, es_sb[:], sink_sb[:], AF.Exp, ["sink"], ["es"])
                for kvh in range(2):
                    CP(P, "vector", esrow[0:1, kvh, :].rearrange("o (g q) -> o g q", q=128),
                       es_sb[0:1, kvh * 4:(kvh + 1) * 4].unsqueeze(2).broadcast_to([1, 4, 128]), ["es"], ["esrow"])
                MSET(P, "vector", e64[:], 0.0, ["e64"])
                MSET(P, "vector", e64[0:1, 64:65], 1.0, ["e64"])

                units = []
                for kvh in range(2):
                    for t in range(NT):
                        def key(kt, m=None, f=None, kvh=kvh):
                            return (kta_sb[:, kt * 128:(kt + 1) * 128], "kta", va_sb[:, kt, kvh, :], "va", m, f)
                        if t < 16:
                            keys = [key(t, mlo_sb[:], flags_sb[:, 0:1] if t == 0 else None), key(t + 1),
                                    key(t + 2, mhi_sb[:], flags_sb[:, 1:2] if t == 15 else None), key(18), key(19)]
                        else:
                            keys = [key(18), key(19)]
                        units.append((qta[:, kvh, t, :], keys, esrow[0:1, kvh, :],
                                      oT[:, kvh * 4:(kvh + 1) * 4, t * 128:(t + 1) * 128]))
                for kvh in range(2):
                    for t in range(NT):
                        kts = range(NKT) if t < 16 else (NKT - 2, NKT - 1)
                        keys = [(ktb_sb[:, kt * 128:(kt + 1) * 128], "ktb", vb_sb[:, kt, kvh, :], "vb", None, None)
                                for kt in kts]
                        units.append((qtb[:, kvh, t, :], keys, None,
                                      oT[:, 8 + kvh * 4:8 + (kvh + 1) * 4, t * 128:(t + 1) * 128]))
                flat = [(ui, i) for ui, u in enumerate(units) for i in range(len(u[1]))]
                LA = 2

                def issue_S(j):
                    ui, i = flat[j]
                    q_src, keys, _, _ = units[ui]
                    qb = ui % 2
                    QN = "q%d" % qb
                    if i == 0:
                        DMA(P, "sync", q_sb[qb][:], q_src, "c_q%d" % qb, [], [QN])
                    si = j % 3
                    MM(P, ps_s[si][:], keys[i][0], q_sb[qb][:], True, True, [QN, keys[i][1]], ["ps_s%d" % si])

                def issue_rest(j):
                    ui, i = flat[j]
                    q_src, keys, sink_rhs, out_ap = units[ui]
                    kT_ap, kname, v_ap, vname, mask_ap, flag_ap = keys[i]
                    n = len(keys)
                    si = j % 3
                    oi = ui % 2
                    ON = "ps_o%d" % oi
                    ACT(P, pt[si][:], ps_s[si][:], AF.Exp, ["ps_s%d" % si], ["pt%d" % si], scale=0.125)
                    rhs, rn = pt[si][:], "pt%d" % si
                    if mask_ap is not None:
                        mi = cnt.nxt("m", 2)
                        if flag_ap is not None:
                            STT(P, ptm[mi][:], pt[si][:], flag_ap, mask_ap, ALU.mult, ALU.mult,
                                [rn, "flags", "mlo", "mhi"], ["ptm%d" % mi])
                        else:
                            TT(P, "vector", ptm[mi][:], pt[si][:], mask_ap, ALU.mult, [rn, "mlo", "mhi"],
                               ["ptm%d" % mi])
                        rhs, rn = ptm[mi][:], "ptm%d" % mi
                    MM(P, ps_o[oi][:], v_ap, rhs, i == 0, (i == n - 1) and sink_rhs is None, [rn, vname], [ON])
                    if i == n - 1:
                        if sink_rhs is not None:
                            MM(P, ps_o[oi][:], e64[0:1, :], sink_rhs, False, True, ["e64", "esrow"], [ON])
                        RCP(P, rrow[64:65, :], ps_o[oi][64:65, :], [ON], ["rrow"])
                        MM(P, ps_b[:], onesf[64:65, 0:64], rrow[64:65, :], True, True, ["rrow", "onesf"], ["ps_b"])
                        CP(P, "scalar", bc_sb[:], ps_b[:], ["ps_b"], ["bc"])
                        TT(P, "vector", out_ap, ps_o[oi][0:64, :].rearrange("d (g q) -> d g q", q=128),
                           bc_sb[:].rearrange("d (g q) -> d g q", q=128), ALU.mult, [ON, "bc"], ["oT"])

                for j in range(min(LA, len(flat))):
                    issue_S(j)
                for j in range(len(flat)):
                    if j + LA < len(flat):
                        issue_S(j + LA)
                    issue_rest(j)
            else:
                kt_sbs = [S1("kt_sb%d" % i, [128, NKT * 128], BF16) for i in range(2)]
                v_sbs = [S1("v_sb%d" % i, [128, NKT, 128], BF16) for i in range(2)]
                q1_sb = [S1("q1_sb%d" % i, [128, 512], BF16) for i in range(2)]
                q2_sb = [S1("q2_sb%d" % i, [128, 512], BF16) for i in range(2)]
                p1 = [S1("p1_%d" % i, [128, 512], BF16) for i in range(3)]
                p2 = [S1("p2_%d" % i, [128, 512], BF16) for i in range(3)]
                E0 = S1("E0", [128, 128], F32)
                E1 = S1("E1", [128, 128], BF16)
                acc1 = S1("acc1", [128, 512], F32)
                acc2 = S1("acc2", [128, 512], F32)
                rr = S1("rr", [33, 512], F32)
                b1 = S1("b1", [128, 512], F32)
                o1s = S1("o1s", [128, 512], F32)
                o2s = S1("o2s", [128, 512], F32)
                sums_sb = S1("sums_sb", [33, 512], F32)
                Os = S1("Os", [128, 512], F32)
                sqo = S1("sqo", [128, 512], F32)
                rs = S1("rs", [128, 512], F32)
                rs2 = S1("rs2", [128, 512], F32)
                lamp_sb = S1("lamp_sb", [128, 4, 64], F32)
                lp = S1("lp", [128, 2, 64], F32)
                s2 = S1("s2", [128, 2], F32)
                e2 = S1("e2", [128, 2], F32)
                lamv = S1("lamv", [128, 1], F32)
                nlam = S1("nlam", [128, 1], F32)
                li_sb = S1("li_sb", [128, 1], F32)
                oml_sb = S1("oml_sb", [128, 1], F32)
                sub_sb = S1("sub_sb", [128, 1], F32)
                subs = S1("subs", [128, 1], F32)
                ps_s1 = [PS1("ps_s1_%d" % i, [128, 512], F32) for i in range(2)]
                ps_s2 = [PS1("ps_s2_%d" % i, [128, 512], F32) for i in range(2)]
                ps_o1 = PS1("ps_o1", [128, 512], F32)
                ps_o2 = PS1("ps_o2", [128, 512], F32)
                ps_sum = PS1("ps_sum", [128, 512], F32)
                ps_x = PS1("ps_x", [128, 512], F32)

                DMA(P, "sync", lamp_sb[:], lamp[:, :, :], "c_lamp", [], ["lamp"])
                DMA(P, "sync", li_sb[:], laminit[:, :], "c_li", [], ["li"])
                DMA(P, "sync", oml_sb[:], oml[:, :], "c_oml", [], ["oml"])
                DMA(P, "sync", sub_sb[:], subln[:, :], "c_sub", [], ["sub"])
                TT(P, "vector", lp[:, 0, :], lamp_sb[:, 0, :], lamp_sb[:, 1, :], ALU.mult, ["lamp"], ["lp"])
                TT(P, "vector", lp[:, 1, :], lamp_sb[:, 2, :], lamp_sb[:, 3, :], ALU.mult, ["lamp"], ["lp"])
                RED(P, s2[:], lp[:], ALU.add, ["lp"], ["s2"])
                ACT(P, e2[:], s2[:], AF.Exp, ["s2"], ["e2"])
                TT(P, "vector", lamv[:], e2[:, 1:2], e2[:, 0:1], ALU.subtract, ["e2"], ["lamv"])
                TT(P, "vector", nlam[:], lamv[:], li_sb[:], ALU.subtract, ["lamv", "li"], ["nlam"])
                TT(P, "vector", subs[:], sub_sb[:], oml_sb[:], ALU.mult, ["sub", "oml"], ["subs"])
                MSET(P, "vector", E0[:], 0.0, ["E0"])
                MSET(P, "vector", E0[:, 0:1], 1.0, ["E0"])
                MSET(P, "vector", E1[:], 0.0, ["E1"])
                MSET(P, "vector", E1[:, 32:33], 1.0, ["E1"])
                items = []
                for h in range(8):
                    for u in range(5):
                        tok0, N = (u * 512, 512) if u < 4 else (OWN, CTX)
                        kts = list(range(NKT)) if u < 4 else [NKT - 2, NKT - 1]
                        for i, kt in enumerate(kts):
                            items.append((h, u, tok0, N, kt, i, len(kts)))

                def kv_load(h):
                    DMA(P, "sync", kt_sbs[h % 2][:], ktc[h, :, :], "c_kt%d" % (h % 2), [], ["kt%d" % (h % 2)])
                    DMA(P, "sync", v_sbs[h % 2][:], vc[h, :, :, :], "c_v%d" % (h % 2), [], ["v%d" % (h % 2)])

                def issue_S(j):
                    h, u, tok0, N, kt, i, n = items[j]
                    qb = (h * 5 + u) % 2
                    QN = "q%d" % qb
                    if i == 0:
                        if u == 0 and h == 0:
                            kv_load(0)
                            kv_load(1)
                        DMA(P, "sync", q1_sb[qb][:, 0:N], qtc[0, :, h, tok0:tok0 + N], "c_q%d" % qb, [], [QN])
                        DMA(P, "sync", q2_sb[qb][:, 0:N], qtc[1, :, h, tok0:tok0 + N], "c_q%d" % qb, [], [QN])
                    b = j % 2
                    ksl = slice(kt * 128, (kt + 1) * 128)
                    kt_sb = kt_sbs[h % 2]
                    KT = "kt%d" % (h % 2)
                    MM(P, ps_s1[b][:, 0:N], kt_sb[:, ksl], q1_sb[qb][:, 0:N], True, True, [QN, KT], ["ps_s1_%d" % b])
                    MM(P, ps_s2[b][:, 0:N], kt_sb[:, ksl], q2_sb[qb][:, 0:N], True, True, [QN, KT], ["ps_s2_%d" % b])

                def issue_rest(j):
                    h, u, tok0, N, kt, i, n = items[j]
                    b = j % 2
                    v_sb = v_sbs[h % 2]
                    VN = "v%d" % (h % 2)
                    pb = j % 3
                    ACT(P, p1[pb][:, 0:N], ps_s1[b][:, 0:N], AF.Exp, ["ps_s1_%d" % b], ["p1_%d" % pb], scale=0.125)
                    ACT(P, p2[pb][:, 0:N], ps_s2[b][:, 0:N], AF.Exp, ["ps_s2_%d" % b], ["p2_%d" % pb], scale=0.125)
                    MM(P, ps_o2[:, 0:N], v_sb[:, kt, :], p2[pb][:, 0:N], i == 0, i == n - 1, [VN, "p2_%d" % pb], ["ps_o2"])
                    MM(P, ps_o1[:, 0:N], v_sb[:, kt, :], p1[pb][:, 0:N], i == 0, i == n - 1, [VN, "p1_%d" % pb], ["ps_o1"])
                    MM(P, ps_sum[:, 0:N], E1[:], p2[pb][:, 0:N], i == 0, False, ["E1", "p2_%d" % pb], ["ps_sum"])
                    if i == 0:
                        CP(P, "vector", acc1[:, 0:N], p1[pb][:, 0:N], ["p1_%d" % pb], ["acc1"])
                    else:
                        TT(P, "vector", acc1[:, 0:N], acc1[:, 0:N], p1[pb][:, 0:N], ALU.add, ["p1_%d" % pb, "acc1"], ["acc1"])
                    if i != n - 1:
                        return
                    MM(P, ps_sum[:, 0:N], E0[:], acc1[:, 0:N], False, True, ["E0", "acc1"], ["ps_sum"])
                    if u == 4 and 1 <= h + 1 < 7:
                        kv_load(h + 2)
                    flush()
                    CP(P, "vector", sums_sb[0:33, 0:N], ps_sum[0:33, 0:N], ["ps_sum"], ["sums_sb"])
                    CP(P, "vector", o1s[:, 0:N], ps_o1[:, 0:N], ["ps_o1"], ["o1s"])
                    CP(P, "scalar", o2s[:, 0:N], ps_o2[:, 0:N], ["ps_o2"], ["o2s"])
                    pending.append([8, (lambda h=h, tok0=tok0, N=N: finalize(h, tok0, N))])

                def finalize(h, tok0, N):
                    RCP(P, rr[0:33, 0:N], sums_sb[0:33, 0:N], ["sums_sb"], ["rr"])
                    TS(P, "vector", rr[32:33, 0:N], rr[32:33, 0:N], nlam[32:33, 0:1], ALU.mult, ["rr", "nlam"], ["rr"])
                    MM(P, ps_x[:, 0:N], onesf[0:1, :], rr[0:1, 0:N], True, True, ["rr", "onesf"], ["ps_x"])
                    CP(P, "scalar", b1[:, 0:N], ps_x[:, 0:N], ["ps_x"], ["b1"])
                    TT(P, "vector", o1s[:, 0:N], o1s[:, 0:N], b1[:, 0:N], ALU.mult, ["o1s", "b1"], ["o1s"])
                    MM(P, ps_x[:, 0:N], onesf[32:33, :], rr[32:33, 0:N], True, True, ["rr", "onesf"], ["ps_x"])
                    CP(P, "scalar", b1[:, 0:N], ps_x[:, 0:N], ["ps_x"], ["b1"])
                    TT(P, "vector", o2s[:, 0:N], o2s[:, 0:N], b1[:, 0:N], ALU.mult, ["o2s", "b1"], ["o2s"])
                    TT(P, "gpsimd", Os[:, 0:N], o1s[:, 0:N], o2s[:, 0:N], ALU.add, ["o1s", "o2s"], ["Os"])
                    TT(P, "gpsimd", sqo[:, 0:N], Os[:, 0:N], Os[:, 0:N], ALU.mult, ["Os"], ["sqo"])
                    MM(P, ps_x[:, 0:N], onesf[:], sqo[:, 0:N], True, True, ["sqo", "onesf"], ["ps_x"])
                    ACT(P, rs[:, 0:N], ps_x[:, 0:N], AF.Sqrt, ["ps_x"], ["rs"], scale=1.0 / 128, bias=epst[:, 0:1])
                    RCP(P, rs2[:, 0:N], rs[:, 0:N], ["rs"], ["rs2"])
                    STT(P, oT[:, h, tok0:tok0 + N], Os[:, 0:N], subs[:, 0:1], rs2[:, 0:N], ALU.mult, ALU.mult,
                        ["Os", "subs", "rs2"], ["oT"])

                pending = []

                def tick():
                    for p_ in pending:
                        p_[0] -= 1
                    while pending and pending[0][0] <= 0:
                        pending.pop(0)[1]()

                def flush():
                    while pending:
                        pending.pop(0)[1]()

                issue_S(0)
                for j in range(len(items)):
                    if j + 1 < len(items):
                        issue_S(j + 1)
                    tick()
                    issue_rest(j)
                flush()
            P.emit()

        with contextlib.ExitStack() as s2_:
            def S2(name, shape, dt):
                return s2_.enter_context(nc.sbuf_tensor(name, shape, dt))

            def PS2(name, shape, dt):
                return s2_.enter_context(nc.psum_tensor(name, shape, dt))

            KC = 16 if ab else 8
            KP = 64 if ab else 128
            GT1b = S2("GT1b", [128, 2, D], F32)
            SH2b = S2("SH2b", [128, 2, D], F32)
            G2b = S2("G2b", [128, 2, D], F32)
            nffn_sb = S2("nffn_sb", [128, D], F32)
            wout_sb = S2("wout_sb", [KP, KC, D], BF16)
            wr_sb = S2("wr_sb", [128, 8, 36], F32)
            br_sb = S2("br_sb", [128, 36], F32)
            xt = [S2("xt%d" % i, [128, D], F32) for i in range(2)]
            x1 = [S2("x1_%d" % i, [128, D], F32) for i in range(2)]
            junk = S2("junk", [128, D], BF16)
            ss = [S2("ss%d" % i, [128, 1], F32) for i in range(2)]
            rt = [S2("rt%d" % i, [128, 1], F32) for i in range(2)]
            rstd = [S2("rstd%d" % i, [128, 1], F32) for i in range(2)]
            h2f = [S2("h2f%d" % i, [128, D], F32) for i in range(2)]
            h2Ts = [S2("h2T%d" % i, [128, D], F32) for i in range(2)]
            h2bt = [S2("h2bt%d" % i, [128, D], BF16) for i in range(2)]
            lgs = [S2("lg%d" % i, [128, 36], F32) for i in range(2)]
            gmax = S2("gmax", [128, 1], F32)
            ngmax = S2("ngmax", [128, 1], F32)
            gm = S2("gm", [128, 4], F32)
            ge = S2("ge", [128, 4], F32)
            gsum = S2("gsum", [128, 1], F32)
            gp = S2("gp", [128, 1], F32)
            pen = S2("pen", [128, 4], F32)
            lem = S2("lem", [128, 32], F32)
            m8 = S2("m8", [128, 8], F32)
            dm = S2("dm", [128, 1], F32)
            rr_ = S2("rr_", [128, 1], F32)
            den = S2("den", [128, 1], F32)
            rden = S2("rden", [128, 1], F32)
            pq = [PS2("pq%d" % i, [128, 512], F32) for i in range(2)]
            pTf = PS2("pTf", [128, D], F32)
            ps_lg = PS2("ps_lg", [128, 36], F32)

            P = Prog(nc)
            mod_sb, sel_sb, psb = load_mod_bcast(nc, P, s2_, modi, sel, 2048, 3072, "o")
            DMA(P, "sync", nffn_sb[:], nffn[:, :], "c_nf", [], ["nffn"])
            for r in range(2):
                mod_broadcast(P, GT1b[:, r, :], psb, sel_sb, mod_sb, r, 0, None, [], "GT1b", 0)
                mod_broadcast(P, SH2b[:, r, :], psb, sel_sb, mod_sb, r, 1024, None, [], "SH2b", 0)
                mod_broadcast(P, G2b[:, r, :], psb, sel_sb, mod_sb, r, 2048, nffn_sb, ["nffn"], "G2b", 0)
            wv = wout.rearrange("(c p) n -> p c n", p=KP)
            for c in range(KC):
                DMA(P, "gpsimd", wout_sb[:, c, :], wv[:, c, :], "c_wo", [], ["wout%d" % c])
            DMA(P, "sync", wr_sb[:], wr.rearrange("(c p) n -> p c n", p=128), "c_wr", [], ["wr"])
            DMA(P, "sync", br_sb[:], br[:, :], "c_br", [], ["br"])
            def stage1(t):
                b = t % 2
                r = 0 if t < 16 else 1
                X, X1, H2 = "x%d" % b, "x1_%d" % b, "h2f%d" % b
                if t == 0:
                    DMA(P, "sync", xt[0][:], xin[0:128, :], "c_x0", [], ["x0"])
                if t + 1 < NT:
                    nb_ = (t + 1) % 2
                    DMA(P, "sync", xt[nb_][:], xin[(t + 1) * 128:(t + 2) * 128, :], "c_x%d" % nb_, [], ["x%d" % nb_])
                for half in range(2):
                    hs = slice(half * 512, (half + 1) * 512)
                    for c in range(KC):
                        MM(P, pq[half][:], oT[:, c, t * 128:(t + 1) * 128], wout_sb[:, c, hs], c == 0, c == KC - 1,
                           ["wout%d" % c], ["pq%d" % half])
                    TT(P, "vector", x1[b][:, hs], pq[half][:], GT1b[:, r, hs], ALU.mult, ["pq%d" % half, "GT1b"], [X1])
                TT(P, "gpsimd", x1[b][:], x1[b][:], xt[b][:], ALU.add, [X1, X], [X1])
                DMA(P, "sync", x1s[t * 128:(t + 1) * 128, :], x1[b][:], "c_x1s%d" % b, [X1], ["x1s%d" % t])
                rms_rstd(P, x1[b][:], junk[:], ss[b], rt[b], rstd[b], epst, D, [X1], str(b))
                STT(P, h2f[b][:], x1[b][:], rstd[b][:, 0:1], G2b[:, r, :], ALU.mult, ALU.mult, [X1, "rstd%d" % b, "G2b"], [H2])
                TT(P, "gpsimd", h2f[b][:], h2f[b][:], SH2b[:, r, :], ALU.add, [H2, "SH2b"], [H2])
                CP(P, "scalar", h2bt[b][:], h2f[b][:], [H2], ["h2bt%d" % b])
                DMA(P, "sync", h2s[t * 128:(t + 1) * 128, :], h2bt[b][:], "c_h2s%d" % b, ["h2bt%d" % b], ["h2s%d" % t])
                for c in range(8):
                    TR(P, pTf[:, c * 128:(c + 1) * 128], h2f[b][:, c * 128:(c + 1) * 128], identf[:], [H2], ["pTf"])
                h2T = h2Ts[b]
                CP(P, "vector", h2T[:], pTf[:], ["pTf"], ["h2T%d" % b])
                for c in range(8):
                    MM(P, ps_lg[:], h2T[:, c * 128:(c + 1) * 128], wr_sb[:, c, :], c == 0, c == 7, ["h2T%d" % b, "wr"], ["ps_lg"])
                TT(P, "vector", lgs[b][:], ps_lg[:], br_sb[:], ALU.add, ["ps_lg", "br"], ["lg%d" % b])

            def stage2(t):
                b = t % 2
                lg = lgs[b]
                RED(P, gmax[:], lg[:, 0:4], ALU.max, ["lg%d" % b], ["gmax"])
                TS(P, "vector", gm[:], lg[:, 0:4], gmax[:, 0:1], ALU.is_equal, ["lg%d" % b, "gmax"], ["gm"])
                TS(P, "vector", ngmax[:], gmax[:], -1.0, ALU.mult, ["gmax"], ["ngmax"])
                ACT(P, ge[:], lg[:, 0:4], AF.Exp, ["lg%d" % b, "ngmax"], ["ge", "gsum"], bias=ngmax[:, 0:1], accum_out=gsum[:, 0:1])
                RCP(P, gp[:], gsum[:], ["gsum"], ["gp"])
                TS(P, "vector", pen[:], gm[:], 1e30, ALU.mult, ["gm"], ["pen"], s2=-1e30, op1=ALU.add)
                TT(P, "vector", lem[:].rearrange("p (g e) -> p g e", e=8), lg[:, 4:36].rearrange("p (g e) -> p g e", e=8),
                   pen[:].unsqueeze(2).broadcast_to([128, 4, 8]), ALU.add, ["lg%d" % b, "pen"], ["lem"])
                P.op("vector", lambda e: e.max(out=m8[:], in_=lem[:]), ["lem"], ["m8"])
                TS(P, "vector", M0b[:, t, :], lem[:], m8[:, 0:1], ALU.is_equal, ["lem", "m8"], ["M0b"])
                TS(P, "vector", M1b[:, t, :], lem[:], m8[:, 1:2], ALU.is_equal, ["lem", "m8"], ["M1b"])
                TT(P, "vector", dm[:], m8[:, 1:2], m8[:, 0:1], ALU.subtract, ["m8"], ["dm"])
                ACT(P, rr_[:], dm[:], AF.Exp, ["dm"], ["rr_"])
                TS(P, "vector", den[:], rr_[:], 1.0, ALU.add, ["rr_"], ["den"])
                RCP(P, rden[:], den[:], ["den"], ["rden"])
                TT(P, "vector", wts[:, t, 0:1], rden[:], gp[:], ALU.mult, ["rden", "gp"], ["wts"])
                TT(P, "vector", wts[:, t, 1:2], wts[:, t, 0:1], rr_[:], ALU.mult, ["wts", "rr_"], ["wts"])

            stage1(0)
            for t in range(NT):
                if t + 1 < NT:
                    stage1(t + 1)
                stage2(t)
            P.emit()
        with contextlib.ExitStack() as s3:
            def S3(name, shape, dt):
                return s3.enter_context(nc.sbuf_tensor(name, shape, dt))

            def PS3(name, shape, dt):
                return s3.enter_context(nc.psum_tensor(name, shape, dt))

            tri_sb = S3("tri_sb", [128, 128], BF16)
            tri32_sb = S3("tri32_sb", [32, 32], BF16)
            ones_b = S3("ones_b", [128, 128], BF16)
            thr_sb = S3("thr_sb", [128, 36], F32)
            bio_sb = S3("bio_sb", [128, NBLK], F32)
            pio_sb = S3("pio_sb", [128, 1], F32)
            Ms = S3("Ms", [128, NT, 32], BF16)
            Cs = S3("Cs", [128, NT, 32], F32)
            cntf = S3("cntf", [128, 32], F32)
            cmp1 = S3("cmp1", [128, 32, 36], F32)
            nblk = S3("nblk", [128, 32], F32)
            nblkb = S3("nblkb", [128, 32], BF16)
            nbT = S3("nbT", [32, 128], BF16)
            Sx = S3("Sx", [128, 32], F32)
            pend = S3("pend", [128, 32], F32)
            base = S3("base", [128, 32], F32)
            tall = S3("tall", [128, NT, 32], F32)
            prod = S3("prod", [128, NT, 32], F32)
            destf = S3("destf", [128, NT, 2], F32)
            cmp2 = S3("cmp2", [128, NBLK, 32], F32)
            be = S3("be", [128, NBLK], F32)
            idxf = S3("idxf", [128, NBLK], F32)
            ps_c = [PS3("ps_c%d" % i, [128, 32], F32) for i in range(2)]
            ps_t = PS3("ps_t", [32, 128], BF16)
            ps_S = PS3("ps_S", [128, 32], F32)

            P = Prog(nc)
            DMA(P, "sync", tri_sb[:], tri128[:, :], "c_tri", [], ["tri"])
            DMA(P, "sync", tri32_sb[:], tri32[:, :], "c_tri32", [], ["tri32"])
            DMA(P, "sync", thr_sb[:], thr[:, :], "c_thr", [], ["thr"])
            DMA(P, "sync", bio_sb[:], biota[:, :], "c_bio", [], ["bio"])
            DMA(P, "sync", pio_sb[:], piota[:, :], "c_pio", [], ["pio"])
            MSET(P, "vector", ones_b[:], 1.0, ["ones_b"])
            TT(P, "vector", Ms[:], M0b[:], M1b[:], ALU.add, [], ["Ms"])
            for t in range(NT):
                pc = ps_c[t % 2]
                pn = "ps_c%d" % (t % 2)
                MM(P, pc[:], tri_sb[:], Ms[:, t, :], True, t == 0, ["tri", "Ms"], [pn])
                for i in range(t):
                    MM(P, pc[:], ones_b[:], Ms[:, i, :], False, i == t - 1, ["ones_b", "Ms"], [pn])
                CP(P, "vector", Cs[:, t, :], pc[:], [pn], ["Cs"])
            pc = ps_c[NT % 2]
            pn = "ps_c%d" % (NT % 2)
            for i in range(NT):
                MM(P, pc[:], ones_b[:], Ms[:, i, :], i == 0, i == NT - 1, ["ones_b", "Ms"], [pn])
            CP(P, "vector", cntf[:], pc[:], [pn], ["cntf"])
            TT(P, "vector", cmp1[:], cntf[:].unsqueeze(2).broadcast_to([128, 32, 36]),
               thr_sb[:].unsqueeze(1).broadcast_to([128, 32, 36]), ALU.is_gt, ["cntf", "thr"], ["cmp1"])
            RED(P, nblk[:], cmp1[:], ALU.add, ["cmp1"], ["nblk"])
            CP(P, "vector", nblkb[:], nblk[:], ["nblk"], ["nblkb"])
            TR(P, ps_t[:], nblkb[:], ident[:], ["nblkb"], ["ps_t"])
            CP(P, "vector", nbT[:], ps_t[:], ["ps_t"], ["nbT"])
            MM(P, ps_S[:], nbT[:], tri32_sb[:], True, True, ["nbT", "tri32"], ["ps_S"])
            CP(P, "vector", Sx[:], ps_S[:], ["ps_S"], ["Sx"])
            TT(P, "vector", pend[:], Sx[:], nblk[:], ALU.add, ["Sx", "nblk"], ["pend"])
            TS(P, "vector", base[:], Sx[:], 128.0, ALU.mult, ["Sx"], ["base"])
            TT(P, "vector", tall[:], Cs[:], base[:].unsqueeze(1).broadcast_to([128, NT, 32]), ALU.add, ["Cs", "base"], ["tall"])
            TT(P, "vector", prod[:], tall[:], M0b[:], ALU.mult, ["tall"], ["prod"])
            RED(P, destf[:, :, 0], prod[:], ALU.add, ["prod"], ["destf"])
            TT(P, "vector", prod[:], tall[:], M1b[:], ALU.mult, ["tall"], ["prod"])
            RED(P, destf[:, :, 1], prod[:], ALU.add, ["prod"], ["destf"])
            CP(P, "vector", desti[:], destf[:], ["destf"], ["desti"])
            TT(P, "vector", cmp2[:], pend[:].unsqueeze(1).broadcast_to([128, NBLK, 32]),
               bio_sb[:].unsqueeze(2).broadcast_to([128, NBLK, 32]), ALU.is_le, ["pend", "bio"], ["cmp2"])
            RED(P, be[:], cmp2[:], ALU.add, ["cmp2"], ["be"])
            TS(P, "vector", be[:], be[:], 31.0, ALU.min, ["be"], ["be"])
            TS(P, "vector", idxf[:], be[:], 256.0, ALU.mult, ["be", "pio"], ["idxf"], s2=pio_sb[:, 0:1], op1=ALU.add)
            CP(P, "vector", idxA[:], idxf[:], ["idxf"], ["idxA"])
            TS(P, "vector", idxf[:], idxf[:], 1.0, ALU.add, ["idxf", "idxA"], ["idxf"])
            CP(P, "vector", idxB[:], idxf[:], ["idxf"], ["idxB"])
            P.emit()

        sA.close()
        with contextlib.ExitStack() as s4:
            def S4(name, shape, dt):
                return s4.enter_context(nc.sbuf_tensor(name, shape, dt))

            def PS4(name, shape, dt):
                return s4.enter_context(nc.psum_tensor(name, shape, dt))

            NW = 3
            GT2b = S4("GT2b", [128, 2, D], F32)
            W1b = [S4("W1b%d" % i, [128, 4096], BF16) for i in range(NW)]
            W3b = [S4("W3b%d" % i, [128, 4096], BF16) for i in range(NW)]
            W2b = [S4("W2b%d" % i, [128, 4096], BF16) for i in range(NW)]
            xb = [S4("xb%d" % i, [128, D], BF16) for i in range(2)]
            xbT = [S4("xbT%d" % i, [128, 8, 128], BF16) for i in range(2)]
            s1t = S4("s1t", [128, 512], F32)
            gT = [S4("gT%d" % i, [128, 4, 128], BF16) for i in range(2)]
            ysb = [S4("ysb%d" % i, [128, D], F32) for i in range(2)]
            y0 = [S4("y0_%d" % i, [128, D], F32) for i in range(2)]
            y1 = [S4("y1_%d" % i, [128, D], F32) for i in range(2)]
            xc = [S4("xc%d" % i, [128, D], F32) for i in range(2)]
            f0 = S4("f0", [128, D], F32)
            f1 = S4("f1", [128, D], F32)
            f2 = f0
            xo_sb = [S4("xo_sb%d" % i, [128, D], F32) for i in range(2)]
            pxT = PS4("pxT", [128, 8, 128], BF16)
            ps1 = [PS4("ps1_%d" % i, [128, 4, 128], F32) for i in range(2)]
            ps3 = [PS4("ps3_%d" % i, [128, 4, 128], F32) for i in range(2)]
            psy = [PS4("psy%d" % i, [128, 512], F32) for i in range(2)]

            P = Prog(nc)
            for t in range(NT):
                b = t % 2
                DMA(P, "sync", xb[b][:], h2s[t * 128:(t + 1) * 128, :], "c_xb%d" % b, [], ["xb%d" % b])
                for k in range(2):
                    P.dma("gpsimd", (lambda t, k, b: lambda e: e.indirect_dma_start(
                        out=buf[:, :], out_offset=bass.IndirectOffsetOnAxis(ap=desti[:, t, k:k + 1], axis=0),
                        in_=xb[b][:, :], in_offset=None))(t, k, b), "c_sc%d" % b, ["xb%d" % b], ["buf_%d_%d" % (t, k)])
            w1v = w1.rearrange("e (p h c) f -> (e p h) (c f)", h=2, c=4)
            w3v = w3.rearrange("e (p h c) f -> (e p h) (c f)", h=2, c=4)
            w2v = w2.rearrange("e (p h c) n -> (e p h) (c n)", h=2, c=2)
            mod_sb, sel_sb, psb = load_mod_bcast(nc, P, s4, modi, sel, 5120, 1024, "m", psb=psy)
            for r in range(2):
                mod_broadcast(P, GT2b[:, r, :], psb, sel_sb, mod_sb, r, 0, None, [], "GT2b", 0, pname="psy")

            def stage_w(blk):
                wb = blk % NW
                for (wv_, Wt, nm) in ((w1v, W1b, "W1"), (w3v, W3b, "W3"), (w2v, W2b, "W2")):
                    for hh, idx in ((0, idxA), (1, idxB)):
                        P.dma("gpsimd", (lambda wv_, Wt, hh, idx, blk, wb: lambda e: e.indirect_dma_start(
                            out=Wt[wb][:, hh * 2048:(hh + 1) * 2048], out_offset=None, in_=wv_[:, :],
                            in_offset=bass.IndirectOffsetOnAxis(ap=idx[:, blk:blk + 1], axis=0)))(wv_, Wt, hh, idx, blk, wb),
                            "c_%s%d_%d" % (nm, wb, hh), [], ["%s%d_%d" % (nm, wb, hh)])

            def stage_a(blk):
                b = blk % 2
                wb = blk % NW
                DMA(P, "sync", xb[b][:], buf[blk * 128:(blk + 1) * 128, :], "c_xb%d" % b,
                    ["buf_%d_%d" % (t_, k_) for t_ in range(NT) for k_ in range(2)], ["xb%d" % b])
                xv = xb[b][:].rearrange("s (p c) -> s c p", c=8)
                for c in range(8):
                    TR(P, pxT[:, c, :], xv[:, c, :], ident[:], ["xb%d" % b], ["pxT"])
                CP(P, "vector", xbT[b][:], pxT[:], ["pxT"], ["xbT%d" % b])
                W1v = W1b[wb][:].rearrange("p (cc j q) -> p cc j q", cc=8, q=4)
                W3v = W3b[wb][:].rearrange("p (cc j q) -> p cc j q", cc=8, q=4)
                for cq in range(4):
                    for c in range(8):
                        MM(P, ps1[b][:, cq, :], W1v[:, c, :, cq], xbT[b][:, c, :], c == 0, c == 7,
                           ["W1%d_0" % wb, "W1%d_1" % wb, "xbT%d" % b], ["ps1_%d" % b])
                for cq in range(4):
                    for c in range(8):
                        MM(P, ps3[b][:, cq, :], W3v[:, c, :, cq], xbT[b][:, c, :], c == 0, c == 7,
                           ["W3%d_0" % wb, "W3%d_1" % wb, "xbT%d" % b], ["ps3_%d" % b])

            def stage_b(blk):
                b = blk % 2
                wb = blk % NW
                ACT(P, s1t[:], ps1[b][:].rearrange("p a s -> p (a s)"), AF.Silu, ["ps1_%d" % b], ["s1t"])
                TT(P, "vector", gT[b][:].rearrange("p a s -> p (a s)"), s1t[:], ps3[b][:].rearrange("p a s -> p (a s)"),
                   ALU.mult, ["s1t", "ps3_%d" % b], ["gT%d" % b])
                for half in range(2):
                    for cq in range(4):
                        MM(P, psy[half][:], gT[b][:, cq, :], W2b[wb][:, cq * 1024 + half * 512: cq * 1024 + (half + 1) * 512],
                           cq == 0, cq == 3, ["gT%d" % b, "W2%d_0" % wb, "W2%d_1" % wb], ["psy%d" % half])
                    CP(P, "scalar" if half == 0 else "vector", ysb[b][:, half * 512:(half + 1) * 512], psy[half][:],
                       ["psy%d" % half], ["ysb%d" % b])
                DMA(P, "sync", ybuf[blk * 128:(blk + 1) * 128, :], ysb[b][:], "c_yb%d" % b, ["ysb%d" % b], ["ybuf%d" % blk])

            stage_w(0)
            stage_w(1)
            stage_a(0)
            for blk in range(NBLK):
                if blk + 2 < NBLK:
                    stage_w(blk + 2)
                if blk + 1 < NBLK:
                    stage_a(blk + 1)
                stage_b(blk)
            for t in range(NT):
                b = t % 2
                r = 0 if t < 16 else 1
                for k, yt in ((0, y0), (1, y1)):
                    P.dma("gpsimd", (lambda t, k, yt, b: lambda e: e.indirect_dma_start(
                        out=yt[b][:, :], out_offset=None, in_=ybuf[:, :],
                        in_offset=bass.IndirectOffsetOnAxis(ap=desti[:, t, k:k + 1], axis=0)))(t, k, yt, b),
                        "c_y%d_%d" % (k, b), ["ybuf%d" % q_ for q_ in range(NBLK)], ["y%d_%d" % (k, b)])
                DMA(P, "sync", xc[b][:], x1s[t * 128:(t + 1) * 128, :], "c_xc%d" % b, [], ["xc%d" % b])
                TS(P, "vector", f0[:], y0[b][:], wts[:, t, 0:1], ALU.mult, ["y0_%d" % b], ["f0"])
                STT(P, f1[:], y1[b][:], wts[:, t, 1:2], f0[:], ALU.mult, ALU.add, ["y1_%d" % b, "f0"], ["f1"])
                TT(P, "gpsimd", f2[:], f1[:], GT2b[:, r, :], ALU.mult, ["f1", "GT2b"], ["f0"])
                TT(P, "vector", xo_sb[b][:], f2[:], xc[b][:], ALU.add, ["f0", "xc%d" % b], ["xo%d" % b])
                DMA(P, "sync", xo[t * 128:(t + 1) * 128, :], xo_sb[b][:], "c_xo%d" % b, ["xo%d" % b], [])
            P.emit()
        sM.close()
    return nc


def post_consts():
    k = np.arange(128)
    tri128 = (k[:, None] < k[None, :]).astype(NPBF)
    e = np.arange(32)
    tri32 = (e[:, None] < e[None, :]).astype(NPBF)
    thr = np.ascontiguousarray(np.broadcast_to((128.0 * np.arange(36)).astype(np.float32), (128, 36)))
    biota = np.ascontiguousarray(np.broadcast_to(np.arange(NBLK, dtype=np.float32), (128, NBLK)))
    piota = (2.0 * np.arange(128, dtype=np.float32)).reshape(128, 1)
    sel = np.zeros((2, 2, 128), np.float32)
    sel[0, 0] = 1
    sel[1, 1] = 1
    q = np.arange(128)
    mlo = np.tile((k[:, None] >= q[None, :]).astype(NPBF), (1, 4))
    mhi = np.tile((k[:, None] <= q[None, :]).astype(NPBF), (1, 4))
    return dict(tri128=tri128, tri32=tri32, thr=thr, biota=biota, piota=piota, sel=sel), dict(mlo=mlo, mhi=mhi)


def post_inputs(inp, l, x, ctx, qkvs, mod):
    i = l // 2
    kind = "ab" if l % 2 == 0 else "c"
    common, masks = post_consts()
    common.update({
        "modi": np.ascontiguousarray(mod),
        "wout": (inp["w_out_ab"][i] if kind == "ab" else inp["w_out_c"][i]),
        "nffn": np.ascontiguousarray(np.broadcast_to(inp["norm_ffn"][l], (128, D))),
        "wr": np.ascontiguousarray(np.concatenate([inp["w_group"][l], inp["w_expert"][l]], axis=1)),
        "br": np.ascontiguousarray(np.broadcast_to(np.concatenate([inp["b_group"][l], inp["b_expert"][l]]), (128, 36))),
        "w1": inp["w1"][l], "w3": inp["w3"][l], "w2": inp["w2"][l],
    })
    maps = []
    if kind == "ab":
        common.update(masks)
        common["sink"] = np.ascontiguousarray(inp["sink_a"][i].reshape(1, 8))
        lat = [q[:OWN] for q in qkvs]
        cx = qkvs[0][OWN:]
        kb_all = np.concatenate([q_[:, 1152:1280] for q_ in lat] + [cx[:, 1152:1280]], axis=0)
        vb_all = np.concatenate([q_[:, 1408:1536] for q_ in lat] + [cx[:, 1408:1536]], axis=0)
        ktb = np.ascontiguousarray(kb_all.T)
        vb1 = np.concatenate([vb_all.reshape(NKT * 128, 2, 64), np.ones((NKT * 128, 2, 1), NPBF)], axis=-1)
        vb = np.ascontiguousarray(vb1.reshape(NKT, 128, 2, 65).transpose(1, 0, 2, 3))

        def qpad(qcols):
            qt = qcols.reshape(NT, 128, 2, 4, 64).transpose(4, 2, 0, 3, 1).reshape(64, 2, NT, 512)
            out = np.zeros((128, 2, NT, 512), NPBF)
            out[0:64, 0] = qt[:, 0]
            out[64:128, 1] = qt[:, 1]
            return out
        zero = np.zeros((128, 128), NPBF)
        for c in range(NCORES):
            q = qkvs[c]
            qta = qpad(q[:, 0:512])
            qtb = qpad(q[:, 512:1024])

            def halo(cols):
                left = qkvs[c - 1][OWN - 128:OWN, cols] if c > 0 else zero
                right = qkvs[c + 1][0:128, cols] if c < NCORES - 1 else zero
                return np.concatenate([left, q[:OWN, cols], right, cx[:, cols]], axis=0)
            ka = halo(slice(1024, 1152))
            va_ = halo(slice(1280, 1408))
            kta = np.ascontiguousarray(ka.T)
            va1 = np.concatenate([va_.reshape(2560, 2, 64), np.ones((2560, 2, 1), NPBF)], axis=-1)
            va = np.ascontiguousarray(va1.reshape(20, 128, 2, 65).transpose(1, 0, 2, 3))
            flags = np.ones((128, 2), np.float32)
            if c == 0:
                flags[:, 0] = 0
            if c == NCORES - 1:
                flags[:, 1] = 0
            m = dict(common)
            m.update(xin=np.ascontiguousarray(np.concatenate([x[c * OWN:(c + 1) * OWN], ctx], axis=0)),
                     qta=qta, qtb=qtb, kta=kta, va=va, ktb=ktb, vb=vb, flags=flags)
            maps.append(m)
    else:
        lam_init = 0.8 - 0.6 * math.exp(-0.3 * l)
        common["lamp"] = np.ascontiguousarray(np.broadcast_to(inp["lam_c"][i], (128, 4, 64)))
        common["laminit"] = np.full((128, 1), lam_init, np.float32)
        common["oml"] = np.full((128, 1), 1.0 - lam_init, np.float32)
        common["subln"] = np.ascontiguousarray(inp["subln_c"][i].reshape(128, 1))
        lat = [q[:OWN] for q in qkvs]
        cx = qkvs[0][OWN:]
        k_all = np.concatenate([q_[:, 1024:2048] for q_ in lat] + [cx[:, 1024:2048]], axis=0)
        v_all = np.concatenate([q_[:, 2048:3072] for q_ in lat] + [cx[:, 2048:3072]], axis=0)
        ktc = np.ascontiguousarray(k_all.reshape(NKT * 128, 8, 128).transpose(1, 2, 0))
        vc = np.ascontiguousarray(v_all.reshape(NKT, 128, 8, 128).transpose(2, 1, 0, 3))
        for c in range(NCORES):
            q = qkvs[c]
            qt = q[:, 0:1024].reshape(TOK, 8, 128).transpose(2, 1, 0)
            qtc = np.zeros((2, 128, 8, TOK), NPBF)
            qtc[0, 0:64] = qt[0:64]
            qtc[1, 64:128] = qt[64:128]
            m = dict(common)
            m.update(xin=np.ascontiguousarray(np.concatenate([x[c * OWN:(c + 1) * OWN], ctx], axis=0)),
                     qtc=qtc, ktc=ktc, vc=vc)
            maps.append(m)
    return maps


def run_layer(inp, l, x, ctx):
    kind = "ab" if l % 2 == 0 else "c"
    pre = get_prog(("pre", kind), lambda: build_pre(kind))
    res = run_bass_kernel_spmd(pre, pre_inputs(inp, l, x, ctx), core_ids=list(range(NCORES)))
    qkvs = [np.asarray(r["qkv"]) for r in res.results]
    mod = np.asarray(res.results[0]["modo"])
    post = get_prog(("post", kind), lambda: build_post(kind))
    res = run_bass_kernel_spmd(post, post_inputs(inp, l, x, ctx, qkvs, mod), core_ids=list(range(NCORES)))
    xo = [np.asarray(r["xo"]) for r in res.results]
    x_new = np.concatenate([o[:OWN] for o in xo], axis=0)
    ctx_new = xo[0][OWN:]
    return x_new, ctx_new


def kernel(**inputs):
    inp = {k: np.asarray(v) for k, v in inputs.items()}
    x = np.ascontiguousarray(inp["x"][0].astype(np.float32))
    ctx = np.ascontiguousarray(inp["ctx"][0].astype(np.float32))
    for l in range(4):
        x, ctx = run_layer(inp, l, x, ctx)
    return x[None].astype(np.float32)
```

```python
import contextlib
import math
import numpy as np
import ml_dtypes
import concourse.bass as bass
import concourse.mybir as mybir
from concourse.bass_utils import run_bass_kernel_spmd

F32 = mybir.dt.float32
BF16 = mybir.dt.bfloat16
I32 = mybir.dt.int32
AF = mybir.ActivationFunctionType
ALU = mybir.AluOpType
AX = mybir.AxisListType
NPBF = ml_dtypes.bfloat16

ENGS = ("tensor", "vector", "scalar", "gpsimd", "sync")
NCORES = 8
SEQ = 16384
D = 1024
CTX = 256
OWN = SEQ // NCORES
NT = (OWN + CTX) // 128
TOK = NT * 128
NKT = (SEQ + CTX) // 128
NBLK = 2 * NT + 32
EPS = 1e-6


class _Op:
    __slots__ = ("eng", "fn", "deps", "is_dma", "chan", "chan_val", "needs_inc", "inc_val")

    def __init__(self, eng, fn, is_dma=False, chan=None):
        self.eng = eng
        self.fn = fn
        self.deps = []
        self.is_dma = is_dma
        self.chan = chan
        self.chan_val = 0
        self.needs_inc = False
        self.inc_val = 0


class Prog:
    _uid = 0

    def __init__(self, nc):
        self.nc = nc
        self.ops = {e: [] for e in ENGS}
        self.last_write = {}
        self.reads_since = {}
        self.chan_count = {}

    def _add(self, op, reads, writes):
        deps = []
        for r in reads:
            w = self.last_write.get(r)
            if w is not None:
                deps.append(w)
        for r in writes:
            w = self.last_write.get(r)
            if w is not None:
                deps.append(w)
            deps.extend(self.reads_since.get(r, ()))
        seen = set()
        for d in deps:
            if d is op or id(d) in seen:
                continue
            seen.add(id(d))
            if (not d.is_dma) and (not op.is_dma) and d.eng == "tensor" and op.eng == "tensor":
                continue
            op.deps.append(d)
        for r in reads:
            self.reads_since.setdefault(r, []).append(op)
        for r in writes:
            self.last_write[r] = op
            self.reads_since[r] = []
        self.ops[op.eng].append(op)
        return op

    def op(self, eng, fn, reads=(), writes=()):
        return self._add(_Op(eng, fn), reads, writes)

    def dma(self, eng, fn, chan, reads=(), writes=()):
        o = _Op(eng, fn, is_dma=True, chan=chan)
        self.chan_count[chan] = self.chan_count.get(chan, 0) + 16
        o.chan_val = self.chan_count[chan]
        return self._add(o, reads, writes)

    def emit(self):
        nc = self.nc
        for e in ENGS:
            for o in self.ops[e]:
                for d in o.deps:
                    if not d.is_dma:
                        d.needs_inc = True
        for e in ENGS:
            c = 0
            for o in self.ops[e]:
                if (not o.is_dma) and o.needs_inc:
                    c += 1
                    o.inc_val = c
        chans = sorted(self.chan_count.keys(), key=str)
        prog = self
        with contextlib.ExitStack() as st:
            Prog._uid += 1
            u = Prog._uid
            esem = {e: st.enter_context(nc.semaphore("se%d_%s" % (u, e))) for e in ENGS if e != "sync"}
            csem = {c: st.enter_context(nc.semaphore("sc%d_%d" % (u, i))) for i, c in enumerate(chans)}
            block = st.enter_context(nc.Block())

            def make(ename):
                def body(eng):
                    waited = {}
                    for o in prog.ops[ename]:
                        for d in o.deps:
                            if d.is_dma:
                                key, val, sem = ("c", d.chan), d.chan_val, csem[d.chan]
                            else:
                                key, val, sem = ("e", d.eng), d.inc_val, esem[d.eng]
                            if waited.get(key, 0) >= val:
                                continue
                            waited[key] = val
                            eng.wait_ge(sem, val)
                        ins = o.fn(eng)
                        if o.is_dma:
                            ins.then_inc(csem[o.chan], 16)
                        elif o.needs_inc:
                            ins.then_inc(esem[ename], 1)
                    if ename == "sync":
                        for c in chans:
                            eng.wait_ge(csem[c], prog.chan_count[c])
                return body

            for e in ENGS:
                getattr(block, e)(make(e))


def MM(P, out, lhsT, rhs, start, stop, reads, writes):
    P.op("tensor", lambda e: e.matmul(out, lhsT=lhsT, rhs=rhs, start=start, stop=stop), reads, writes)


def TR(P, out, in_, ident, reads, writes):
    P.op("tensor", lambda e: e.transpose(out, in_, ident), reads, writes)


def ACT(P, out, in_, func, reads, writes, scale=None, bias=None, accum_out=None):
    kw = {}
    if scale is not None:
        kw["scale"] = scale
    if bias is not None:
        kw["bias"] = bias
    if accum_out is not None:
        kw["accum_out"] = accum_out
    P.op("scalar", lambda e: e.activation(out=out, in_=in_, func=func, **kw), reads, writes)


def TT(P, eng, out, in0, in1, op, reads, writes):
    P.op(eng, lambda e: e.tensor_tensor(out=out, in0=in0, in1=in1, op=op), reads, writes)


def TS(P, eng, out, in0, s1, op0, reads, writes, s2=None, op1=None):
    if op1 is None:
        P.op(eng, lambda e: e.tensor_scalar(out=out, in0=in0, scalar1=s1, scalar2=None, op0=op0), reads, writes)
    else:
        P.op(eng, lambda e: e.tensor_scalar(out=out, in0=in0, scalar1=s1, scalar2=s2, op0=op0, op1=op1), reads, writes)


def STT(P, out, in0, scalar, in1, op0, op1, reads, writes):
    P.op("vector", lambda e: e.scalar_tensor_tensor(out=out, in0=in0, scalar=scalar, in1=in1, op0=op0, op1=op1),
         reads, writes)


def CP(P, eng, out, in_, reads, writes):
    if eng == "scalar":
        P.op(eng, lambda e: e.copy(out=out, in_=in_), reads, writes)
    else:
        P.op(eng, lambda e: e.tensor_copy(out=out, in_=in_), reads, writes)


def RED(P, out, in_, op, reads, writes):
    P.op("vector", lambda e: e.tensor_reduce(out=out, in_=in_, axis=AX.X, op=op), reads, writes)


def RCP(P, out, in_, reads, writes):
    P.op("vector", lambda e: e.reciprocal(out=out, in_=in_), reads, writes)


def MSET(P, eng, ap, val, writes):
    P.op(eng, lambda e: e.memset(ap, val), (), writes)


def DMA(P, eng, out, in_, chan, reads, writes):
    P.dma(eng, lambda e: e.dma_start(out=out, in_=in_), chan, reads, writes)


def make_ident(P, nc, ident, idf):
    P.op("gpsimd", lambda e: e.iota(idf[:], pattern=[[1, 128]], base=0, channel_multiplier=-1,
                                     allow_small_or_imprecise_dtypes=True), (), ["idf"])
    TS(P, "vector", ident[:], idf[:], 0.0, ALU.is_equal, ["idf"], ["ident"])


def rms_rstd(P, x_ap, junk_ap, ss, rt, rstd, epst, n, rd, tag):
    ACT(P, junk_ap, x_ap, AF.Square, rd, ["junk" + tag, "ss" + tag], accum_out=ss[:, 0:1])
    ACT(P, rt[:, 0:1], ss[:, 0:1], AF.Sqrt, ["ss" + tag], ["rt" + tag], scale=1.0 / n, bias=epst[:, 0:1])
    RCP(P, rstd[:, 0:1], rt[:, 0:1], ["rt" + tag], ["rstd" + tag])


def mod_broadcast(P, dst_ap, psb, sel_sb, mod_sb, r, col0, mul_ap, reads_extra, wname, k, pname="psb"):
    for half in range(2):
        ps = psb[(k + half) % 2]
        pn = pname + "%d" % ((k + half) % 2)
        MM(P, ps[:], sel_sb[:, r, :], mod_sb[:, col0 + half * 512: col0 + (half + 1) * 512], True, True,
           ["sel", "mod"], [pn])
        d = dst_ap[:, half * 512:(half + 1) * 512]
        if mul_ap is None:
            CP(P, "scalar", d, ps[:], [pn], [wname])
        else:
            STT(P, d, ps[:], 1.0, mul_ap[:, half * 512:(half + 1) * 512], ALU.add, ALU.mult,
                [pn] + reads_extra, [wname])


def load_mod_bcast(nc, P, stack, modi, sel, col0, ncols, tag, psb=None):
    mod_sb = stack.enter_context(nc.sbuf_tensor("mod_sb" + tag, [2, ncols], F32))
    sel_sb = stack.enter_context(nc.sbuf_tensor("sel_sb" + tag, [2, 2, 128], F32))
    if psb is None:
        psb = [stack.enter_context(nc.psum_tensor("psb%s%d" % (tag, i), [128, 512], F32)) for i in range(2)]
    DMA(P, "sync", mod_sb[:], modi[:, col0:col0 + ncols], "c_mod", [], ["mod"])
    DMA(P, "sync", sel_sb[:], sel[:, :, :], "c_sel", [], ["sel"])
    return mod_sb, sel_sb, psb


def build_pre(kind):
    ncol = 1536 if kind == "ab" else 3072
    nnorm = 1280 if kind == "ab" else 2048
    G = nnorm // 64
    nc = bass.Bass("TRN2", target_bir_lowering=False)

    def din(name, shape, dt=F32):
        return nc.dram_tensor(name, shape, dt, kind="ExternalInput").ap()

    xin = din("xin", [TOK, D])
    scT = din("scT", [128, 8, 2])
    wmod = din("wmod", [D, 6 * D])
    bmod2 = din("bmod2", [2, 6 * D])
    nmix = din("nmix", [128, D])
    win = din("win", [D, ncol])
    gains = din("gains", [128, nnorm])
    cs = din("cs", [128, 16, 64])
    sel = din("sel", [2, 2, 128])
    qkv = nc.dram_tensor("qkv", [TOK, ncol], BF16, kind="ExternalOutput").ap()
    modo = nc.dram_tensor("modo", [2, 6 * D], F32, kind="ExternalOutput").ap()

    with contextlib.ExitStack() as st:
        def S(name, shape, dt):
            return st.enter_context(nc.sbuf_tensor(name, shape, dt))

        def PS(name, shape, dt):
            return st.enter_context(nc.psum_tensor(name, shape, dt))

        ident = S("ident", [128, 128], BF16)
        idf = S("idf", [128, 128], F32)
        win_sb = S("win_sb", [128, 8, ncol], BF16)
        Gb = S("Gb", [128, 2, D], F32)
        SHb = S("SHb", [128, 2, D], F32)
        gains_sb = S("gains_sb", [128, nnorm], F32)
        cs_sb = S("cs_sb", [128, 16, 64], F32)
        epst = S("epst", [128, 1], F32)
        st0 = contextlib.ExitStack()

        def S0(name, shape, dt):
            return st0.enter_context(nc.sbuf_tensor(name, shape, dt))

        def PS0(name, shape, dt):
            return st0.enter_context(nc.psum_tensor(name, shape, dt))

        mod_sb = S0("mod_sb", [2, 6 * D], F32)
        bm_sb = S0("bm_sb", [2, 6 * D], F32)
        sel_sb = S0("sel_sb", [2, 2, 128], F32)
        nmix_sb = S0("nmix_sb", [128, D], F32)
        sct = S0("sct", [128, 8, 2], F32)
        wmt = [S0("wm%d" % i, [128, 8, 512], F32) for i in range(2)]
        psm = [PS0("psm%d" % i, [2, 512], F32) for i in range(2)]
        psb = [PS0("psb%d" % i, [128, 512], F32) for i in range(2)]

        P = Prog(nc)
        make_ident(P, nc, ident, idf)
        MSET(P, "vector", epst[:], EPS, ["eps"])
        DMA(P, "sync", sct[:], scT[:, :, :], "c_sc", [], ["sct"])
        ACT(P, sct[:], sct[:], AF.Silu, ["sct"], ["sct"])
        DMA(P, "sync", bm_sb[:], bmod2[:, :], "c_bm", [], ["bm"])
        DMA(P, "sync", sel_sb[:], sel[:, :, :], "c_sel", [], ["sel"])
        DMA(P, "sync", nmix_sb[:], nmix[:, :], "c_nm", [], ["nmix"])
        DMA(P, "sync", gains_sb[:], gains[:, :], "c_gn", [], ["gains"])
        DMA(P, "sync", cs_sb[:], cs[:, :, :], "c_cs", [], ["cs"])
        for c in range(8):
            DMA(P, "gpsimd", win_sb[:, c, :], win[c * 128:(c + 1) * 128, :], "c_win", [], ["win%d" % c])
        wmod_v = wmod.rearrange("(c p) n -> p c n", p=128)
        for j in range(12):
            b = j % 2
            DMA(P, "sync", wmt[b][:], wmod_v[:, :, j * 512:(j + 1) * 512], "c_wm%d" % b, [], ["wm%d" % b])
            for c in range(8):
                MM(P, psm[b][:], sct[:, c, :], wmt[b][:, c, :], c == 0, c == 7, ["sct", "wm%d" % b], ["psm%d" % b])
            TT(P, "vector", mod_sb[:, j * 512:(j + 1) * 512], psm[b][:], bm_sb[:, j * 512:(j + 1) * 512], ALU.add,
               ["psm%d" % b, "bm"], ["mod"])
        DMA(P, "sync", modo[:, :], mod_sb[:], "c_mo", ["mod"], [])
        for r in range(2):
            mod_broadcast(P, SHb[:, r, :], psb, sel_sb, mod_sb, r, 0, None, [], "SHb", 0)
            mod_broadcast(P, Gb[:, r, :], psb, sel_sb, mod_sb, r, 1024, nmix_sb, ["nmix"], "Gb", 0)
        P.emit()
        st0.close()

        xt = [S("xt%d" % i, [128, D], F32) for i in range(2)]
        junk = S("junk", [128, D], BF16)
        ss = [S("ss%d" % i, [128, 1], F32) for i in range(2)]
        rt = [S("rt%d" % i, [128, 1], F32) for i in range(2)]
        rstd = [S("rstd%d" % i, [128, 1], F32) for i in range(2)]
        hb = [S("hb%d" % i, [128, D], BF16) for i in range(2)]
        hT = [S("hT%d" % i, [128, D], BF16) for i in range(2)]
        qf = [S("qf%d" % i, [128, ncol], F32) for i in range(2)]
        sqs = [S("sq%d" % i, [128, nnorm], F32) for i in range(2)]
        ssqs = [S("ssq%d" % i, [128, G], F32) for i in range(2)]
        rqs = [S("rq%d" % i, [128, G], F32) for i in range(2)]
        rq2s = [S("rq2%d" % i, [128, G], F32) for i in range(2)]
        t1 = S("t1", [128, G, 32], F32)
        t2 = S("t2", [128, G, 32], F32)
        t3 = S("t3", [128, G, 32], F32)
        t4 = S("t4", [128, G, 32], F32)
        ob = [S("ob%d" % i, [128, ncol], BF16) for i in range(2)]
        pT = PS("pT", [128, D], BF16)
        pq = [PS("pq%d" % i, [128, 512], F32) for i in range(3)]

        P = Prog(nc)
        nq_box = [0]

        def stage_a(t):
            b = t % 2
            r = 0 if t < 16 else 1
            X = "x%d" % b
            if t == 0:
                DMA(P, "sync", xt[0][:], xin[0:128, :], "c_x0", [], ["x0"])
            if t + 1 < NT:
                nb_ = (t + 1) % 2
                DMA(P, "sync", xt[nb_][:], xin[(t + 1) * 128:(t + 2) * 128, :], "c_x%d" % nb_, [], ["x%d" % nb_])
            rms_rstd(P, xt[b][:], junk[:], ss[b], rt[b], rstd[b], epst, D, [X], str(b))
            STT(P, xt[b][:], xt[b][:], rstd[b][:, 0:1], Gb[:, r, :], ALU.mult, ALU.mult, [X, "rstd%d" % b], [X])
            TT(P, "gpsimd", hb[b][:], xt[b][:], SHb[:, r, :], ALU.add, [X], ["hb%d" % b])
            for c in range(8):
                TR(P, pT[:, c * 128:(c + 1) * 128], hb[b][:, c * 128:(c + 1) * 128], ident[:], ["hb%d" % b], ["pT"])
            CP(P, "scalar", hT[b][:], pT[:], ["pT"], ["hT%d" % b])
            for jc in range(ncol // 512):
                pp = nq_box[0] % 3
                nq_box[0] += 1
                for c in range(8):
                    MM(P, pq[pp][:], hT[b][:, c * 128:(c + 1) * 128], win_sb[:, c, jc * 512:(jc + 1) * 512],
                       c == 0, c == 7, ["hT%d" % b], ["pq%d" % pp])
                CP(P, "scalar" if jc % 2 == 0 else "vector", qf[b][:, jc * 512:(jc + 1) * 512], pq[pp][:],
                   ["pq%d" % pp], ["qf%d" % b])
            QF = "qf%d" % b
            OB = "ob%d" % b

        def stage_b(t):
            b = t % 2
            QF = "qf%d" % b
            OB = "ob%d" % b
            sq, ssq, rq, rq2 = sqs[b], ssqs[b], rqs[b], rq2s[b]
            SQ, SSQ, RQ, RQ2 = "sq%d" % b, "ssq%d" % b, "rq%d" % b, "rq2%d" % b
            TT(P, "gpsimd", sq[:], qf[b][:, 0:nnorm], qf[b][:, 0:nnorm], ALU.mult, [QF], [SQ])
            RED(P, ssq[:], sq[:].rearrange("p (g d) -> p g d", d=64), ALU.add, [SQ], [SSQ])
            ACT(P, rq[:], ssq[:], AF.Sqrt, [SSQ], [RQ], scale=1.0 / 64, bias=epst[:, 0:1])
            RCP(P, rq2[:], rq[:], [RQ], [RQ2])
            qfv = qf[b][:, 0:nnorm].rearrange("p (g d) -> p g d", d=64)
            TT(P, "vector", qfv, qfv, rq2[:].unsqueeze(2).broadcast_to([128, G, 64]), ALU.mult, [QF, RQ2], [QF])
            if t < 16:
                TT(P, "gpsimd", qf[b][:, 0:nnorm], qf[b][:, 0:nnorm], gains_sb[:], ALU.mult, [QF], [QF])
                qv = qf[b][:, 0:nnorm].rearrange("p (g h d) -> p g h d", h=2, d=32)
                ov = ob[b][:, 0:nnorm].rearrange("p (g h d) -> p g h d", h=2, d=32)
                cosb = cs_sb[:, t, 0:32].unsqueeze(1).broadcast_to([128, G, 32])
                sinb = cs_sb[:, t, 32:64].unsqueeze(1).broadcast_to([128, G, 32])
                TT(P, "vector", t1[:], qv[:, :, 0, :], cosb, ALU.mult, [QF], ["t1"])
                TT(P, "vector", t2[:], qv[:, :, 1, :], sinb, ALU.mult, [QF], ["t2"])
                TT(P, "vector", ov[:, :, 0, :], t1[:], t2[:], ALU.subtract, ["t1", "t2"], [OB + "a"])
                TT(P, "gpsimd", t3[:], qv[:, :, 1, :], cosb, ALU.mult, [QF], ["t3"])
                TT(P, "gpsimd", t4[:], qv[:, :, 0, :], sinb, ALU.mult, [QF], ["t4"])
                TT(P, "gpsimd", ov[:, :, 1, :], t3[:], t4[:], ALU.add, ["t3", "t4"], [OB + "b"])
            else:
                TT(P, "gpsimd", ob[b][:, 0:nnorm], qf[b][:, 0:nnorm], gains_sb[:], ALU.mult, [QF], [OB + "a", OB + "b"])
            CP(P, "scalar", ob[b][:, nnorm:ncol], qf[b][:, nnorm:ncol], [QF], [OB + "c"])
            DMA(P, "sync", qkv[t * 128:(t + 1) * 128, :], ob[b][:], "c_o%d" % b, [OB + "a", OB + "b", OB + "c"], [])

        stage_a(0)
        for t in range(NT):
            if t + 1 < NT:
                stage_a(t + 1)
            stage_b(t)
        P.emit()
    return nc


_PROG_CACHE = {}


def get_prog(key, builder):
    if key not in _PROG_CACHE:
        _PROG_CACHE[key] = builder()
    return _PROG_CACHE[key]


def rope_tables():
    rows_n = SEQ // 64
    rows = np.repeat(np.arange(rows_n, dtype=np.float32), 64)
    cols = np.tile(np.arange(64, dtype=np.float32), rows_n)
    inv = (np.float32(10000.0) ** (-np.arange(0, 32, 2, dtype=np.float32) / np.float32(32))).astype(np.float32)
    ang = np.concatenate([rows[:, None] * inv, cols[:, None] * inv], axis=-1).astype(np.float32)
    return np.cos(ang).astype(np.float32), np.sin(ang).astype(np.float32)


def pre_inputs(inp, l, x, ctx):
    i = l // 2
    kind = "ab" if l % 2 == 0 else "c"
    cc = np.stack([inp["c"][0], inp["c_ctx"]]).astype(np.float32)
    scT = np.ascontiguousarray(cc.reshape(2, 8, 128).transpose(2, 1, 0))
    bmod2 = np.ascontiguousarray(np.broadcast_to(inp["b_mod"][l], (2, 6 * D)))
    nmix = np.ascontiguousarray(np.broadcast_to(inp["norm_mix"][l], (128, D)))
    if kind == "ab":
        w = inp["w_in_ab"][i]
        win = np.concatenate([w[:, 0:512], w[:, 768:1280], w[:, 512:640], w[:, 1280:1408], w[:, 640:768],
                              w[:, 1408:1536]], axis=1)
        g = np.concatenate([np.tile(inp["qn_a"][i], 8), np.tile(inp["qn_b"][i], 8), np.tile(inp["kn_a"][i], 2),
                            np.tile(inp["kn_b"][i], 2)])
    else:
        win = inp["w_in_c"][i]
        g = np.concatenate([np.tile(inp["qn_c"][i], 16), np.tile(inp["kn_c"][i], 16)])
    win = np.ascontiguousarray(win.astype(np.float32))
    gains = np.ascontiguousarray(np.broadcast_to(g.astype(np.float32), (128, g.shape[0])))
    cos, sin = rope_tables()
    sel = np.zeros((2, 2, 128), np.float32)
    sel[0, 0] = 1
    sel[1, 1] = 1
    maps = []
    for c in range(NCORES):
        sl = slice(c * OWN, (c + 1) * OWN)
        cs = np.concatenate([cos[sl], sin[sl]], axis=-1).reshape(16, 128, 64).transpose(1, 0, 2)
        maps.append({
            "xin": np.ascontiguousarray(np.concatenate([x[sl], ctx], axis=0)),
            "scT": scT, "wmod": inp["w_mod"][l], "bmod2": bmod2, "nmix": nmix, "win": win, "gains": gains,
            "cs": np.ascontiguousarray(cs), "sel": sel,
        })
    return maps


class _Cnt:
    def __init__(self):
        self.d = {}

    def nxt(self, k, n):
        v = self.d.get(k, 0)
        self.d[k] = v + 1
        return v % n


def build_post(kind):
    ab = kind == "ab"
    nc = bass.Bass("TRN2", target_bir_lowering=False)

    def din(name, shape, dt=F32):
        return nc.dram_tensor(name, shape, dt, kind="ExternalInput").ap()

    xin = din("xin", [TOK, D])
    modi = din("modi", [2, 6 * D])
    sel = din("sel", [2, 2, 128])
    wout = din("wout", [D, D])
    nffn = din("nffn", [128, D])
    wr = din("wr", [D, 36])
    br = din("br", [128, 36])
    w1 = din("w1", [32, D, 512])
    w3 = din("w3", [32, D, 512])
    w2 = din("w2", [32, 512, D])
    tri128 = din("tri128", [128, 128], BF16)
    tri32 = din("tri32", [32, 32], BF16)
    thr = din("thr", [128, 36])
    biota = din("biota", [128, NBLK])
    piota = din("piota", [128, 1])
    if ab:
        qta = din("qta", [128, 2, NT, 512], BF16)
        qtb = din("qtb", [128, 2, NT, 512], BF16)
        kta = din("kta", [128, 2560], BF16)
        va = din("va", [128, 20, 2, 65], BF16)
        ktb = din("ktb", [128, NKT * 128], BF16)
        vb = din("vb", [128, NKT, 2, 65], BF16)
        sink = din("sink", [1, 8])
        flags = din("flags", [128, 2])
        mlo = din("mlo", [128, 512], BF16)
        mhi = din("mhi", [128, 512], BF16)
    else:
        qtc = din("qtc", [2, 128, 8, TOK], BF16)
        ktc = din("ktc", [8, 128, NKT * 128], BF16)
        vc = din("vc", [8, 128, NKT, 128], BF16)
        lamp = din("lamp", [128, 4, 64])
        laminit = din("laminit", [128, 1])
        oml = din("oml", [128, 1])
        subln = din("subln", [128, 1])
    xo = nc.dram_tensor("xo", [TOK, D], F32, kind="ExternalOutput").ap()
    x1s = nc.dram_tensor("x1s", [TOK, D], F32, kind="Internal").ap()
    buf = nc.dram_tensor("buf", [NBLK * 128, D], BF16, kind="Internal").ap()
    ybuf = nc.dram_tensor("ybuf", [NBLK * 128, D], F32, kind="Internal").ap()
    h2s = nc.dram_tensor("h2s", [TOK, D], BF16, kind="Internal").ap()

    with contextlib.ExitStack() as st:
        def S(name, shape, dt):
            return st.enter_context(nc.sbuf_tensor(name, shape, dt))

        ident = S("ident", [128, 128], BF16)
        identf = S("identf", [128, 128], F32)
        onesf = S("onesf", [128, 128], F32)
        epst = S("epst", [128, 1], F32)

        if True:
            P = Prog(nc)
            P.op("gpsimd", lambda e: e.iota(identf[:], pattern=[[1, 128]], base=0, channel_multiplier=-1,
                                             allow_small_or_imprecise_dtypes=True), (), ["idf0"])
            TS(P, "vector", ident[:], identf[:], 0.0, ALU.is_equal, ["idf0"], ["ident"])
            TS(P, "vector", identf[:], identf[:], 0.0, ALU.is_equal, ["idf0", "ident"], ["idf0"])
            MSET(P, "vector", epst[:], EPS, ["eps"])
            MSET(P, "vector", onesf[:], 1.0, ["onesf"])
            P.emit()

        sM = contextlib.ExitStack()
        M0b = sM.enter_context(nc.sbuf_tensor("M0b", [128, NT, 32], BF16))
        M1b = sM.enter_context(nc.sbuf_tensor("M1b", [128, NT, 32], BF16))
        wts = sM.enter_context(nc.sbuf_tensor("wts", [128, NT, 2], F32))
        desti = sM.enter_context(nc.sbuf_tensor("desti", [128, NT, 2], I32))
        idxA = sM.enter_context(nc.sbuf_tensor("idxA", [128, NBLK], I32))
        idxB = sM.enter_context(nc.sbuf_tensor("idxB", [128, NBLK], I32))
        sA = contextlib.ExitStack()
        if ab:
            oT = sA.enter_context(nc.sbuf_tensor("oT", [64, 16, TOK], BF16))
        else:
            oT = sA.enter_context(nc.sbuf_tensor("oT", [128, 8, TOK], BF16))

        with contextlib.ExitStack() as s1:
            def S1(name, shape, dt):
                return s1.enter_context(nc.sbuf_tensor(name, shape, dt))

            def PS1(name, shape, dt):
                return s1.enter_context(nc.psum_tensor(name, shape, dt))

            P = Prog(nc)
            cnt = _Cnt()
            if ab:
                kta_sb = S1("kta_sb", [128, 2560], BF16)
                va_sb = S1("va_sb", [128, 20, 2, 65], BF16)
                ktb_sb = S1("ktb_sb", [128, NKT * 128], BF16)
                vb_sb = S1("vb_sb", [128, NKT, 2, 65], BF16)
                q_sb = [S1("q_sb%d" % i, [128, 512], BF16) for i in range(2)]
                pt = [S1("pt%d" % i, [128, 512], BF16) for i in range(3)]
                ptm = [S1("ptm%d" % i, [128, 512], BF16) for i in range(2)]
                rrow = S1("rrow", [65, 512], F32)
                bc_sb = S1("bc_sb", [64, 512], F32)
                sink_sb = S1("sink_sb", [1, 8], F32)
                es_sb = S1("es_sb", [1, 8], F32)
                esrow = S1("esrow", [1, 2, 512], F32)
                e64 = S1("e64", [1, 65], F32)
                flags_sb = S1("flags_sb", [128, 2], F32)
                mlo_sb = S1("mlo_sb", [128, 512], BF16)
                mhi_sb = S1("mhi_sb", [128, 512], BF16)
                ps_s = [PS1("ps_s%d" % i, [128, 512], F32) for i in range(3)]
                ps_o = [PS1("ps_o%d" % i, [65, 512], F32) for i in range(2)]
                ps_b = PS1("ps_b", [64, 512], F32)

                DMA(P, "sync", kta_sb[:], kta[:, :], "c_kta", [], ["kta"])
                DMA(P, "sync", ktb_sb[:], ktb[:, :], "c_ktb", [], ["ktb"])
                DMA(P, "sync", vb_sb[:], vb[:, :, :, :], "c_vb", [], ["vb"])
                DMA(P, "sync", va_sb[:], va[:, :, :, :], "c_va", [], ["va"])
                DMA(P, "sync", sink_sb[:], sink[:, :], "c_sink", [], ["sink"])
                DMA(P, "sync", flags_sb[:], flags[:, :], "c_fl", [], ["flags"])
                DMA(P, "sync", mlo_sb[:], mlo[:, :], "c_mlo", [], ["mlo"])
                DMA(P, "sync", mhi_sb[:], mhi[:, :], "c_mhi", [], ["mhi"])
                ACT(P, es_sb[:], sink_sb[:], AF.Exp, ["sink"], ["es"])
                for kvh in range(2):
                    CP(P, "vector", esrow[0:1, kvh, :].rearrange("o (g q) -> o g q", q=128),
                       es_sb[0:1, kvh * 4:(kvh + 1) * 4].unsqueeze(2).broadcast_to([1, 4, 128]), ["es"], ["esrow"])
                MSET(P, "vector", e64[:], 0.0, ["e64"])
                MSET(P, "vector", e64[0:1, 64:65], 1.0, ["e64"])

                units = []
                for kvh in range(2):
                    for t in range(NT):
                        def key(kt, m=None, f=None, kvh=kvh):
                            return (kta_sb[:, kt * 128:(kt + 1) * 128], "kta", va_sb[:, kt, kvh, :], "va", m, f)
                        if t < 16:
                            keys = [key(t, mlo_sb[:], flags_sb[:, 0:1] if t == 0 else None), key(t + 1),
                                    key(t + 2, mhi_sb[:], flags_sb[:, 1:2] if t == 15 else None), key(18), key(19)]
                        else:
                            keys = [key(18), key(19)]
                        units.append((qta[:, kvh, t, :], keys, esrow[0:1, kvh, :],
                                      oT[:, kvh * 4:(kvh + 1) * 4, t * 128:(t + 1) * 128]))
                for kvh in range(2):
                    for t in range(NT):
                        kts = range(NKT) if t < 16 else (NKT - 2, NKT - 1)
                        keys = [(ktb_sb[:, kt * 128:(kt + 1) * 128], "ktb", vb_sb[:, kt, kvh, :], "vb", None, None)
                                for kt in kts]
                        units.append((qtb[:, kvh, t, :], keys, None,
                                      oT[:, 8 + kvh * 4:8 + (kvh + 1) * 4, t * 128:(t + 1) * 128]))
                flat = [(ui, i) for ui, u in enumerate(units) for i in range(len(u[1]))]
                LA = 2

                def issue_S(j):
                    ui, i = flat[j]
                    q_src, keys, _, _ = units[ui]
                    qb = ui % 2
                    QN = "q%d" % qb
                    if i == 0:
                        DMA(P, "sync", q_sb[qb][:], q_src, "c_q%d" % qb, [], [QN])
                    si = j % 3
                    MM(P, ps_s[si][:], keys[i][0], q_sb[qb][:], True, True, [QN, keys[i][1]], ["ps_s%d" % si])

                def issue_rest(j):
                    ui, i = flat[j]
                    q_src, keys, sink_rhs, out_ap = units[ui]
                    kT_ap, kname, v_ap, vname, mask_ap, flag_ap = keys[i]
                    n = len(keys)
                    si = j % 3
                    oi = ui % 2
                    ON = "ps_o%d" % oi
                    ACT(P, pt[si][:], ps_s[si][:], AF.Exp, ["ps_s%d" % si], ["pt%d" % si], scale=0.125)
                    rhs, rn = pt[si][:], "pt%d" % si
                    if mask_ap is not None:
                        mi = cnt.nxt("m", 2)
                        if flag_ap is not None:
                            STT(P, ptm[mi][:], pt[si][:], flag_ap, mask_ap, ALU.mult, ALU.mult,
                                [rn, "flags", "mlo", "mhi"], ["ptm%d" % mi])
                        else:
                            TT(P, "vector", ptm[mi][:], pt[si][:], mask_ap, ALU.mult, [rn, "mlo", "mhi"],
                               ["ptm%d" % mi])
                        rhs, rn = ptm[mi][:], "ptm%d" % mi
                    if i == 0:
                        flush_upto(ui - 2)
                    MM(P, ps_o[oi][:], v_ap, rhs, i == 0, (i == n - 1) and sink_rhs is None, [rn, vname], [ON])
                    if i == n - 1:
                        if sink_rhs is not None:
                            MM(P, ps_o[oi][:], e64[0:1, :], sink_rhs, False, True, ["e64", "esrow"], [ON])
                        pending.append([4, ui, (lambda oi=oi, ON=ON, out_ap=out_ap: finalize(oi, ON, out_ap))])

                def finalize(oi, ON, out_ap):
                    RCP(P, rrow[64:65, :], ps_o[oi][64:65, :], [ON], ["rrow"])
                    MM(P, ps_b[:], onesf[64:65, 0:64], rrow[64:65, :], True, True, ["rrow", "onesf"], ["ps_b"])
                    CP(P, "scalar", bc_sb[:], ps_b[:], ["ps_b"], ["bc"])
                    TT(P, "vector", out_ap, ps_o[oi][0:64, :].rearrange("d (g q) -> d g q", q=128),
                       bc_sb[:].rearrange("d (g q) -> d g q", q=128), ALU.mult, [ON, "bc"], ["oT"])

                pending = []

                def tick():
                    for p_ in pending:
                        p_[0] -= 1
                    while pending and pending[0][0] <= 0:
                        pending.pop(0)[2]()

                def flush_upto(uidx):
                    while pending and pending[0][1] <= uidx:
                        pending.pop(0)[2]()

                for j in range(min(LA, len(flat))):
                    issue_S(j)
                for j in range(len(flat)):
                    if j + LA < len(flat):
                        issue_S(j + LA)
                    tick()
                    issue_rest(j)
                flush_upto(len(units))
            else:
                kt_sbs = [S1("kt_sb%d" % i, [128, NKT * 128], BF16) for i in range(2)]
                v_sbs = [S1("v_sb%d" % i, [128, NKT, 128], BF16) for i in range(2)]
                q1_sb = [S1("q1_sb%d" % i, [128, 512], BF16) for i in range(2)]
                q2_sb = [S1("q2_sb%d" % i, [128, 512], BF16) for i in range(2)]
                p1 = [S1("p1_%d" % i, [128, 512], BF16) for i in range(3)]
                p2 = [S1("p2_%d" % i, [128, 512], BF16) for i in range(3)]
                E0 = S1("E0", [128, 128], F32)
                E1 = S1("E1", [128, 128], BF16)
                acc1 = S1("acc1", [128, 512], F32)
                acc2 = S1("acc2", [128, 512], F32)
                rr = S1("rr", [33, 512], F32)
                b1 = S1("b1", [128, 512], F32)
                o1s = S1("o1s", [128, 512], F32)
                o2s = S1("o2s", [128, 512], F32)
                sums_sb = S1("sums_sb", [33, 512], F32)
                Os = S1("Os", [128, 512], F32)
                sqo = S1("sqo", [128, 512], F32)
                rs = S1("rs", [128, 512], F32)
                rs2 = S1("rs2", [128, 512], F32)
                lamp_sb = S1("lamp_sb", [128, 4, 64], F32)
                lp = S1("lp", [128, 2, 64], F32)
                s2 = S1("s2", [128, 2], F32)
                e2 = S1("e2", [128, 2], F32)
                lamv = S1("lamv", [128, 1], F32)
                nlam = S1("nlam", [128, 1], F32)
                li_sb = S1("li_sb", [128, 1], F32)
                oml_sb = S1("oml_sb", [128, 1], F32)
                sub_sb = S1("sub_sb", [128, 1], F32)
                subs = S1("subs", [128, 1], F32)
                ps_s1 = [PS1("ps_s1_%d" % i, [128, 512], F32) for i in range(2)]
                ps_s2 = [PS1("ps_s2_%d" % i, [128, 512], F32) for i in range(2)]
                ps_o1 = PS1("ps_o1", [128, 512], F32)
                ps_o2 = PS1("ps_o2", [128, 512], F32)
                ps_sum = PS1("ps_sum", [128, 512], F32)
                ps_x = PS1("ps_x", [128, 512], F32)

                DMA(P, "sync", lamp_sb[:], lamp[:, :, :], "c_lamp", [], ["lamp"])
                DMA(P, "sync", li_sb[:], laminit[:, :], "c_li", [], ["li"])
                DMA(P, "sync", oml_sb[:], oml[:, :], "c_oml", [], ["oml"])
                DMA(P, "sync", sub_sb[:], subln[:, :], "c_sub", [], ["sub"])
                TT(P, "vector", lp[:, 0, :], lamp_sb[:, 0, :], lamp_sb[:, 1, :], ALU.mult, ["lamp"], ["lp"])
                TT(P, "vector", lp[:, 1, :], lamp_sb[:, 2, :], lamp_sb[:, 3, :], ALU.mult, ["lamp"], ["lp"])
                RED(P, s2[:], lp[:], ALU.add, ["lp"], ["s2"])
                ACT(P, e2[:], s2[:], AF.Exp, ["s2"], ["e2"])
                TT(P, "vector", lamv[:], e2[:, 1:2], e2[:, 0:1], ALU.subtract, ["e2"], ["lamv"])
                TT(P, "vector", nlam[:], lamv[:], li_sb[:], ALU.subtract, ["lamv", "li"], ["nlam"])
                TT(P, "vector", subs[:], sub_sb[:], oml_sb[:], ALU.mult, ["sub", "oml"], ["subs"])
                MSET(P, "vector", E0[:], 0.0, ["E0"])
                MSET(P, "vector", E0[:, 0:1], 1.0, ["E0"])
                MSET(P, "vector", E1[:], 0.0, ["E1"])
                MSET(P, "vector", E1[:, 32:33], 1.0, ["E1"])
                items = []
                for h in range(8):
                    for u in range(5):
                        tok0, N = (u * 512, 512) if u < 4 else (OWN, CTX)
                        kts = list(range(NKT)) if u < 4 else [NKT - 2, NKT - 1]
                        for i, kt in enumerate(kts):
                            items.append((h, u, tok0, N, kt, i, len(kts)))

                def kv_load(h):
                    DMA(P, "sync", kt_sbs[h % 2][:], ktc[h, :, :], "c_kt%d" % (h % 2), [], ["kt%d" % (h % 2)])
                    DMA(P, "sync", v_sbs[h % 2][:], vc[h, :, :, :], "c_v%d" % (h % 2), [], ["v%d" % (h % 2)])

                def issue_S(j):
                    h, u, tok0, N, kt, i, n = items[j]
                    qb = (h * 5 + u) % 2
                    QN = "q%d" % qb
                    if i == 0:
                        if u == 0 and h == 0:
                            kv_load(0)
                            kv_load(1)
                        DMA(P, "sync", q1_sb[qb][:, 0:N], qtc[0, :, h, tok0:tok0 + N], "c_q%d" % qb, [], [QN])
                        DMA(P, "sync", q2_sb[qb][:, 0:N], qtc[1, :, h, tok0:tok0 + N], "c_q%d" % qb, [], [QN])
                    b = j % 2
                    ksl = slice(kt * 128, (kt + 1) * 128)
                    kt_sb = kt_sbs[h % 2]
                    KT = "kt%d" % (h % 2)
                    MM(P, ps_s1[b][:, 0:N], kt_sb[:, ksl], q1_sb[qb][:, 0:N], True, True, [QN, KT], ["ps_s1_%d" % b])
                    MM(P, ps_s2[b][:, 0:N], kt_sb[:, ksl], q2_sb[qb][:, 0:N], True, True, [QN, KT], ["ps_s2_%d" % b])

                def issue_rest(j):
                    h, u, tok0, N, kt, i, n = items[j]
                    b = j % 2
                    v_sb = v_sbs[h % 2]
                    VN = "v%d" % (h % 2)
                    pb = j % 3
                    ACT(P, p1[pb][:, 0:N], ps_s1[b][:, 0:N], AF.Exp, ["ps_s1_%d" % b], ["p1_%d" % pb], scale=0.125)
                    ACT(P, p2[pb][:, 0:N], ps_s2[b][:, 0:N], AF.Exp, ["ps_s2_%d" % b], ["p2_%d" % pb], scale=0.125)
                    MM(P, ps_o2[:, 0:N], v_sb[:, kt, :], p2[pb][:, 0:N], i == 0, i == n - 1, [VN, "p2_%d" % pb], ["ps_o2"])
                    MM(P, ps_o1[:, 0:N], v_sb[:, kt, :], p1[pb][:, 0:N], i == 0, i == n - 1, [VN, "p1_%d" % pb], ["ps_o1"])
                    MM(P, ps_sum[:, 0:N], E1[:], p2[pb][:, 0:N], i == 0, False, ["E1", "p2_%d" % pb], ["ps_sum"])
                    if i == 0:
                        CP(P, "vector", acc1[:, 0:N], p1[pb][:, 0:N], ["p1_%d" % pb], ["acc1"])
                    else:
                        TT(P, "vector", acc1[:, 0:N], acc1[:, 0:N], p1[pb][:, 0:N], ALU.add, ["p1_%d" % pb, "acc1"], ["acc1"])
                    if i != n - 1:
                        return
                    MM(P, ps_sum[:, 0:N], E0[:], acc1[:, 0:N], False, True, ["E0", "acc1"], ["ps_sum"])
                    if u == 4 and 1 <= h + 1 < 7:
                        kv_load(h + 2)
                    flush()
                    CP(P, "vector", sums_sb[0:33, 0:N], ps_sum[0:33, 0:N], ["ps_sum"], ["sums_sb"])
                    CP(P, "vector", o1s[:, 0:N], ps_o1[:, 0:N], ["ps_o1"], ["o1s"])
                    CP(P, "scalar", o2s[:, 0:N], ps_o2[:, 0:N], ["ps_o2"], ["o2s"])
                    pending.append([8, (lambda h=h, tok0=tok0, N=N: finalize(h, tok0, N))])

                def finalize(h, tok0, N):
                    RCP(P, rr[0:33, 0:N], sums_sb[0:33, 0:N], ["sums_sb"], ["rr"])
                    TS(P, "vector", rr[32:33, 0:N], rr[32:33, 0:N], nlam[32:33, 0:1], ALU.mult, ["rr", "nlam"], ["rr"])
                    MM(P, ps_x[:, 0:N], onesf[0:1, :], rr[0:1, 0:N], True, True, ["rr", "onesf"], ["ps_x"])
                    CP(P, "scalar", b1[:, 0:N], ps_x[:, 0:N], ["ps_x"], ["b1"])
                    TT(P, "vector", o1s[:, 0:N], o1s[:, 0:N], b1[:, 0:N], ALU.mult, ["o1s", "b1"], ["o1s"])
                    MM(P, ps_x[:, 0:N], onesf[32:33, :], rr[32:33, 0:N], True, True, ["rr", "onesf"], ["ps_x"])
                    CP(P, "scalar", b1[:, 0:N], ps_x[:, 0:N], ["ps_x"], ["b1"])
                    TT(P, "vector", o2s[:, 0:N], o2s[:, 0:N], b1[:, 0:N], ALU.mult, ["o2s", "b1"], ["o2s"])
                    TT(P, "gpsimd", Os[:, 0:N], o1s[:, 0:N], o2s[:, 0:N], ALU.add, ["o1s", "o2s"], ["Os"])
                    TT(P, "gpsimd", sqo[:, 0:N], Os[:, 0:N], Os[:, 0:N], ALU.mult, ["Os"], ["sqo"])
                    MM(P, ps_x[:, 0:N], onesf[:], sqo[:, 0:N], True, True, ["sqo", "onesf"], ["ps_x"])
                    ACT(P, rs[:, 0:N], ps_x[:, 0:N], AF.Sqrt, ["ps_x"], ["rs"], scale=1.0 / 128, bias=epst[:, 0:1])
                    RCP(P, rs2[:, 0:N], rs[:, 0:N], ["rs"], ["rs2"])
                    STT(P, oT[:, h, tok0:tok0 + N], Os[:, 0:N], subs[:, 0:1], rs2[:, 0:N], ALU.mult, ALU.mult,
                        ["Os", "subs", "rs2"], ["oT"])

                pending = []

                def tick():
                    for p_ in pending:
                        p_[0] -= 1
                    while pending and pending[0][0] <= 0:
                        pending.pop(0)[1]()

                def flush():
                    while pending:
                        pending.pop(0)[1]()

                issue_S(0)
                for j in range(len(items)):
                    if j + 1 < len(items):
                        issue_S(j + 1)
                    tick()
                    issue_rest(j)
                flush()
            P.emit()

        with contextlib.ExitStack() as s2_:
            def S2(name, shape, dt):
                return s2_.enter_context(nc.sbuf_tensor(name, shape, dt))

            def PS2(name, shape, dt):
                return s2_.enter_context(nc.psum_tensor(name, shape, dt))

            KC = 16 if ab else 8
            KP = 64 if ab else 128
            GT1b = S2("GT1b", [128, 2, D], F32)
            SH2b = S2("SH2b", [128, 2, D], F32)
            G2b = S2("G2b", [128, 2, D], F32)
            nffn_sb = S2("nffn_sb", [128, D], F32)
            wout_sb = S2("wout_sb", [KP, KC, D], BF16)
            wr_sb = S2("wr_sb", [128, 8, 36], F32)
            br_sb = S2("br_sb", [128, 36], F32)
            xt = [S2("xt%d" % i, [128, D], F32) for i in range(2)]
            x1 = [S2("x1_%d" % i, [128, D], F32) for i in range(2)]
            junk = S2("junk", [128, D], BF16)
            ss = [S2("ss%d" % i, [128, 1], F32) for i in range(2)]
            rt = [S2("rt%d" % i, [128, 1], F32) for i in range(2)]
            rstd = [S2("rstd%d" % i, [128, 1], F32) for i in range(2)]
            h2f = [S2("h2f%d" % i, [128, D], F32) for i in range(2)]
            h2Ts = [S2("h2T%d" % i, [128, D], F32) for i in range(2)]
            h2bt = [S2("h2bt%d" % i, [128, D], BF16) for i in range(2)]
            lgs = [S2("lg%d" % i, [128, 36], F32) for i in range(2)]
            gmax = S2("gmax", [128, 1], F32)
            ngmax = S2("ngmax", [128, 1], F32)
            gm = S2("gm", [128, 4], F32)
            ge = S2("ge", [128, 4], F32)
            gsum = S2("gsum", [128, 1], F32)
            gp = S2("gp", [128, 1], F32)
            pen = S2("pen", [128, 4], F32)
            lem = S2("lem", [128, 32], F32)
            m8 = S2("m8", [128, 8], F32)
            dm = S2("dm", [128, 1], F32)
            rr_ = S2("rr_", [128, 1], F32)
            den = S2("den", [128, 1], F32)
            rden = S2("rden", [128, 1], F32)
            pq = [PS2("pq%d" % i, [128, 512], F32) for i in range(2)]
            pTf = PS2("pTf", [128, D], F32)
            ps_lg = PS2("ps_lg", [128, 36], F32)

            P = Prog(nc)
            mod_sb, sel_sb, psb = load_mod_bcast(nc, P, s2_, modi, sel, 2048, 3072, "o")
            DMA(P, "sync", nffn_sb[:], nffn[:, :], "c_nf", [], ["nffn"])
            for r in range(2):
                mod_broadcast(P, GT1b[:, r, :], psb, sel_sb, mod_sb, r, 0, None, [], "GT1b", 0)
                mod_broadcast(P, SH2b[:, r, :], psb, sel_sb, mod_sb, r, 1024, None, [], "SH2b", 0)
                mod_broadcast(P, G2b[:, r, :], psb, sel_sb, mod_sb, r, 2048, nffn_sb, ["nffn"], "G2b", 0)
            wv = wout.rearrange("(c p) n -> p c n", p=KP)
            for c in range(KC):
                DMA(P, "gpsimd", wout_sb[:, c, :], wv[:, c, :], "c_wo", [], ["wout%d" % c])
            DMA(P, "sync", wr_sb[:], wr.rearrange("(c p) n -> p c n", p=128), "c_wr", [], ["wr"])
            DMA(P, "sync", br_sb[:], br[:, :], "c_br", [], ["br"])
            def stage1(t):
                b = t % 2
                r = 0 if t < 16 else 1
                X, X1, H2 = "x%d" % b, "x1_%d" % b, "h2f%d" % b
                if t == 0:
                    DMA(P, "sync", xt[0][:], xin[0:128, :], "c_x0", [], ["x0"])
                if t + 1 < NT:
                    nb_ = (t + 1) % 2
                    DMA(P, "sync", xt[nb_][:], xin[(t + 1) * 128:(t + 2) * 128, :], "c_x%d" % nb_, [], ["x%d" % nb_])
                for half in range(2):
                    hs = slice(half * 512, (half + 1) * 512)
                    for c in range(KC):
                        MM(P, pq[half][:], oT[:, c, t * 128:(t + 1) * 128], wout_sb[:, c, hs], c == 0, c == KC - 1,
                           ["wout%d" % c], ["pq%d" % half])
                    TT(P, "vector", x1[b][:, hs], pq[half][:], GT1b[:, r, hs], ALU.mult, ["pq%d" % half, "GT1b"], [X1])
                TT(P, "gpsimd", x1[b][:], x1[b][:], xt[b][:], ALU.add, [X1, X], [X1])
                DMA(P, "sync", x1s[t * 128:(t + 1) * 128, :], x1[b][:], "c_x1s%d" % b, [X1], ["x1s%d" % t])
                rms_rstd(P, x1[b][:], junk[:], ss[b], rt[b], rstd[b], epst, D, [X1], str(b))
                STT(P, h2f[b][:], x1[b][:], rstd[b][:, 0:1], G2b[:, r, :], ALU.mult, ALU.mult, [X1, "rstd%d" % b, "G2b"], [H2])
                TT(P, "gpsimd", h2f[b][:], h2f[b][:], SH2b[:, r, :], ALU.add, [H2, "SH2b"], [H2])
                CP(P, "scalar", h2bt[b][:], h2f[b][:], [H2], ["h2bt%d" % b])
                DMA(P, "sync", h2s[t * 128:(t + 1) * 128, :], h2bt[b][:], "c_h2s%d" % b, ["h2bt%d" % b], ["h2s%d" % t])
                for c in range(8):
                    TR(P, pTf[:, c * 128:(c + 1) * 128], h2f[b][:, c * 128:(c + 1) * 128], identf[:], [H2], ["pTf"])
                h2T = h2Ts[b]
                CP(P, "vector", h2T[:], pTf[:], ["pTf"], ["h2T%d" % b])
                for c in range(8):
                    MM(P, ps_lg[:], h2T[:, c * 128:(c + 1) * 128], wr_sb[:, c, :], c == 0, c == 7, ["h2T%d" % b, "wr"], ["ps_lg"])
                TT(P, "vector", lgs[b][:], ps_lg[:], br_sb[:], ALU.add, ["ps_lg", "br"], ["lg%d" % b])

            def stage2(t):
                b = t % 2
                lg = lgs[b]
                RED(P, gmax[:], lg[:, 0:4], ALU.max, ["lg%d" % b], ["gmax"])
                TS(P, "vector", gm[:], lg[:, 0:4], gmax[:, 0:1], ALU.is_equal, ["lg%d" % b, "gmax"], ["gm"])
                TS(P, "vector", ngmax[:], gmax[:], -1.0, ALU.mult, ["gmax"], ["ngmax"])
                ACT(P, ge[:], lg[:, 0:4], AF.Exp, ["lg%d" % b, "ngmax"], ["ge", "gsum"], bias=ngmax[:, 0:1], accum_out=gsum[:, 0:1])
                RCP(P, gp[:], gsum[:], ["gsum"], ["gp"])
                TS(P, "vector", pen[:], gm[:], 1e30, ALU.mult, ["gm"], ["pen"], s2=-1e30, op1=ALU.add)
                TT(P, "vector", lem[:].rearrange("p (g e) -> p g e", e=8), lg[:, 4:36].rearrange("p (g e) -> p g e", e=8),
                   pen[:].unsqueeze(2).broadcast_to([128, 4, 8]), ALU.add, ["lg%d" % b, "pen"], ["lem"])
                P.op("vector", lambda e: e.max(out=m8[:], in_=lem[:]), ["lem"], ["m8"])
                TS(P, "vector", M0b[:, t, :], lem[:], m8[:, 0:1], ALU.is_equal, ["lem", "m8"], ["M0b"])
                TS(P, "vector", M1b[:, t, :], lem[:], m8[:, 1:2], ALU.is_equal, ["lem", "m8"], ["M1b"])
                TT(P, "vector", dm[:], m8[:, 1:2], m8[:, 0:1], ALU.subtract, ["m8"], ["dm"])
                ACT(P, rr_[:], dm[:], AF.Exp, ["dm"], ["rr_"])
                TS(P, "vector", den[:], rr_[:], 1.0, ALU.add, ["rr_"], ["den"])
                RCP(P, rden[:], den[:], ["den"], ["rden"])
                TT(P, "vector", wts[:, t, 0:1], rden[:], gp[:], ALU.mult, ["rden", "gp"], ["wts"])
                TT(P, "vector", wts[:, t, 1:2], wts[:, t, 0:1], rr_[:], ALU.mult, ["wts", "rr_"], ["wts"])

            stage1(0)
            for t in range(NT):
                if t + 1 < NT:
                    stage1(t + 1)
                stage2(t)
            P.emit()
        with contextlib.ExitStack() as s3:
            def S3(name, shape, dt):
                return s3.enter_context(nc.sbuf_tensor(name, shape, dt))

            def PS3(name, shape, dt):
                return s3.enter_context(nc.psum_tensor(name, shape, dt))

            tri_sb = S3("tri_sb", [128, 128], BF16)
            tri32_sb = S3("tri32_sb", [32, 32], BF16)
            ones_b = S3("ones_b", [128, 128], BF16)
            thr_sb = S3("thr_sb", [128, 36], F32)
            bio_sb = S3("bio_sb", [128, NBLK], F32)
            pio_sb = S3("pio_sb", [128, 1], F32)
            Ms = S3("Ms", [128, NT, 32], BF16)
            Cs = S3("Cs", [128, NT, 32], F32)
            cntf = S3("cntf", [128, 32], F32)
            cmp1 = S3("cmp1", [128, 32, 36], F32)
            nblk = S3("nblk", [128, 32], F32)
            nblkb = S3("nblkb", [128, 32], BF16)
            nbT = S3("nbT", [32, 128], BF16)
            Sx = S3("Sx", [128, 32], F32)
            pend = S3("pend", [128, 32], F32)
            base = S3("base", [128, 32], F32)
            tall = S3("tall", [128, NT, 32], F32)
            prod = S3("prod", [128, NT, 32], F32)
            destf = S3("destf", [128, NT, 2], F32)
            cmp2 = S3("cmp2", [128, NBLK, 32], F32)
            be = S3("be", [128, NBLK], F32)
            idxf = S3("idxf", [128, NBLK], F32)
            ps_c = [PS3("ps_c%d" % i, [128, 32], F32) for i in range(2)]
            ps_t = PS3("ps_t", [32, 128], BF16)
            ps_S = PS3("ps_S", [128, 32], F32)

            P = Prog(nc)
            DMA(P, "sync", tri_sb[:], tri128[:, :], "c_tri", [], ["tri"])
            DMA(P, "sync", tri32_sb[:], tri32[:, :], "c_tri32", [], ["tri32"])
            DMA(P, "sync", thr_sb[:], thr[:, :], "c_thr", [], ["thr"])
            DMA(P, "sync", bio_sb[:], biota[:, :], "c_bio", [], ["bio"])
            DMA(P, "sync", pio_sb[:], piota[:, :], "c_pio", [], ["pio"])
            MSET(P, "vector", ones_b[:], 1.0, ["ones_b"])
            TT(P, "vector", Ms[:], M0b[:], M1b[:], ALU.add, [], ["Ms"])
            for t in range(NT):
                pc = ps_c[t % 2]
                pn = "ps_c%d" % (t % 2)
                MM(P, pc[:], tri_sb[:], Ms[:, t, :], True, t == 0, ["tri", "Ms"], [pn])
                for i in range(t):
                    MM(P, pc[:], ones_b[:], Ms[:, i, :], False, i == t - 1, ["ones_b", "Ms"], [pn])
                CP(P, "vector", Cs[:, t, :], pc[:], [pn], ["Cs"])
            pc = ps_c[NT % 2]
            pn = "ps_c%d" % (NT % 2)
            for i in range(NT):
                MM(P, pc[:], ones_b[:], Ms[:, i, :], i == 0, i == NT - 1, ["ones_b", "Ms"], [pn])
            CP(P, "vector", cntf[:], pc[:], [pn], ["cntf"])
            TT(P, "vector", cmp1[:], cntf[:].unsqueeze(2).broadcast_to([128, 32, 36]),
               thr_sb[:].unsqueeze(1).broadcast_to([128, 32, 36]), ALU.is_gt, ["cntf", "thr"], ["cmp1"])
            RED(P, nblk[:], cmp1[:], ALU.add, ["cmp1"], ["nblk"])
            CP(P, "vector", nblkb[:], nblk[:], ["nblk"], ["nblkb"])
            TR(P, ps_t[:], nblkb[:], ident[:], ["nblkb"], ["ps_t"])
            CP(P, "vector", nbT[:], ps_t[:], ["ps_t"], ["nbT"])
            MM(P, ps_S[:], nbT[:], tri32_sb[:], True, True, ["nbT", "tri32"], ["ps_S"])
            CP(P, "vector", Sx[:], ps_S[:], ["ps_S"], ["Sx"])
            TT(P, "vector", pend[:], Sx[:], nblk[:], ALU.add, ["Sx", "nblk"], ["pend"])
            TS(P, "vector", base[:], Sx[:], 128.0, ALU.mult, ["Sx"], ["base"])
            TT(P, "vector", tall[:], Cs[:], base[:].unsqueeze(1).broadcast_to([128, NT, 32]), ALU.add, ["Cs", "base"], ["tall"])
            TT(P, "vector", prod[:], tall[:], M0b[:], ALU.mult, ["tall"], ["prod"])
            RED(P, destf[:, :, 0], prod[:], ALU.add, ["prod"], ["destf"])
            TT(P, "vector", prod[:], tall[:], M1b[:], ALU.mult, ["tall"], ["prod"])
            RED(P, destf[:, :, 1], prod[:], ALU.add, ["prod"], ["destf"])
            CP(P, "vector", desti[:], destf[:], ["destf"], ["desti"])
            TT(P, "vector", cmp2[:], pend[:].unsqueeze(1).broadcast_to([128, NBLK, 32]),
               bio_sb[:].unsqueeze(2).broadcast_to([128, NBLK, 32]), ALU.is_le, ["pend", "bio"], ["cmp2"])
            RED(P, be[:], cmp2[:], ALU.add, ["cmp2"], ["be"])
            TS(P, "vector", be[:], be[:], 31.0, ALU.min, ["be"], ["be"])
            TS(P, "vector", idxf[:], be[:], 256.0, ALU.mult, ["be", "pio"], ["idxf"], s2=pio_sb[:, 0:1], op1=ALU.add)
            CP(P, "vector", idxA[:], idxf[:], ["idxf"], ["idxA"])
            TS(P, "vector", idxf[:], idxf[:], 1.0, ALU.add, ["idxf", "idxA"], ["idxf"])
            CP(P, "vector", idxB[:], idxf[:], ["idxf"], ["idxB"])
            P.emit()

        sA.close()
        with contextlib.ExitStack() as s4:
            def S4(name, shape, dt):
                return s4.enter_context(nc.sbuf_tensor(name, shape, dt))

            def PS4(name, shape, dt):
                return s4.enter_context(nc.psum_tensor(name, shape, dt))

            NW = 3
            GT2b = S4("GT2b", [128, 2, D], F32)
            W1b = [S4("W1b%d" % i, [128, 4096], BF16) for i in range(NW)]
            W3b = [S4("W3b%d" % i, [128, 4096], BF16) for i in range(NW)]
            W2b = [S4("W2b%d" % i, [128, 4096], BF16) for i in range(NW)]
            xb = [S4("xb%d" % i, [128, D], BF16) for i in range(2)]
            xbT = [S4("xbT%d" % i, [128, 8, 128], BF16) for i in range(2)]
            s1t = S4("s1t", [128, 512], F32)
            gT = [S4("gT%d" % i, [128, 4, 128], BF16) for i in range(2)]
            ysb = [S4("ysb%d" % i, [128, D], F32) for i in range(2)]
            y0 = [S4("y0_%d" % i, [128, D], F32) for i in range(2)]
            y1 = [S4("y1_%d" % i, [128, D], F32) for i in range(2)]
            xc = [S4("xc%d" % i, [128, D], F32) for i in range(2)]
            f0 = S4("f0", [128, D], F32)
            f1 = S4("f1", [128, D], F32)
            f2 = f0
            xo_sb = [S4("xo_sb%d" % i, [128, D], F32) for i in range(2)]
            pxT = PS4("pxT", [128, 8, 128], BF16)
            ps1 = [PS4("ps1_%d" % i, [128, 4, 128], F32) for i in range(2)]
            ps3 = [PS4("ps3_%d" % i, [128, 4, 128], F32) for i in range(2)]
            psy = [PS4("psy%d" % i, [128, 512], F32) for i in range(2)]

            P = Prog(nc)
            for t in range(NT):
                b = t % 2
                DMA(P, "sync", xb[b][:], h2s[t * 128:(t + 1) * 128, :], "c_xb%d" % b, [], ["xb%d" % b])
                for k in range(2):
                    P.dma("gpsimd", (lambda t, k, b: lambda e: e.indirect_dma_start(
                        out=buf[:, :], out_offset=bass.IndirectOffsetOnAxis(ap=desti[:, t, k:k + 1], axis=0),
                        in_=xb[b][:, :], in_offset=None))(t, k, b), "c_sc%d" % b, ["xb%d" % b], ["buf_%d_%d" % (t, k)])
            w1v = w1.rearrange("e (p h c) f -> (e p h) (c f)", h=2, c=4)
            w3v = w3.rearrange("e (p h c) f -> (e p h) (c f)", h=2, c=4)
            w2v = w2.rearrange("e (p h c) n -> (e p h) (c n)", h=2, c=2)
            mod_sb, sel_sb, psb = load_mod_bcast(nc, P, s4, modi, sel, 5120, 1024, "m", psb=psy)
            for r in range(2):
                mod_broadcast(P, GT2b[:, r, :], psb, sel_sb, mod_sb, r, 0, None, [], "GT2b", 0, pname="psy")

            def stage_w(blk):
                wb = blk % NW
                for (wv_, Wt, nm) in ((w1v, W1b, "W1"), (w3v, W3b, "W3"), (w2v, W2b, "W2")):
                    for hh, idx in ((0, idxA), (1, idxB)):
                        P.dma("gpsimd", (lambda wv_, Wt, hh, idx, blk, wb: lambda e: e.indirect_dma_start(
                            out=Wt[wb][:, hh * 2048:(hh + 1) * 2048], out_offset=None, in_=wv_[:, :],
                            in_offset=bass.IndirectOffsetOnAxis(ap=idx[:, blk:blk + 1], axis=0)))(wv_, Wt, hh, idx, blk, wb),
                            "c_%s%d_%d" % (nm, wb, hh), [], ["%s%d_%d" % (nm, wb, hh)])

            def stage_a(blk):
                b = blk % 2
                wb = blk % NW
                DMA(P, "sync", xb[b][:], buf[blk * 128:(blk + 1) * 128, :], "c_xb%d" % b,
                    ["buf_%d_%d" % (t_, k_) for t_ in range(NT) for k_ in range(2)], ["xb%d" % b])
                xv = xb[b][:].rearrange("s (p c) -> s c p", c=8)
                for c in range(8):
                    TR(P, pxT[:, c, :], xv[:, c, :], ident[:], ["xb%d" % b], ["pxT"])
                CP(P, "vector", xbT[b][:], pxT[:], ["pxT"], ["xbT%d" % b])
                W1v = W1b[wb][:].rearrange("p (cc j q) -> p cc j q", cc=8, q=4)
                W3v = W3b[wb][:].rearrange("p (cc j q) -> p cc j q", cc=8, q=4)
                for cq in range(4):
                    for c in range(8):
                        MM(P, ps1[b][:, cq, :], W1v[:, c, :, cq], xbT[b][:, c, :], c == 0, c == 7,
                           ["W1%d_0" % wb, "W1%d_1" % wb, "xbT%d" % b], ["ps1_%d" % b])
                for cq in range(4):
                    for c in range(8):
                        MM(P, ps3[b][:, cq, :], W3v[:, c, :, cq], xbT[b][:, c, :], c == 0, c == 7,
                           ["W3%d_0" % wb, "W3%d_1" % wb, "xbT%d" % b], ["ps3_%d" % b])

            def stage_b(blk):
                b = blk % 2
                wb = blk % NW
                ACT(P, s1t[:], ps1[b][:].rearrange("p a s -> p (a s)"), AF.Silu, ["ps1_%d" % b], ["s1t"])
                TT(P, "vector", gT[b][:].rearrange("p a s -> p (a s)"), s1t[:], ps3[b][:].rearrange("p a s -> p (a s)"),
                   ALU.mult, ["s1t", "ps3_%d" % b], ["gT%d" % b])
                for half in range(2):
                    for cq in range(4):
                        MM(P, psy[half][:], gT[b][:, cq, :], W2b[wb][:, cq * 1024 + half * 512: cq * 1024 + (half + 1) * 512],
                           cq == 0, cq == 3, ["gT%d" % b, "W2%d_0" % wb, "W2%d_1" % wb], ["psy%d" % half])
                    CP(P, "scalar" if half == 0 else "vector", ysb[b][:, half * 512:(half + 1) * 512], psy[half][:],
                       ["psy%d" % half], ["ysb%d" % b])
                DMA(P, "sync", ybuf[blk * 128:(blk + 1) * 128, :], ysb[b][:], "c_yb%d" % b, ["ysb%d" % b], ["ybuf%d" % blk])

            stage_w(0)
            stage_w(1)
            stage_a(0)
            for blk in range(NBLK):
                if blk + 2 < NBLK:
                    stage_w(blk + 2)
                if blk + 1 < NBLK:
                    stage_a(blk + 1)
                stage_b(blk)
            for t in range(NT):
                b = t % 2
                r = 0 if t < 16 else 1
                for k, yt in ((0, y0), (1, y1)):
                    P.dma("gpsimd", (lambda t, k, yt, b: lambda e: e.indirect_dma_start(
                        out=yt[b][:, :], out_offset=None, in_=ybuf[:, :],
                        in_offset=bass.IndirectOffsetOnAxis(ap=desti[:, t, k:k + 1], axis=0)))(t, k, yt, b),
                        "c_y%d_%d" % (k, b), ["ybuf%d" % q_ for q_ in range(NBLK)], ["y%d_%d" % (k, b)])
                DMA(P, "sync", xc[b][:], x1s[t * 128:(t + 1) * 128, :], "c_xc%d" % b, [], ["xc%d" % b])
                TS(P, "vector", f0[:], y0[b][:], wts[:, t, 0:1], ALU.mult, ["y0_%d" % b], ["f0"])
                STT(P, f1[:], y1[b][:], wts[:, t, 1:2], f0[:], ALU.mult, ALU.add, ["y1_%d" % b, "f0"], ["f1"])
                TT(P, "gpsimd", f2[:], f1[:], GT2b[:, r, :], ALU.mult, ["f1", "GT2b"], ["f0"])
                TT(P, "vector", xo_sb[b][:], f2[:], xc[b][:], ALU.add, ["f0", "xc%d" % b], ["xo%d" % b])
                DMA(P, "sync", xo[t * 128:(t + 1) * 128, :], xo_sb[b][:], "c_xo%d" % b, ["xo%d" % b], [])
            P.emit()
        sM.close()
    return nc


def post_consts():
    k = np.arange(128)
    tri128 = (k[:, None] < k[None, :]).astype(NPBF)
    e = np.arange(32)
    tri32 = (e[:, None] < e[None, :]).astype(NPBF)
    thr = np.ascontiguousarray(np.broadcast_to((128.0 * np.arange(36)).astype(np.float32), (128, 36)))
    biota = np.ascontiguousarray(np.broadcast_to(np.arange(NBLK, dtype=np.float32), (128, NBLK)))
    piota = (2.0 * np.arange(128, dtype=np.float32)).reshape(128, 1)
    sel = np.zeros((2, 2, 128), np.float32)
    sel[0, 0] = 1
    sel[1, 1] = 1
    q = np.arange(128)
    mlo = np.tile((k[:, None] >= q[None, :]).astype(NPBF), (1, 4))
    mhi = np.tile((k[:, None] <= q[None, :]).astype(NPBF), (1, 4))
    return dict(tri128=tri128, tri32=tri32, thr=thr, biota=biota, piota=piota, sel=sel), dict(mlo=mlo, mhi=mhi)


def post_inputs(inp, l, x, ctx, qkvs, mod):
    i = l // 2
    kind = "ab" if l % 2 == 0 else "c"
    common, masks = post_consts()
    common.update({
        "modi": np.ascontiguousarray(mod),
        "wout": (inp["w_out_ab"][i] if kind == "ab" else inp["w_out_c"][i]),
        "nffn": np.ascontiguousarray(np.broadcast_to(inp["norm_ffn"][l], (128, D))),
        "wr": np.ascontiguousarray(np.concatenate([inp["w_group"][l], inp["w_expert"][l]], axis=1)),
        "br": np.ascontiguousarray(np.broadcast_to(np.concatenate([inp["b_group"][l], inp["b_expert"][l]]), (128, 36))),
        "w1": inp["w1"][l], "w3": inp["w3"][l], "w2": inp["w2"][l],
    })
    maps = []
    if kind == "ab":
        common.update(masks)
        common["sink"] = np.ascontiguousarray(inp["sink_a"][i].reshape(1, 8))
        lat = [q[:OWN] for q in qkvs]
        cx = qkvs[0][OWN:]
        kb_all = np.concatenate([q_[:, 1152:1280] for q_ in lat] + [cx[:, 1152:1280]], axis=0)
        vb_all = np.concatenate([q_[:, 1408:1536] for q_ in lat] + [cx[:, 1408:1536]], axis=0)
        ktb = np.ascontiguousarray(kb_all.T)
        vb1 = np.concatenate([vb_all.reshape(NKT * 128, 2, 64), np.ones((NKT * 128, 2, 1), NPBF)], axis=-1)
        vb = np.ascontiguousarray(vb1.reshape(NKT, 128, 2, 65).transpose(1, 0, 2, 3))

        def qpad(qcols):
            qt = qcols.reshape(NT, 128, 2, 4, 64).transpose(4, 2, 0, 3, 1).reshape(64, 2, NT, 512)
            out = np.zeros((128, 2, NT, 512), NPBF)
            out[0:64, 0] = qt[:, 0]
            out[64:128, 1] = qt[:, 1]
            return out
        zero = np.zeros((128, 128), NPBF)
        for c in range(NCORES):
            q = qkvs[c]
            qta = qpad(q[:, 0:512])
            qtb = qpad(q[:, 512:1024])

            def halo(cols):
                left = qkvs[c - 1][OWN - 128:OWN, cols] if c > 0 else zero
                right = qkvs[c + 1][0:128, cols] if c < NCORES - 1 else zero
                return np.concatenate([left, q[:OWN, cols], right, cx[:, cols]], axis=0)
            ka = halo(slice(1024, 1152))
            va_ = halo(slice(1280, 1408))
            kta = np.ascontiguousarray(ka.T)
            va1 = np.concatenate([va_.reshape(2560, 2, 64), np.ones((2560, 2, 1), NPBF)], axis=-1)
            va = np.ascontiguousarray(va1.reshape(20, 128, 2, 65).transpose(1, 0, 2, 3))
            flags = np.ones((128, 2), np.float32)
            if c == 0:
                flags[:, 0] = 0
            if c == NCORES - 1:
                flags[:, 1] = 0
            m = dict(common)
            m.update(xin=np.ascontiguousarray(np.concatenate([x[c * OWN:(c + 1) * OWN], ctx], axis=0)),
                     qta=qta, qtb=qtb, kta=kta, va=va, ktb=ktb, vb=vb, flags=flags)
            maps.append(m)
    else:
        lam_init = 0.8 - 0.6 * math.exp(-0.3 * l)
        common["lamp"] = np.ascontiguousarray(np.broadcast_to(inp["lam_c"][i], (128, 4, 64)))
        common["laminit"] = np.full((128, 1), lam_init, np.float32)
        common["oml"] = np.full((128, 1), 1.0 - lam_init, np.float32)
        common["subln"] = np.ascontiguousarray(inp["subln_c"][i].reshape(128, 1))
        lat = [q[:OWN] for q in qkvs]
        cx = qkvs[0][OWN:]
        k_all = np.concatenate([q_[:, 1024:2048] for q_ in lat] + [cx[:, 1024:2048]], axis=0)
        v_all = np.concatenate([q_[:, 2048:3072] for q_ in lat] + [cx[:, 2048:3072]], axis=0)
        ktc = np.ascontiguousarray(k_all.reshape(NKT * 128, 8, 128).transpose(1, 2, 0))
        vc = np.ascontiguousarray(v_all.reshape(NKT, 128, 8, 128).transpose(2, 1, 0, 3))
        for c in range(NCORES):
            q = qkvs[c]
            qt = q[:, 0:1024].reshape(TOK, 8, 128).transpose(2, 1, 0)
            qtc = np.zeros((2, 128, 8, TOK), NPBF)
            qtc[0, 0:64] = qt[0:64]
            qtc[1, 64:128] = qt[64:128]
            m = dict(common)
            m.update(xin=np.ascontiguousarray(np.concatenate([x[c * OWN:(c + 1) * OWN], ctx], axis=0)),
                     qtc=qtc, ktc=ktc, vc=vc)
            maps.append(m)
    return maps


def run_layer(inp, l, x, ctx):
    kind = "ab" if l % 2 == 0 else "c"
    pre = get_prog(("pre", kind), lambda: build_pre(kind))
    res = run_bass_kernel_spmd(pre, pre_inputs(inp, l, x, ctx), core_ids=list(range(NCORES)))
    qkvs = [np.asarray(r["qkv"]) for r in res.results]
    mod = np.asarray(res.results[0]["modo"])
    post = get_prog(("post", kind), lambda: build_post(kind))
    res = run_bass_kernel_spmd(post, post_inputs(inp, l, x, ctx, qkvs, mod), core_ids=list(range(NCORES)))
    xo = [np.asarray(r["xo"]) for r in res.results]
    x_new = np.concatenate([o[:OWN] for o in xo], axis=0)
    ctx_new = xo[0][OWN:]
    return x_new, ctx_new


def kernel(**inputs):
    inp = {k: np.asarray(v) for k, v in inputs.items()}
    x = np.ascontiguousarray(inp["x"][0].astype(np.float32))
    ctx = np.ascontiguousarray(inp["ctx"][0].astype(np.float32))
    for l in range(4):
        x, ctx = run_layer(inp, l, x, ctx)
    return x[None].astype(np.float32)
```
